# Optimizing a Trainium2 kernel written in Bass

```python
import math
import numpy as np
import jax
import jax.numpy as jnp
from jax import lax

D_MODEL = 1024
BATCH = 32
SEQ = 2048
DEPTH = 2

MIX_W = D_MODEL // 4
N_BRANCH = 4
POOL_GROUPS = 4
POOL_WINDOWS = (2, 4, 8, 16)
POOL_GW = MIX_W // POOL_GROUPS
GLA_HEADS = 4
GLA_DK = MIX_W // (2 * GLA_HEADS)
GLA_DV = MIX_W // GLA_HEADS
GLA_LOWRANK = 16
GLA_GATE_NORM = 16.0
GLA_CHUNK = 64
GDN_HEADS = 4
GDN_DH = MIX_W // GDN_HEADS
GDN_CONV = 4
GDN_CHUNK = 64
NSA_HEADS = 4
NSA_GROUPS = 2
NSA_HPG = NSA_HEADS // NSA_GROUPS
NSA_DH = MIX_W // NSA_HEADS
NSA_KV = NSA_GROUPS * NSA_DH
NSA_CMP_LEN = 32
NSA_CMP_STRIDE = 16
NSA_SEL_LEN = 64
NSA_N_SEL = 16
NSA_WINDOW = 512
NSA_SEL_QBLOCK = 16
NSA_WIN_QBLOCK = 128
MEM_LEN = 256
X_HEADS = 4
X_DH = 128
D_FF = 2816
FFN_CONV = 3
EPS = 1e-6
IN_SPLITS = (MIX_W,
             GLA_HEADS * GLA_DK, GLA_HEADS * GLA_DK, GLA_HEADS * GLA_DV, GLA_HEADS * GLA_DV, GLA_LOWRANK,
             MIX_W, MIX_W, MIX_W, GDN_HEADS, GDN_HEADS, MIX_W,
             NSA_HEADS * NSA_DH, NSA_KV, NSA_KV, NSA_KV, NSA_KV, NSA_KV, NSA_KV, 3 * NSA_HEADS)
N_IN = sum(IN_SPLITS)

kernel_name = 'hybrid_pool_gla_gdn_nsa_block'


def rmsnorm(x, g):
    xf = x.astype(jnp.float32)
    y = xf * lax.rsqrt(jnp.mean(xf * xf, axis=-1, keepdims=True) + EPS)
    return (y * g.astype(jnp.float32)).astype(x.dtype)


def causal_dwconv(x, w):
    width = w.shape[0]
    s = x.shape[1]
    xp = jnp.pad(x, ((0, 0), (width - 1, 0), (0, 0)))
    return sum(xp[:, i:i + s] * w[i] for i in range(width))


def masked_softmax(s, mask):
    s = jnp.where(mask, s.astype(jnp.float32), -jnp.inf)
    m = jnp.max(s, axis=-1, keepdims=True)
    m = jnp.where(jnp.isfinite(m), m, 0.0)
    e = jnp.exp(s - m)
    den = jnp.sum(e, axis=-1, keepdims=True)
    return e / jnp.where(den > 0, den, 1.0)


def _chunk(t, c):
    b, s, h = t.shape[:3]
    t = t.reshape((b, s // c, c, h) + t.shape[3:])
    return jnp.moveaxis(t, 3, 1)


def _unchunk(t):
    b, h, n, c, d = t.shape
    return jnp.moveaxis(t, 1, 3).reshape(b, n * c, h, d)


def pool_mixer(u, w_grp, scale):
    b, s, _ = u.shape
    uf = u.astype(jnp.float32).reshape(b, s, POOL_GROUPS, POOL_GW)
    cs = jnp.pad(jnp.cumsum(uf, axis=1), ((0, 0), (1, 0), (0, 0), (0, 0)))
    t = jnp.arange(s)
    pooled = []
    for gi, w in enumerate(POOL_WINDOWS):
        lo = jnp.maximum(t + 1 - w, 0)
        cnt = (t + 1 - lo).astype(jnp.float32)
        csg = cs[:, :, gi]
        pooled.append((csg[:, t + 1] - csg[:, lo]) / cnt[None, :, None])
    pooled = jnp.stack(pooled, axis=2)
    mixed = jnp.einsum('bsgc,gcd->bsgd', pooled - uf, w_grp.astype(jnp.float32))
    return (mixed.reshape(b, s, MIX_W) * scale.astype(jnp.float32)).astype(u.dtype)


def gla_mixer(q, k, v, r, lr, w_lr, b_lr, g_norm):
    b, s, _ = q.shape
    h, dk, dv, c = GLA_HEADS, GLA_DK, GLA_DV, GLA_CHUNK
    f32 = jnp.float32
    q = q.astype(f32).reshape(b, s, h, dk) * dk ** -0.5
    k = k.astype(f32).reshape(b, s, h, dk)
    v = v.astype(f32).reshape(b, s, h, dv)
    gk = jax.nn.log_sigmoid(lr.astype(f32) @ w_lr.astype(f32) + b_lr.astype(f32)) / GLA_GATE_NORM
    gk = gk.reshape(b, s, h, dk)
    qc, kc, vc, gc = (_chunk(t, c) for t in (q, k, v, gk))
    bcum = jnp.cumsum(gc, axis=3)
    blast = bcum[:, :, :, -1:, :]
    q_e = qc * jnp.exp(bcum)
    k_e = kc * jnp.exp(-bcum)
    causal = jnp.tril(jnp.ones((c, c), bool))
    att = jnp.where(causal, jnp.einsum('bhnid,bhnjd->bhnij', q_e, k_e), 0.0)
    o_intra = jnp.einsum('bhnij,bhnjv->bhniv', att, vc)
    k_upd = kc * jnp.exp(blast - bcum)
    dec = jnp.exp(blast[:, :, :, 0, :])

    def step(st, inp):
        q_n, k_n, v_n, d_n = inp
        o_n = jnp.einsum('bhcd,bhdv->bhcv', q_n, st)
        st = st * d_n[..., None] + jnp.einsum('bhcd,bhcv->bhdv', k_n, v_n)
        return st, o_n

    st0 = jnp.zeros((b, h, dk, dv), f32)
    _, o_inter = lax.scan(step, st0, tuple(jnp.moveaxis(t, 2, 0) for t in (q_e, k_upd, vc, dec)))
    o = _unchunk(o_intra + jnp.moveaxis(o_inter, 0, 2))
    o = rmsnorm(o, g_norm) * jax.nn.silu(r.astype(f32).reshape(b, s, h, dv))
    return o.reshape(b, s, MIX_W).astype(r.dtype)


def gdn_mixer(q, k, v, beta_raw, a_raw, gate, conv_w, a_log, dt_bias, g_norm):
    b, s, _ = q.shape
    h, dh, c = GDN_HEADS, GDN_DH, GDN_CHUNK
    f32 = jnp.float32
    qkv = jnp.concatenate([q, k, v], axis=-1).astype(f32)
    qkv = jax.nn.silu(causal_dwconv(qkv, conv_w.astype(f32)))
    q, k, v = (t.reshape(b, s, h, dh) for t in jnp.split(qkv, 3, axis=-1))
    q = q * lax.rsqrt(jnp.sum(q * q, axis=-1, keepdims=True) + EPS) * dh ** -0.5
    k = k * lax.rsqrt(jnp.sum(k * k, axis=-1, keepdims=True) + EPS)
    beta = jax.nn.sigmoid(beta_raw.astype(f32))
    g = -jnp.exp(a_log.astype(f32)) * jax.nn.softplus(a_raw.astype(f32) + dt_bias.astype(f32))
    qc, kc, vc, bc, gc = (_chunk(t, c) for t in (q, k, v, beta, g))
    gcum = jnp.cumsum(gc, axis=-1)
    incl = jnp.tril(jnp.ones((c, c), bool))
    strict = jnp.tril(jnp.ones((c, c), bool), -1)
    decay = jnp.exp(jnp.where(incl, gcum[..., :, None] - gcum[..., None, :], -jnp.inf))
    kb = kc * bc[..., None]
    vb = vc * bc[..., None]
    a_mat = jnp.where(strict, jnp.einsum('bhnid,bhnjd->bhnij', kb, kc) * decay, 0.0) + jnp.eye(c, dtype=f32)
    u = lax.linalg.triangular_solve(a_mat, vb, left_side=True, lower=True, unit_diagonal=True)
    w = lax.linalg.triangular_solve(a_mat, kb * jnp.exp(gcum)[..., None], left_side=True, lower=True,
                                    unit_diagonal=True)
    a_qk = jnp.einsum('bhnid,bhnjd->bhnij', qc, kc) * decay
    q_dec = qc * jnp.exp(gcum)[..., None]
    k_dec = kc * jnp.exp(gcum[..., -1:] - gcum)[..., None]
    dec = jnp.exp(gcum[..., -1])

    def step(st, inp):
        u_n, w_n, qd_n, aqk_n, kd_n, d_n = inp
        v_new = u_n - jnp.einsum('bhcd,bhdv->bhcv', w_n, st)
        o_n = jnp.einsum('bhcd,bhdv->bhcv', qd_n, st) + jnp.einsum('bhij,bhjv->bhiv', aqk_n, v_new)
        st = st * d_n[..., None, None] + jnp.einsum('bhcd,bhcv->bhdv', kd_n, v_new)
        return st, o_n

    st0 = jnp.zeros((b, h, dh, dh), f32)
    _, o = lax.scan(step, st0, tuple(jnp.moveaxis(t, 2, 0) for t in (u, w, q_dec, a_qk, k_dec, dec)))
    o = _unchunk(jnp.moveaxis(o, 0, 2))
    o = rmsnorm(o, g_norm) * jax.nn.silu(gate.astype(f32).reshape(b, s, h, dh))
    return o.reshape(b, s, MIX_W).astype(gate.dtype)


def _compress(t, pe, w1, w2):
    b, s, g, dh = t.shape
    n_sub = NSA_CMP_LEN // NSA_CMP_STRIDE
    n_chunks = s // NSA_CMP_STRIDE
    n_cmp = n_chunks - n_sub + 1
    tc = t.reshape(b, n_chunks, NSA_CMP_STRIDE, g, dh)
    blk = jnp.concatenate([tc[:, i:i + n_cmp] for i in range(n_sub)], axis=2)
    blk = blk + pe[None, None, :, None, :]
    flat = jnp.moveaxis(blk, 3, 2).reshape(b, n_cmp, g, NSA_CMP_LEN * dh)
    return jax.nn.gelu(flat @ w1) @ w2


def nsa_mixer(q, kc, vc, ks, vs, kw, vw, gate_raw, pe, cmp_w1, cmp_w2):
    b, s, _ = q.shape
    g, j, dh = NSA_GROUPS, NSA_HPG, NSA_DH
    f32 = jnp.float32
    scale = dh ** -0.5
    q = q.astype(f32).reshape(b, s, g, j, dh)
    kc, vc, ks, vs, kw, vw = (t.astype(f32).reshape(b, s, g, dh) for t in (kc, vc, ks, vs, kw, vw))
    t_pos = jnp.arange(s)

    ck = _compress(kc, pe[0], cmp_w1[0], cmp_w2[0])
    cv = _compress(vc, pe[1], cmp_w1[1], cmp_w2[1])
    n_cmp = ck.shape[1]
    cmp_end = jnp.arange(n_cmp) * NSA_CMP_STRIDE + NSA_CMP_LEN - 1
    p_cmp = masked_softmax(jnp.einsum('bsgjd,bngd->bgjsn', q, ck) * scale,
                           cmp_end[None, :] <= t_pos[:, None])
    o_cmp = jnp.einsum('bgjsn,bngd->bsgjd', p_cmp, cv)

    n_slc = s // NSA_SEL_LEN
    c_start = np.arange(n_cmp) * NSA_CMP_STRIDE
    s_start = np.arange(n_slc) * NSA_SEL_LEN
    cover = ((c_start[:, None] <= s_start[None, :] + NSA_SEL_LEN - 1)
             & (c_start[:, None] + NSA_CMP_LEN - 1 >= s_start[None, :])).astype(np.float32)
    imp = jnp.einsum('bgjsn,nm->bgsm', p_cmp, jnp.asarray(cover))
    blk = jnp.arange(n_slc)[None, :]
    cur = (t_pos // NSA_SEL_LEN)[:, None]
    forced = (blk == 0) | (blk == cur) | (blk == cur - 1)
    imp = jnp.where(forced, jnp.inf, jnp.where(blk > cur, -jnp.inf, imp))
    n_top = min(NSA_N_SEL, n_slc)
    _, idx = lax.top_k(imp, n_top)

    qb_len = NSA_SEL_QBLOCK
    nqb = s // qb_len
    ks_b = jnp.moveaxis(ks.reshape(b, n_slc, NSA_SEL_LEN, g, dh), 3, 1)
    vs_b = jnp.moveaxis(vs.reshape(b, n_slc, NSA_SEL_LEN, g, dh), 3, 1)
    gather = jax.vmap(jax.vmap(lambda tab, ii: tab[ii]))
    q_blocks = jnp.moveaxis(q.reshape(b, nqb, qb_len, g, j, dh), 1, 0)
    idx_blocks = jnp.moveaxis(idx.reshape(b, g, nqb, qb_len, n_top), 2, 0)
    pos_blocks = t_pos.reshape(nqb, qb_len)
    within = jnp.arange(NSA_SEL_LEN)

    def sel_block(args):
        qb, ib, pb = args
        kg = gather(ks_b, ib)
        vg = gather(vs_b, ib)
        kpos = ib[..., None] * NSA_SEL_LEN + within
        valid = (kpos <= pb[None, None, :, None, None]).reshape(b, g, 1, qb_len, n_top * NSA_SEL_LEN)
        sc = jnp.einsum('bqgjd,bgqkld->bgjqkl', qb, kg) * scale
        p = masked_softmax(sc.reshape(b, g, j, qb_len, n_top * NSA_SEL_LEN), valid).reshape(sc.shape)
        return jnp.einsum('bgjqkl,bgqkld->bqgjd', p, vg)

    o_slc = lax.map(sel_block, (q_blocks, idx_blocks, pos_blocks))
    o_slc = jnp.moveaxis(o_slc, 0, 1).reshape(b, s, g, j, dh)

    qw_len = NSA_WIN_QBLOCK
    span = NSA_WINDOW + qw_len
    nqw = s // qw_len
    kw_p = jnp.pad(kw, ((0, 0), (NSA_WINDOW, 0), (0, 0), (0, 0)))
    vw_p = jnp.pad(vw, ((0, 0), (NSA_WINDOW, 0), (0, 0), (0, 0)))
    qw_blocks = jnp.moveaxis(q.reshape(b, nqw, qw_len, g, j, dh), 1, 0)
    starts = jnp.arange(nqw) * qw_len

    def win_block(args):
        qb, st = args
        kb = lax.dynamic_slice_in_dim(kw_p, st, span, axis=1)
        vb = lax.dynamic_slice_in_dim(vw_p, st, span, axis=1)
        qp = st + jnp.arange(qw_len)
        kp = st - NSA_WINDOW + jnp.arange(span)
        valid = (kp[None, :] <= qp[:, None]) & (kp[None, :] > qp[:, None] - NSA_WINDOW) & (kp[None, :] >= 0)
        p = masked_softmax(jnp.einsum('bqgjd,bkgd->bgjqk', qb, kb) * scale, valid)
        return jnp.einsum('bgjqk,bkgd->bqgjd', p, vb)

    o_win = lax.map(win_block, (qw_blocks, starts))
    o_win = jnp.moveaxis(o_win, 0, 1).reshape(b, s, g, j, dh)

    gt = jax.nn.sigmoid(gate_raw.astype(f32)).reshape(b, s, g, j, 3)
    o = gt[..., 0:1] * o_cmp + gt[..., 1:2] * o_slc + gt[..., 2:3] * o_win
    return o.reshape(b, s, MIX_W).astype(gate_raw.dtype)


def mem_cross_attn(h, mem_n, w_q, w_kv, w_o):
    b, s, _ = h.shape
    m = mem_n.shape[1]
    q = (h @ w_q).reshape(b, s, X_HEADS, X_DH)
    kv = (mem_n @ w_kv).reshape(b, m, 2, X_HEADS, X_DH)
    sc = jnp.einsum('bshd,bmhd->bhsm', q, kv[:, :, 0]).astype(jnp.float32) * X_DH ** -0.5
    p = jax.nn.softmax(sc, axis=-1).astype(kv.dtype)
    o = jnp.einsum('bhsm,bmhd->bshd', p, kv[:, :, 1]).reshape(b, s, X_HEADS * X_DH)
    return o @ w_o


def conv_ffn(h, w_up, conv_w, conv_b, w_down):
    u, v = jnp.split(h @ w_up, 2, axis=-1)
    u = causal_dwconv(u, conv_w) + conv_b
    return (jax.nn.gelu(u) * v) @ w_down


def setup_inputs(seed: int = 0) -> dict:
    key = jax.random.key(seed)
    k = jax.random.split(key, 32)
    f32 = jnp.float32
    L = DEPTH

    def nrm(i, shape, fan_in):
        return jax.random.normal(k[i], shape, f32) * fan_in ** -0.5

    def gain(i, shape):
        return 1.0 + 0.02 * jax.random.normal(k[i], shape, f32)

    def small(i, shape):
        return 0.01 * jax.random.normal(k[i], shape, f32)

    dt = jnp.exp(jax.random.uniform(k[11], (L, GDN_HEADS), f32, math.log(1e-3), math.log(1e-1)))
    return {
        'x': jax.random.normal(k[0], (BATCH, SEQ, D_MODEL), f32),
        'mem': jax.random.normal(k[1], (BATCH, MEM_LEN, D_MODEL), f32),
        'g_mix': gain(2, (L, D_MODEL)),
        'w_in': nrm(3, (L, D_MODEL, N_IN), D_MODEL),
        'pool_w': nrm(4, (L, POOL_GROUPS, POOL_GW, POOL_GW), POOL_GW),
        'pool_scale': gain(5, (L, MIX_W)),
        'gla_w_lr': nrm(6, (L, GLA_LOWRANK, GLA_HEADS * GLA_DK), GLA_LOWRANK),
        'gla_b_lr': small(7, (L, GLA_HEADS * GLA_DK)),
        'gla_g_norm': gain(8, (L, GLA_DV)),
        'gdn_conv': nrm(9, (L, GDN_CONV, 3 * MIX_W), GDN_CONV),
        'gdn_a_log': jnp.log(jax.random.uniform(k[10], (L, GDN_HEADS), f32, 1.0, 16.0)),
        'gdn_dt_bias': dt + jnp.log(-jnp.expm1(-dt)),
        'gdn_g_norm': gain(12, (L, GDN_DH)),
        'nsa_pe': 0.1 * jax.random.normal(k[13], (L, 2, NSA_CMP_LEN, NSA_DH), f32),
        'nsa_cmp_w1': nrm(14, (L, 2, NSA_CMP_LEN * NSA_DH, NSA_DH), NSA_CMP_LEN * NSA_DH),
        'nsa_cmp_w2': nrm(15, (L, 2, NSA_DH, NSA_DH), NSA_DH),
        'w_branch': nrm(16, (L, N_BRANCH, MIX_W, D_MODEL), MIX_W),
        'w_gate': nrm(17, (L, N_BRANCH, D_MODEL, D_MODEL), D_MODEL),
        'b_gate': small(18, (L, N_BRANCH, D_MODEL)),
        'w_out': nrm(19, (L, D_MODEL, D_MODEL), D_MODEL),
        'g_cross': gain(20, (L, D_MODEL)),
        'g_mem': gain(21, (L, D_MODEL)),
        'w_xq': nrm(22, (L, D_MODEL, X_HEADS * X_DH), D_MODEL),
        'w_mem_kv': nrm(23, (L, D_MODEL, 2 * X_HEADS * X_DH), D_MODEL),
        'w_xo': nrm(24, (L, X_HEADS * X_DH, D_MODEL), X_HEADS * X_DH),
        'g_ffn': gain(25, (L, D_MODEL)),
        'w_up': nrm(26, (L, D_MODEL, 2 * D_FF), D_MODEL),
        'ffn_conv': nrm(27, (L, FFN_CONV, D_FF), FFN_CONV),
        'ffn_conv_b': small(28, (L, D_FF)),
        'w_down': nrm(29, (L, D_FF, D_MODEL), D_FF),
        'g_final': gain(30, (D_MODEL,)),
    }


def reference(x, mem, g_mix, w_in, pool_w, pool_scale, gla_w_lr, gla_b_lr, gla_g_norm, gdn_conv,
              gdn_a_log, gdn_dt_bias, gdn_g_norm, nsa_pe, nsa_cmp_w1, nsa_cmp_w2, w_branch, w_gate,
              b_gate, w_out, g_cross, g_mem, w_xq, w_mem_kv, w_xo, g_ffn, w_up, ffn_conv, ffn_conv_b,
              w_down, g_final):
    bounds = [int(v) for v in np.cumsum(IN_SPLITS)[:-1]]
    for l in range(DEPTH):
        h = rmsnorm(x, g_mix[l])
        z = h @ w_in[l]
        (p_in, a_q, a_k, a_v, a_r, a_lr, d_q, d_k, d_v, d_b, d_a, d_g,
         n_q, n_kc, n_vc, n_ks, n_vs, n_kw, n_vw, n_g) = jnp.split(z, bounds, axis=-1)
        branches = [
            pool_mixer(p_in, pool_w[l], pool_scale[l]),
            gla_mixer(a_q, a_k, a_v, a_r, a_lr, gla_w_lr[l], gla_b_lr[l], gla_g_norm[l]),
            gdn_mixer(d_q, d_k, d_v, d_b, d_a, d_g, gdn_conv[l], gdn_a_log[l], gdn_dt_bias[l], gdn_g_norm[l]),
            nsa_mixer(n_q, n_kc, n_vc, n_ks, n_vs, n_kw, n_vw, n_g, nsa_pe[l], nsa_cmp_w1[l], nsa_cmp_w2[l]),
        ]
        y = 0.0
        for i in range(N_BRANCH):
            gate = jax.nn.sigmoid(h @ w_gate[l, i] + b_gate[l, i])
            y = y + gate * (branches[i] @ w_branch[l, i])
        x = x + y @ w_out[l]
        x = x + mem_cross_attn(rmsnorm(x, g_cross[l]), rmsnorm(mem, g_mem[l]), w_xq[l], w_mem_kv[l], w_xo[l])
        x = x + conv_ffn(rmsnorm(x, g_ffn[l]), w_up[l], ffn_conv[l], ffn_conv_b[l], w_down[l])
    return rmsnorm(x, g_final)
```

```python
from contextlib import ExitStack
import numpy as np
import concourse.bass as bass
import concourse.mybir as mybir
from concourse.bass_utils import run_bass_kernel_spmd

F32 = mybir.dt.float32
BF16 = mybir.dt.bfloat16
AF = mybir.ActivationFunctionType
ALU = mybir.AluOpType

ENGS = ("pe", "act", "dve", "pool", "sp")
NDMASEM = 8

T = 2048
D = 1024
KC = 8
NB = 4
NT = 16
L = 2
DFF = 2816
NF = 22
EPS = 1e-6


class Op:
    __slots__ = ("eng", "fn", "reads", "writes", "dma", "deps", "signal", "sigval", "sem", "prewait", "bar")

    def __init__(self, eng, fn, reads, writes, dma):
        self.eng, self.fn, self.reads, self.writes, self.dma = eng, fn, reads, writes, dma
        self.deps = []
        self.signal = False
        self.sigval = None
        self.sem = None
        self.prewait = None
        self.bar = False


class V:
    __slots__ = ("ap", "key")

    def __init__(self, ap, key):
        self.ap, self.key = ap, key

    def __getitem__(self, idx):
        return V(self.ap[idx], self.key)

    def sub(self, k):
        return V(self.ap, (self.key, k))

    def bc(self, shape):
        return V(self.ap.to_broadcast(list(shape)), self.key)

    def re(self, s, **kw):
        return V(self.ap.rearrange(s, **kw), self.key)


def _k(*vs):
    out = []
    for v in vs:
        if isinstance(v, V):
            out.append(v.key)
    return out


def _a(v):
    return v.ap if isinstance(v, V) else v


class Prog:
    def __init__(self, nc):
        self.nc = nc
        self.ops = []
        self.es = ExitStack()
        self.uid = 0
        self.excl = set()

    def sb(self, name, shape, dt=F32):
        t = self.es.enter_context(self.nc.sbuf_tensor("sb_" + name, list(shape), dt))
        return t

    def ps(self, name, shape, dt=F32):
        return self.es.enter_context(self.nc.psum_tensor(name, list(shape), dt))

    def op(self, eng, fn, reads=(), writes=(), dma=False):
        o = Op(eng, fn, tuple(reads), tuple(writes), dma)
        self.ops.append(o)
        return o

    def barrier(self):
        o = Op("sp", None, (), (), False)
        o.bar = True
        self.ops.append(o)

    def dma(self, q, out, in_, **kw):
        oa, ia = _a(out), _a(in_)
        return self.op(q, lambda e: e.dma_start(out=oa, in_=ia, **kw), _k(in_), _k(out), dma=True)

    def mm(self, out, lhsT, rhs, start=True, stop=True):
        oa, la, ra = out.ap, lhsT.ap, rhs.ap
        return self.op("pe", lambda e: e.matmul(oa, lhsT=la, rhs=ra, start=start, stop=stop),
                       _k(lhsT, rhs) + ([] if start else _k(out)), _k(out))

    def tr(self, out, in_, ident):
        oa, ia, da = out.ap, in_.ap, ident.ap
        return self.op("pe", lambda e: e.transpose(oa, ia, da), _k(in_, ident), _k(out))

    def act(self, out, in_, func, bias=0.0, scale=1.0, accum=None):
        oa, ia, ba, sa = out.ap, in_.ap, _a(bias), _a(scale)
        ca = _a(accum) if accum is not None else None

        def fn(e):
            if ca is not None:
                return e.activation(out=oa, in_=ia, func=func, bias=ba, scale=sa, accum_out=ca)
            return e.activation(out=oa, in_=ia, func=func, bias=ba, scale=sa)
        return self.op("act", fn, _k(in_, bias, scale), _k(out) + (_k(accum) if accum is not None else []))

    def tt(self, eng, out, a, b, op):
        oa, aa, ba = out.ap, a.ap, b.ap
        return self.op(eng, lambda e: e.tensor_tensor(out=oa, in0=aa, in1=ba, op=op), _k(a, b), _k(out))

    def ts(self, eng, out, a, s1, op0, s2=None, op1=None):
        oa, aa, s1a, s2a = out.ap, a.ap, _a(s1), _a(s2)

        def fn(e):
            if op1 is None:
                return e.tensor_scalar(out=oa, in0=aa, scalar1=s1a, scalar2=None, op0=op0)
            return e.tensor_scalar(out=oa, in0=aa, scalar1=s1a, scalar2=s2a, op0=op0, op1=op1)
        return self.op(eng, fn, _k(a, s1, s2), _k(out))

    def stt(self, eng, out, a, scalar, b, op0, op1):
        oa, aa, sa, ba = out.ap, a.ap, _a(scalar), b.ap
        eng = "dve"
        return self.op(eng, lambda e: e.scalar_tensor_tensor(out=oa, in0=aa, scalar=sa, in1=ba, op0=op0, op1=op1),
                       _k(a, scalar, b), _k(out))

    def cp(self, eng, out, in_):
        oa, ia = out.ap, in_.ap
        if eng == "act":
            return self.op(eng, lambda e: e.copy(oa, ia), _k(in_), _k(out))
        return self.op(eng, lambda e: e.tensor_copy(oa, ia), _k(in_), _k(out))

    def memset(self, eng, out, val):
        oa = out.ap
        return self.op(eng, lambda e: e.memset(oa, val), (), _k(out))

    def scan(self, out, d0, d1, init, op0, op1):
        oa, a0, a1 = out.ap, d0.ap, d1.ap
        return self.op("dve", lambda e: e.tensor_tensor_scan(out=oa, data0=a0, data1=a1, initial=init, op0=op0, op1=op1),
                       _k(d0, d1), _k(out))

    def finish(self):
        nc = self.nc
        last_w = {}
        readers = {}
        pend = {e: [] for e in ENGS}
        seg = []
        excl = self.excl

        def _bank(k):
            b = k
            while isinstance(b, tuple):
                b = b[0]
            return b if b in excl else None
        for o in self.ops:
            if o.bar:
                lastc = {}
                dm = []
                for p in seg:
                    if p.dma:
                        dm.append(p)
                    else:
                        lastc[p.eng] = p
                newl = [p for p in list(lastc.values()) + dm if p.fn is not None]
                for e in ENGS:
                    pend[e].extend(newl)
                seg = []
                last_w = {}
                readers = {}
                continue
            rr, ww = [], []
            for k in o.reads:
                b = _bank(k)
                if b is None:
                    rr.append(k)
                else:
                    ww.append(b)
            for k in o.writes:
                b = _bank(k)
                ww.append(k if b is None else b)
            o.reads, o.writes = tuple(rr), tuple(dict.fromkeys(ww))
            seg.append(o)
            deps = set()
            for k in o.reads:
                w = last_w.get(k)
                if w is not None:
                    deps.add(w)
            for k in o.writes:
                w = last_w.get(k)
                if w is not None:
                    deps.add(w)
                for r in readers.get(k, ()):
                    deps.add(r)
            deps.discard(o)
            if o.eng == "pe":
                deps = {d for d in deps if not (d.eng == "pe" and not d.dma)}
            if pend[o.eng]:
                deps.update(pend[o.eng])
                pend[o.eng] = []
            deps.discard(o)
            o.deps = list(deps)
            for d in deps:
                d.signal = True
            for k in o.reads:
                readers.setdefault(k, []).append(o)
            for k in o.writes:
                last_w[k] = o
                readers[k] = []
        self.ops = [o for o in self.ops if not o.bar]
        streams = {e: [] for e in ENGS}
        for o in self.ops:
            streams[o.eng].append(o)
        sems = {e: self.es.enter_context(nc.semaphore("s_" + e)) for e in ENGS}
        dsems = {e: [self.es.enter_context(nc.semaphore("d_%s%d" % (e, i))) for i in range(NDMASEM)] for e in ENGS
                 if any(o.dma for o in streams[e])}
        for e in ENGS:
            c = 0
            nd = 0
            for o in streams[e]:
                if o.dma:
                    o.sem = dsems[e][nd % NDMASEM]
                    o.sigval = 16 * (nd // NDMASEM + 1)
                    if nd >= NDMASEM:
                        o.prewait = (o.sem, 16 * (nd // NDMASEM))
                    nd += 1
                    o.signal = True
                elif o.signal:
                    c += 1
                    o.sem = sems[e]
                    o.sigval = c
        self.stats = {e: len(streams[e]) for e in ENGS}
        nwaits = [0]

        def emit_stream(eng, e):
            known = {}
            for o in streams[e]:
                if o.prewait is not None:
                    s, v = o.prewait
                    if known.get(id(s), 0) < v:
                        eng.wait_ge(s, v)
                        known[id(s)] = v
                        nwaits[0] += 1
                need = {}
                for d in o.deps:
                    s = d.sem
                    if s is None:
                        continue
                    if need.get(id(s), (None, 0))[1] < d.sigval:
                        need[id(s)] = (s, d.sigval)
                for sid, (s, v) in need.items():
                    if known.get(sid, 0) < v:
                        eng.wait_ge(s, v)
                        known[sid] = v
                        nwaits[0] += 1
                ins = o.fn(eng)
                if o.signal and ins is not None:
                    ins.then_inc(o.sem, 16 if o.dma else 1)

        block = self.es.enter_context(nc.Block())

        @block.tensor
        def _(eng):
            emit_stream(eng, "pe")

        @block.scalar
        def _(eng):
            emit_stream(eng, "act")

        @block.vector
        def _(eng):
            emit_stream(eng, "dve")

        @block.gpsimd
        def _(eng):
            emit_stream(eng, "pool")

        @block.sync
        def _(eng):
            emit_stream(eng, "sp")

        self.stats["waits"] = nwaits[0]
        self.es.close()


class Arena:
    def __init__(self, P, nbytes):
        self.P = P
        self.nbytes = nbytes
        self.t16 = P.sb("arena", [128, nbytes // 2], BF16)
        self.t32 = self.t16.bitcast(F32)
        self.off = 0
        self.gen = 0
        self.peak = 0
        self.top_live = False
        self.reserved_top = 0

    def reset(self):
        self.P.barrier()
        self.off = 0
        self.gen += 1

    def mark(self):
        return self.off

    def release(self, m):
        self.P.barrier()
        self.off = m
        self.gen += 1

    def top_view(self, name, free_shape, nbytes):
        e0 = (self.nbytes - nbytes) // 4
        n = nbytes // 4
        ap = self.t32[:, e0:e0 + n].rearrange("p (a b) -> p a b", a=free_shape[0])
        return V(ap, name)

    def alloc(self, name, free_shape, dt=F32):
        n = 1
        for s in free_shape:
            n *= s
        esz = 4 if dt == F32 else 2
        nb = (n * esz + 63) // 64 * 64
        lim = self.nbytes - (self.reserved_top if self.top_live else 0)
        assert self.off + nb <= lim, "arena overflow %s: %d + %d > %d" % (name, self.off, nb, lim)
        base = self.t32 if dt == F32 else self.t16
        e0 = self.off // esz
        ap = base[:, e0:e0 + n]
        self.off += nb
        self.peak = max(self.peak, self.off)
        if len(free_shape) == 2:
            ap = ap.rearrange("p (a b) -> p a b", a=free_shape[0])
        elif len(free_shape) == 3:
            ap = ap.rearrange("p (a b c) -> p a b c", a=free_shape[0], b=free_shape[1])
        return V(ap, "%s#%d" % (name, self.gen))


POOL_WINDOWS = (2, 4, 8, 16)
NFM = 27
(FM_PIN, FM_AQ, FM_AKP, FM_AK, FM_AR, FM_S3, FM_S1, FM_S2, FM_DQ, FM_DK, FM_DV, FM_DG, FM_NQ, FM_NKC, FM_NVC, FM_NKS,
 FM_NKW) = (0, 2, 3, 7, 8, 10, 11, 12, 13, 15, 17, 19, 21, 23, 24, 25, 26)
C_GMIX, C_GCROSS, C_GFFN, C_GMEM, C_BGATE, C_POOLSC, C_POOLINVW, C_GLA_B, C_GLA_GN = 0, 8, 16, 24, 32, 64, 66, 68, 69
C_GDN_CONV, C_GDN_ALOG, C_GDN_DTB, C_GDN_GN, C_FFN_CONV, C_FFN_B, NCST = 70, 94, 95, 96, 100, 166, 192
_off = np.cumsum([0, 256, 128, 128, 256, 256, 16, 256, 256, 256, 4, 4, 256, 256, 128, 128, 128, 128, 128, 128, 12])
(O_PIN, O_AQ, O_AK, O_AV, O_AR, O_ALR, O_DQ, O_DK, O_DV, O_DB, O_DA, O_DG, O_NQ, O_NKC, O_NVC, O_NKS, O_NVS, O_NKW,
 O_NVW, O_NG) = [int(v) for v in _off[:-1]]


def _fm(W):
    K, n = W.shape
    return np.ascontiguousarray(W.reshape(K // 128, 128, n // 128, 128).transpose(2, 1, 0, 3))


def _col(v):
    return np.ascontiguousarray(v.reshape(-1, 128).T)


def host_prep(inp):
    f32 = np.float32
    w = {}
    w_in = inp["w_in"]
    fm = np.zeros((L, NFM, 128, KC, 128), f32)
    tm = np.zeros((L, 128, KC, 512), f32)
    cst = np.zeros((128, L, NCST), f32)
    poolbd = np.zeros((L, 2, 128, 128), f32)
    for l in range(L):
        W = w_in[l]

        def cols(o, n):
            return W[:, o:o + n]

        def put(idx, Wc):
            n = Wc.shape[1]
            pad = np.zeros((D, 128), f32)
            pad[:, :n] = Wc
            fm[l, idx] = _fm(pad)[0]
        put(FM_PIN, cols(O_PIN, 128)); put(FM_PIN + 1, cols(O_PIN + 128, 128))
        put(FM_AQ, cols(O_AQ, 128))
        for h in range(4):
            pad = np.zeros((D, 128), f32)
            pad[:, 32 * h:32 * h + 32] = cols(O_AK + 32 * h, 32)
            put(FM_AKP + h, pad)
        put(FM_AK, cols(O_AK, 128))
        put(FM_AR, cols(O_AR, 128)); put(FM_AR + 1, cols(O_AR + 128, 128))
        s3 = np.zeros((D, 128), f32); s3[:, 0:12] = cols(O_NG, 12); s3[:, 32:48] = cols(O_ALR, 16); put(FM_S3, s3)
        s1 = np.zeros((D, 128), f32)
        s2 = np.zeros((D, 128), f32)
        for r in (0, 32, 64):
            s1[:, r:r + 4] = cols(O_DA, 4)
            s2[:, r:r + 4] = cols(O_DB, 4)
        put(FM_S1, s1); put(FM_S2, s2)
        for j in range(2):
            put(FM_DQ + j, cols(O_DQ + 128 * j, 128)); put(FM_DK + j, cols(O_DK + 128 * j, 128))
            put(FM_DV + j, cols(O_DV + 128 * j, 128)); put(FM_DG + j, cols(O_DG + 128 * j, 128))
            put(FM_NQ + j, cols(O_NQ + 128 * j, 128))
        put(FM_NKC, cols(O_NKC, 128)); put(FM_NVC, cols(O_NVC, 128)); put(FM_NKS, cols(O_NKS, 128)); put(FM_NKW, cols(O_NKW, 128))
        tmw = np.concatenate([cols(O_AV, 256), cols(O_NVS, 128), cols(O_NVW, 128)], axis=1)
        tm[l] = tmw.reshape(KC, 128, 512).transpose(1, 0, 2)
        cst[:, l, C_GMIX:C_GMIX + 8] = _col(inp["g_mix"][l])
        cst[:, l, C_GCROSS:C_GCROSS + 8] = _col(inp["g_cross"][l])
        cst[:, l, C_GFFN:C_GFFN + 8] = _col(inp["g_ffn"][l])
        cst[:, l, C_GMEM:C_GMEM + 8] = _col(inp["g_mem"][l])
        for i in range(4):
            cst[:, l, C_BGATE + 8 * i:C_BGATE + 8 * i + 8] = _col(inp["b_gate"][l, i])
        cst[:, l, C_POOLSC:C_POOLSC + 2] = _col(inp["pool_scale"][l])
        cst[:, l, C_GLA_B] = inp["gla_b_lr"][l]
        cst[:, l, C_GLA_GN] = np.tile(inp["gla_g_norm"][l], 2)
        gc = inp["gdn_conv"][l]
        for which in range(3):
            for pr in range(2):
                for tap in range(4):
                    cst[:, l, C_GDN_CONV + (which * 2 + pr) * 4 + tap] = gc[tap, which * 256 + pr * 128: which * 256 + pr * 128 + 128]
        for r in (0, 32, 64):
            cst[r:r + 4, l, C_GDN_ALOG] = inp["gdn_a_log"][l]
            cst[r:r + 4, l, C_GDN_DTB] = inp["gdn_dt_bias"][l]
        cst[:, l, C_GDN_GN] = np.tile(inp["gdn_g_norm"][l], 2)
        fcv = inp["ffn_conv"][l]
        for tap in range(3):
            cst[:, l, C_FFN_CONV + tap:C_FFN_CONV + 66:3] = _col(fcv[tap])
        cst[:, l, C_FFN_B:C_FFN_B + 22] = _col(inp["ffn_conv_b"][l])
        for c in range(2):
            for gg in range(2):
                poolbd[l, c, 64 * gg:64 * gg + 64, 64 * gg:64 * gg + 64] = inp["pool_w"][l, 2 * c + gg]
    for c in range(2):
        for gg in range(2):
            cst[64 * gg:64 * gg + 64, :, C_POOLINVW + c] = 1.0 / POOL_WINDOWS[2 * c + gg]
    w["w_in_fm"] = fm
    wg = np.zeros((L, 4, 8, 128, KC, 128), f32); wbr = np.zeros((L, 4, 8, 128, 2, 128), f32)
    wo = np.zeros((L, 8, 128, KC, 128), f32); wxq = np.zeros((L, 4, 128, KC, 128), f32)
    wkk = np.zeros((L, 4, 128, KC, 128), f32); wvt = np.zeros((L, 128, KC, 512), f32)
    wxo = np.zeros((L, 8, 128, 4, 128), f32); wup = np.zeros((L, NF, 128, KC, 256), f32)
    for l in range(L):
        for i in range(4):
            wg[l, i] = _fm(inp["w_gate"][l, i])
            wbr[l, i] = _fm(inp["w_branch"][l, i])
        wo[l] = _fm(inp["w_out"][l])
        wxq[l] = _fm(inp["w_xq"][l])
        wkk[l] = _fm(inp["w_mem_kv"][l][:, 0:512])
        wvt[l] = inp["w_mem_kv"][l][:, 512:1024].reshape(KC, 128, 512).transpose(1, 0, 2)
        wxo[l] = _fm(inp["w_xo"][l])
        wu = _fm(inp["w_up"][l][:, 0:DFF]); wv_ = _fm(inp["w_up"][l][:, DFF:2 * DFF])
        wup[l, :, :, :, 0:128] = wu
        wup[l, :, :, :, 128:256] = wv_
    w["w_gate_fm"] = wg; w["w_branch_fm"] = wbr; w["w_out_fm"] = wo; w["w_xq_fm"] = wxq; w["w_kk_fm"] = wkk
    w["w_v_tm"] = wvt; w["w_xo_fm"] = wxo; w["w_up_fm"] = wup
    w["w_down_r"] = np.ascontiguousarray(inp["w_down"].reshape(L, NF, 128, D))
    w["w_in_tm"] = tm
    w["cst"] = cst
    w["pool_bd"] = poolbd
    pool16 = np.zeros((128, 2, 16), f32)
    for c in range(2):
        for gg in range(2):
            wdw = POOL_WINDOWS[2 * c + gg]
            pool16[64 * gg:64 * gg + 64, c, :] = 1.0 / np.minimum(np.arange(16) + 1, wdw)
    w["pool16"] = pool16
    w["ident"] = np.eye(128, dtype=f32)
    w["ones"] = np.ones((128, 128), f32)
    bd64 = np.zeros((128, 128), f32); bd64[:64, :64] = 1; bd64[64:, 64:] = 1
    w["bd64"] = bd64
    w["gfin"] = _col(inp["g_final"])
    wlr = np.zeros((L, 128, 128), f32)
    for l in range(L):
        wlr[l, 32:48, :] = inp["gla_w_lr"][l]
    w["wlr"] = wlr
    rmask = np.ones((128, T), f32); rmask[:, ::128] = 0.0
    w["rmask"] = rmask
    pp = np.arange(128)[:, None]; ff = np.arange(128)[None, :]
    w["tri_ui"] = (pp <= ff).astype(f32)
    w["tri_sl"] = (pp > ff).astype(f32)
    w["bdmask"] = ((np.arange(128)[:, None] // 32) == (np.arange(256)[None, :] // 64)).astype(f32)
    w1bd = np.zeros((L, 2, 128, 32, 128), f32)
    peT = np.zeros((L, 2, 128, 32, 8), f32)
    w2dup = np.zeros((L, 2, 128, 128), f32)
    w2vbd = np.zeros((L, 128, 128), f32)
    for l in range(L):
        for kv in range(2):
            w1 = inp["nsa_cmp_w1"][l, kv].reshape(32, 64, 64)
            for g in range(2):
                w1bd[l, kv, 64 * g:64 * g + 64, :, 64 * g:64 * g + 64] = w1.transpose(1, 0, 2)
                peT[l, kv, 64 * g:64 * g + 64, :, :] = inp["nsa_pe"][l, kv].T[:, :, None]
        w2k = inp["nsa_cmp_w2"][l, 0]
        w2v = inp["nsa_cmp_w2"][l, 1]
        for g in range(2):
            w2dup[l, g, 64 * g:64 * g + 64, 0:64] = w2k
            w2dup[l, g, 64 * g:64 * g + 64, 64:128] = w2k
            w2vbd[l, 64 * g:64 * g + 64, 64 * g:64 * g + 64] = w2v
    w["w1bd"] = w1bd; w["peT"] = peT; w["w2dup"] = w2dup; w["w2vbd"] = w2vbd
    nsadup = np.zeros((L, 4, 128, KC, 128), f32)
    for l in range(L):
        for i, o in enumerate((O_NKS, O_NKS + 64, O_NKW, O_NKW + 64)):
            Wc = np.concatenate([inp["w_in"][l][:, o:o + 64]] * 2, axis=1)
            nsadup[l, i] = _fm(Wc)[0]
    w["nsadup"] = nsadup
    n_i = np.arange(128)[:, None]; q_i = np.arange(T)[None, :]
    w["cmask"] = ((16 * n_i + 31 <= q_i) & (n_i < 127)).astype(f32)
    cover1 = np.zeros((128, 33), f32)
    for n in range(127):
        for m_ in range(32):
            if 16 * n <= 64 * m_ + 63 and 16 * n + 31 >= 64 * m_:
                cover1[n, m_] = 1.0
        cover1[n, 32] = 1.0
    w["cover1"] = cover1
    keepT = np.zeros((128, NT, 32), f32); addT = np.zeros((128, NT, 32), f32); validT = np.zeros((128, NT, 32), f32)
    for qt in range(NT):
        for p in range(128):
            cur = (128 * qt + p) // 64
            for m_ in range(32):
                if m_ > cur:
                    addT[p, qt, m_] = -1.0
                elif m_ == cur:
                    addT[p, qt, m_] = 1e4; validT[p, qt, m_] = 1
                elif m_ == cur - 1:
                    addT[p, qt, m_] = 2e4; validT[p, qt, m_] = 1
                elif m_ == 0:
                    addT[p, qt, m_] = 3e4; validT[p, qt, m_] = 1
                else:
                    keepT[p, qt, m_] = 1; validT[p, qt, m_] = 1
    w["keepT"] = keepT; w["addT"] = addT; w["validT"] = validT
    w["Esel"] = (np.arange(128)[:, None] == (np.arange(T)[None, :] // 64)).astype(f32)
    cc = np.arange(1408)[None, :]; pq = np.arange(128)[:, None]
    w["Wlong"] = ((pq <= cc - 384) & (pq > cc - 896)).astype(f32)
    selg = np.zeros((128, 2, 3, 128), f32)
    for g in range(2):
        for b in range(3):
            for m_ in range(128):
                selg[(g * 2 + m_ // 64) * 3 + b, g, b, m_] = 1.0
    w["selg"] = selg
    selA = np.zeros((128, 4, 128), f32)
    for h in range(4):
        selA[h, h, :] = 1.0
    w["selA"] = selA
    return w


class Ctx:
    pass


def build(nseq=4, nlayers=L, stages=("A", "pool", "gla", "gdn", "nsa", "G", "X", "F"), dbg=(), nsa_level=9):
    nc = bass.Bass("TRN2", target_bir_lowering=False)
    P = Prog(nc)
    c = Ctx()
    c.nsa_level = nsa_level
    c.nc, c.P, c.stages, c.dbg = nc, P, stages, dbg

    def din(name, shape):
        return V(nc.dram_tensor(name, list(shape), F32, kind="ExternalInput").ap(), "dram_" + name)

    c.xT_d = din("xT", [nseq, 128, KC, T])
    c.memT_d = din("memT", [nseq, 128, KC, 256])
    c.w_in_fm = din("w_in_fm", [L, NFM, 128, KC, 128])
    c.w_in_tm = din("w_in_tm", [L, 128, KC, 512])
    c.cst_d = din("cst", [128, L, NCST])
    c.pool_bd_d = din("pool_bd", [L, 2, 128, 128])
    c.pool16_d = din("pool16", [128, 2, 16])
    c.ident_d = din("ident", [128, 128])
    c.ones_d = din("ones", [128, 128])
    c.bd64_d = din("bd64", [128, 128])
    c.gfin_d = din("gfin", [128, KC])
    c.wlr_d = din("wlr", [L, 128, 128])
    c.rmask_d = din("rmask", [128, T])
    c.tri_ui_d = din("tri_ui", [128, 128])
    c.tri_sl_d = din("tri_sl", [128, 128])
    c.bdmask_d = din("bdmask", [128, 256])
    c.selA_d = din("selA", [128, 4, 128])
    c.w_gate_d = din("w_gate_fm", [L, 4, 8, 128, KC, 128])
    c.w_branch_d = din("w_branch_fm", [L, 4, 8, 128, 2, 128])
    c.w_out_d = din("w_out_fm", [L, 8, 128, KC, 128])
    c.w_xq_d = din("w_xq_fm", [L, 4, 128, KC, 128])
    c.w_kk_d = din("w_kk_fm", [L, 4, 128, KC, 128])
    c.w_v_d = din("w_v_tm", [L, 128, KC, 512])
    c.w_xo_d = din("w_xo_fm", [L, 8, 128, 4, 128])
    c.w_up_d = din("w_up_fm", [L, NF, 128, KC, 256])
    c.w_down_d = din("w_down_r", [L, NF, 128, D])
    c.w1bd_d = din("w1bd", [L, 2, 128, 32, 128])
    c.peT_d = din("peT", [L, 2, 128, 32, 8])
    c.w2dup_d = din("w2dup", [L, 2, 128, 128])
    c.w2vbd_d = din("w2vbd", [L, 128, 128])
    c.nsadup_d = din("nsadup", [L, 4, 128, KC, 128])
    c.cmask_d = din("cmask", [128, T])
    c.cover1_d = din("cover1", [128, 33])
    c.keepT_d = din("keepT", [128, NT, 32])
    c.addT_d = din("addT", [128, NT, 32])
    c.validT_d = din("validT", [128, NT, 32])
    c.Esel_d = din("Esel", [128, T])
    c.Wlong_d = din("Wlong", [128, 1408])
    c.selg_d = din("selg", [128, 2, 3, 128])
    c.out_d = V(nc.dram_tensor("outT", [nseq, 128, KC, T], F32, kind="ExternalOutput").ap(), "dram_out")
    c.xs_d = V(nc.dram_tensor("xs", [128, KC, T], F32, kind="Internal").ap(), "dram_xs")
    c.dbg_d = {}
    for name, shape in dbg:
        c.dbg_d[name] = V(nc.dram_tensor("dbg_" + name, list(shape), F32, kind="ExternalOutput").ap(), "dram_dbg_" + name)

    c.cst = V(P.sb("cst", [128, L, NCST])[:], "cst")
    c.ident = V(P.sb("ident", [128, 128])[:], "ident")
    c.ones = V(P.sb("ones", [128, 128])[:], "ones")
    c.bd64 = V(P.sb("bd64", [128, 128])[:], "bd64")
    c.gfin = V(P.sb("gfin", [128, KC])[:], "gfin")
    c.pool16 = V(P.sb("pool16", [128, 2, 16])[:], "pool16")
    c.hT = V(P.sb("hT", [128, KC, T], BF16)[:], "hT")
    c.tri_ui = V(P.sb("tri_ui", [128, 128])[:], "tri_ui")
    c.tri_sl = V(P.sb("tri_sl", [128, 128])[:], "tri_sl")
    c.bdmask = V(P.sb("bdmask", [128, 256])[:], "bdmask")
    c.onec = V(P.sb("onec", [128, 1])[:], "onec")
    c.onesbf = V(P.sb("onesbf", [128, 128], BF16)[:], "onesbf")
    c.bd64bf = V(P.sb("bd64bf", [128, 128], BF16)[:], "bd64bf")
    P.dma("pool", c.bd64bf, c.bd64_d)
    P.memset("pool", c.onesbf, 1.0)
    c.selA = V(P.sb("selA", [128, 4, 128])[:], "selA")
    P.dma("sp", c.selA, c.selA_d)
    P.memset("pool", c.onec, 1.0)
    for dst, src in ((c.tri_ui, c.tri_ui_d), (c.tri_sl, c.tri_sl_d), (c.bdmask, c.bdmask_d)):
        P.dma("sp", dst, src)
    c.epsc = V(P.sb("epsc", [128, 1])[:], "epsc")
    P.memset("pool", c.epsc, EPS)
    c.ar = Arena(P, 156 * 1024)
    c.psb = [V(P.ps("ps%d" % i, [128, 512])[:], "ps%d" % i) for i in range(8)]
    c.ar.reserved_top = KC * T * 4
    c.xT = c.ar.top_view("xT", [KC, T], KC * T * 4)
    P.excl = set("ps%d" % i for i in range(8))
    for dst, src in ((c.cst, c.cst_d), (c.ident, c.ident_d), (c.ones, c.ones_d), (c.bd64, c.bd64_d), (c.gfin, c.gfin_d),
                     (c.pool16, c.pool16_d)):
        P.dma("sp", dst, src)
    c.wq = 0

    for s in range(nseq):
        for l in range(nlayers):
            seq_layer(c, s, l, first=(l == 0), last=(l == nlayers - 1))
    P.barrier()
    P.op("sp", lambda e: None)
    P.finish()
    c.stats = P.stats
    return nc, c


def wdma(c, dst, src):
    c.P.dma("pool", dst, src)


def blk(tb):
    return slice(tb * 512, (tb + 1) * 512)


class Slabs:
    def __init__(self, c, bufs, srcs):
        self.c, self.bufs, self.srcs = c, bufs, srcs
        self.issued = 0
        self.cur = 0

    def _issue(self):
        i = self.issued
        b = self.bufs[i % len(self.bufs)]
        src = self.srcs[i]
        if isinstance(src, (list, tuple)):
            for dst_sel, sv in src:
                wdma(self.c, dst_sel(b), sv)
        else:
            wdma(self.c, b, src)
        self.issued += 1

    def nxt(self):
        i = self.cur
        while self.issued <= min(i + 1, len(self.srcs) - 1):
            self._issue()
        self.cur += 1
        return self.bufs[i % len(self.bufs)]


def proj_fm(c, wsl, rhsT, tb, out_ps, M=128, kc=KC):
    for k in range(kc):
        c.P.mm(out_ps[0:M, :], wsl[:, k, 0:M], rhsT[:, k, blk(tb)], start=(k == 0), stop=(k == kc - 1))


def rsqrt(c, out, in_, scale=1.0, eps=EPS):
    c.P.act(out, in_, AF.Sqrt, bias=c.epsc[0:out.ap.shape[0], :], scale=scale)
    c.P.op("dve", (lambda oa: lambda e: e.reciprocal(oa, oa))(out.ap), _k(out), _k(out))


def phase_norm(c, xT, gcol, out):
    P, ar = c.P, c.ar
    m = ar.mark()
    sq = ar.alloc("sq", [4, 512], BF16)
    rstd = ar.alloc("rstd", [2, 512])
    for tb in range(NB):
        ps = c.psb[tb % 2]
        for k in range(KC):
            s = sq[:, k % 4, :].sub(k % 4)
            P.act(s, xT[:, k, blk(tb)].sub(k), AF.Square)
            P.mm(ps, c.onesbf, s, start=(k == 0), stop=(k == KC - 1))
        r = rstd[:, tb % 2, :].sub(tb % 2)
        rsqrt(c, r, ps, 1.0 / D)
        for k in range(KC):
            eng = "dve" if k % 2 == 0 else "pool"
            P.stt(eng, out[:, k, blk(tb)], xT[:, k, blk(tb)].sub(k), gcol[:, k:k + 1], r, ALU.mult, ALU.mult)
    ar.release(m)


def seq_layer(c, s, l, first, last):
    P, ar = c.P, c.ar
    ar.reset()
    xT = c.xT
    ar.top_live = True
    if first:
        for k in range(KC):
            P.dma("sp", xT[:, k, :].sub(k), c.xT_d[s, :, k, :])
    phase_norm(c, xT, c.cst[:, l, C_GMIX:C_GMIX + 8], c.hT)
    if not first:
        for k in range(KC):
            P.dma("sp", c.xs_d[:, k, :].sub(k), xT[:, k, :].sub(k))
    if "hT" in c.dbg_d:
        P.dma("pool", c.dbg_d["hT"], c.hT)
    ar.reset()
    ar.top_live = False
    obr = ar.alloc("obr", [4, 2, T], BF16)
    if "pool" in c.stages:
        mixer_pool(c, l, obr)
    if "gla" in c.stages:
        mixer_gla(c, l, obr)
    if "gdn" in c.stages:
        mixer_gdn(c, l, obr)
    if "nsa" in c.stages:
        mixer_nsa(c, l, obr)
    if "obr" in c.dbg_d:
        P.dma("pool", c.dbg_d["obr"], obr)
    if "G" not in c.stages:
        return
    ar.top_live = True
    xsrc = c.xT_d[s] if first else c.xs_d
    for k in range(KC):
        P.dma("sp", xT[:, k, :].sub(k), xsrc[:, k, :].sub(k) if not first else xsrc[:, k, :])
    phase_gate(c, l, obr, xT)
    dump_x(c, "x1", xT)
    ar.reset()
    if "X" in c.stages:
        phase_cross(c, s, l, xT)
        dump_x(c, "x2", xT)
        ar.reset()
    if "F" in c.stages:
        phase_ffn(c, l, xT)
        dump_x(c, "x3", xT)
        ar.reset()
    if last:
        phase_final(c, s, xT)


def dump_x(c, name, xT):
    if name in c.dbg_d:
        for k in range(KC):
            c.P.dma("sp", c.dbg_d[name][:, k, :], xT[:, k, :].sub(k))


def add_resid(c, xT, m, tb, ps, i):
    xv = xT[:, m, blk(tb)].sub(m)
    c.P.tt("dve", xv, xv, ps, ALU.add)


def phase_gate(c, l, obr, xT):
    P, ar = c.P, c.ar
    yT = ar.alloc("yT", [KC, T], BF16)
    yacc = ar.alloc("yacc", [T])
    sg = ar.alloc("sg", [2, 512])
    tm = ar.alloc("tm", [2, 512])
    wsl = ar.alloc("wsl", [3, KC, 128], BF16)
    wbl = ar.alloc("wbl", [3, 2, 128], BF16)
    srcs = []
    for m in range(8):
        for i in range(4):
            srcs.append(((lambda b: b[0]), c.w_gate_d[l, i, m]))
    bufs = [(wsl[:, i].sub(i), wbl[:, i].sub(i)) for i in range(3)]
    SLg = Slabs(c, bufs, [[((lambda b: b[0]), c.w_gate_d[l, i, m]), ((lambda b: b[1]), c.w_branch_d[l, i, m])]
                          for m in range(8) for i in range(4)])
    for m in range(8):
        for i in range(4):
            wg, wb = SLg.nxt()
            for tb in range(NB):
                psg = c.psb[(2 * tb) % 4]
                psr = c.psb[(2 * tb) % 4 + 1]
                proj_fm(c, wg, c.hT, tb, psg)
                for k2 in range(2):
                    P.mm(psr, wb[:, k2, :], obr[:, i, k2, blk(tb)], start=(k2 == 0), stop=(k2 == 1))
                sgv = sg[:, tb % 2, :].sub(tb % 2)
                P.act(sgv, psg, AF.Sigmoid, bias=c.cst[:, l, C_BGATE + 8 * i + m:C_BGATE + 8 * i + m + 1], scale=1.0)
                if i == 0:
                    P.tt("dve", yacc[:, blk(tb)].sub(tb), sgv, psr, ALU.mult)
                else:
                    tv = tm[:, tb % 2, :].sub(tb % 2)
                    P.tt("dve", tv, sgv, psr, ALU.mult)
                    P.tt("dve", yacc[:, blk(tb)].sub(tb), yacc[:, blk(tb)].sub(tb), tv, ALU.add)
        for tb in range(NB):
            P.cp("act", yT[:, m, blk(tb)].sub(m), yacc[:, blk(tb)].sub(tb))
    SLo = Slabs(c, [wsl[:, i].sub(i) for i in range(3)], [c.w_out_d[l, m] for m in range(8)])
    for m in range(8):
        wo = SLo.nxt()
        for tb in range(NB):
            ps = c.psb[4 + tb % 4]
            for k in range(KC):
                P.mm(ps, wo[:, k, :], yT[:, k, blk(tb)].sub(k), start=(k == 0), stop=(k == KC - 1))
            add_resid(c, xT, m, tb, ps, tb)


def phase_cross(c, s, l, xT):
    P, ar = c.P, c.ar
    phase_norm(c, xT, c.cst[:, l, C_GCROSS:C_GCROSS + 8], c.hT)
    memT = ar.alloc("memT", [KC, 256])
    memn = ar.alloc("memn", [KC, 256], BF16)
    sq = ar.alloc("msq", [2, 256], BF16)
    rstd = ar.alloc("mrstd", [256])
    KT = ar.alloc("KT", [4, 256], BF16)
    Vtok = ar.alloc("Vtok", [2, 512], BF16)
    wv = ar.alloc("wv", [KC, 512], BF16)
    qh = ar.alloc("qh", [2, 512], BF16)
    ee = ar.alloc("xe", [2, 2, 512], BF16)
    oX = ar.alloc("oX", [4, T], BF16)
    rr = ar.alloc("xr", [2, 512])
    wsl = ar.alloc("wsl", [3, KC, 128], BF16)
    wxo = ar.alloc("wxo", [3, 4, 128], BF16)
    SL = Slabs(c, [wsl[:, i].sub(i) for i in range(3)], [c.w_kk_d[l, hh] for hh in range(4)] + [c.w_xq_d[l, hh] for hh in range(4)])
    SLo = Slabs(c, [wxo[:, i].sub(i) for i in range(3)], [c.w_xo_d[l, m] for m in range(8)])
    wdma(c, wv, c.w_v_d[l])
    P.dma("sp", memT, c.memT_d[s])
    ps = c.psb[0]
    for k in range(KC):
        sv = sq[:, k % 2, :].sub(k % 2)
        P.act(sv, memT[:, k, :], AF.Square)
        P.mm(ps[:, 0:256], c.onesbf, sv, start=(k == 0), stop=(k == KC - 1))
    rsqrt(c, rstd, ps[:, 0:256], 1.0 / D)
    for k in range(KC):
        P.stt("dve", memn[:, k, :], memT[:, k, :], c.cst[:, l, C_GMEM + k:C_GMEM + k + 1], rstd, ALU.mult, ALU.mult)
    n = 0
    for hh in range(4):
        w = SL.nxt()
        ps = c.psb[1 + hh % 2]
        for k in range(KC):
            P.mm(ps[:, 0:256], w[:, k, :], memn[:, k, :], start=(k == 0), stop=(k == KC - 1))
        P.cp("act", KT[:, hh, :], ps[:, 0:256])
    for mt in range(2):
        ps = c.psb[3 + mt]
        for k in range(KC):
            P.mm(ps, memn[:, k, mt * 128:(mt + 1) * 128], wv[:, k, :], start=(k == 0), stop=(k == KC - 1))
        P.cp("act", Vtok[:, mt, :], ps)
    it = 0
    for hh in range(4):
        w = SL.nxt()
        for tb in range(NB):
            r2 = it % 2
            it += 1
            psq = c.psb[r2]
            proj_fm(c, w, c.hT, tb, psq)
            qv = qh[:, r2, :].sub(r2)
            P.ts("dve", qv, psq, 128.0 ** -0.5, ALU.mult)
            psO, psD = c.psb[4 + r2], c.psb[6 + r2]
            for mt in range(2):
                psS = c.psb[2 + mt]
                P.mm(psS, KT[:, hh, mt * 128:(mt + 1) * 128], qv)
                ev = ee[:, r2, mt, :].sub((r2, mt))
                P.act(ev, psS, AF.Exp)
                P.mm(psO, Vtok[:, mt, hh * 128:(hh + 1) * 128], ev, start=(mt == 0), stop=(mt == 1))
                P.mm(psD, c.onesbf, ev, start=(mt == 0), stop=(mt == 1))
            rv = rr[:, r2, :].sub(r2)
            P.op("dve", (lambda oa, ia: lambda e: e.reciprocal(oa, ia))(rv.ap, psD.ap), _k(psD), _k(rv))
            P.tt("dve", oX[:, hh, blk(tb)].sub(hh), psO, rv, ALU.mult)
    for m in range(8):
        w = SLo.nxt()
        for tb in range(NB):
            ps = c.psb[tb % 4]
            for k in range(4):
                P.mm(ps, w[:, k, :], oX[:, k, blk(tb)].sub(k), start=(k == 0), stop=(k == 3))
            add_resid(c, xT, m, tb, ps, tb)


FGROUPS = ((0, 6), (6, 12), (12, 17), (17, 22))


def phase_ffn(c, l, xT):
    P, ar = c.P, c.ar
    phase_norm(c, xT, c.cst[:, l, C_GFFN:C_GFFN + 8], c.hT)
    aT = ar.alloc("aT", [6, T], BF16)
    upad = ar.alloc("upad", [2, 2 + T])
    c1 = ar.alloc("c1", [2, 512])
    gb = ar.alloc("gb", [2, 512], BF16)
    wu = ar.alloc("wu", [3, KC, 256], BF16)
    wd = ar.alloc("wd", [2, 6, D], BF16)
    SLu = Slabs(c, [wu[:, i].sub(i) for i in range(3)], [c.w_up_d[l, f] for f in range(NF)])
    P.memset("pool", upad[:, 0, 0:2].sub(0), 0.0)
    P.memset("pool", upad[:, 1, 0:2].sub(1), 0.0)
    cs = c.cst
    n = 0
    for gi, (f0, f1) in enumerate(FGROUPS):
        wdg = wd[:, gi % 2].sub(gi % 2)
        for fi, f in enumerate(range(f0, f1)):
            P.dma("pool", wdg[:, fi, :], c.w_down_d[l, f])
        for fi, f in enumerate(range(f0, f1)):
            w = SLu.nxt()
            up = upad[:, n % 2].sub(n % 2)
            n += 1
            cw = C_FFN_CONV + 3 * f
            for tb in range(NB):
                psu, psv = c.psb[(2 * tb) % 4], c.psb[(2 * tb) % 4 + 1]
                for k in range(KC):
                    P.mm(psu, w[:, k, 0:128], c.hT[:, k, blk(tb)], start=(k == 0), stop=(k == KC - 1))
                for k in range(KC):
                    P.mm(psv, w[:, k, 128:256], c.hT[:, k, blk(tb)], start=(k == 0), stop=(k == KC - 1))
                b0 = tb * 512
                P.cp("act", up[:, 2 + b0:2 + b0 + 512], psu)
                cv = c1[:, tb % 2, :].sub(tb % 2)
                P.ts("dve", cv, up[:, b0:b0 + 512], cs[:, l, cw:cw + 1], ALU.mult, cs[:, l, C_FFN_B + f:C_FFN_B + f + 1], ALU.add)
                P.stt("dve", cv, up[:, b0 + 1:b0 + 513], cs[:, l, cw + 1:cw + 2], cv, ALU.mult, ALU.add)
                P.stt("dve", cv, up[:, b0 + 2:b0 + 514], cs[:, l, cw + 2:cw + 3], cv, ALU.mult, ALU.add)
                gv = gb[:, tb % 2, :].sub(tb % 2)
                P.act(gv, cv, AF.Gelu_apprx_tanh)
                P.tt("dve", aT[:, fi, blk(tb)].sub(fi), gv, psv, ALU.mult)
        nf = f1 - f0
        for m in range(8):
            for tb in range(NB):
                ps = c.psb[4 + tb % 4]
                for fi in range(nf):
                    P.mm(ps, wdg[:, fi, m * 128:(m + 1) * 128], aT[:, fi, blk(tb)].sub(fi), start=(fi == 0), stop=(fi == nf - 1))
                add_resid(c, xT, m, tb, ps, tb)


def phase_final(c, s, xT):
    P, ar = c.P, c.ar
    sq = ar.alloc("fsq", [4, 512], BF16)
    rstd = ar.alloc("frstd", [2, 512])
    ob = ar.alloc("fob", [4, 512])
    n = 0
    for tb in range(NB):
        ps = c.psb[tb % 2]
        for k in range(KC):
            sv = sq[:, k % 4, :].sub(k % 4)
            P.act(sv, xT[:, k, blk(tb)].sub(k), AF.Square)
            P.mm(ps, c.onesbf, sv, start=(k == 0), stop=(k == KC - 1))
        r = rstd[:, tb % 2, :].sub(tb % 2)
        rsqrt(c, r, ps, 1.0 / D)
        for k in range(KC):
            o = ob[:, n % 4, :].sub(n % 4)
            n += 1
            P.stt("dve", o, xT[:, k, blk(tb)].sub(k), c.gfin[:, k:k + 1], r, ALU.mult, ALU.mult)
            P.dma("sp", c.out_d[s, :, k, blk(tb)], o)
    ar.reset()


def mixer_pool(c, l, obr):
    P, ar = c.P, c.ar
    m = ar.mark()
    W = 16 + T
    upad = ar.alloc("upad", [W])
    st = [ar.alloc("s%d" % i, [W]) for i in range(4)]
    dif = ar.alloc("dif", [T], BF16)
    d32 = ar.alloc("d32", [16])
    wsl = ar.alloc("wsl", [2, KC, 128], BF16)
    pw = ar.alloc("pw", [2, 128], BF16)
    SL = Slabs(c, [wsl[:, i].sub(i) for i in range(2)], [c.w_in_fm[l, FM_PIN], c.w_in_fm[l, FM_PIN + 1]])
    P.memset("pool", upad[:, 0:16], 0.0)
    for i in range(4):
        P.memset("pool", st[i][:, 0:16], 0.0)
    for ch in range(2):
        w = SL.nxt()
        wdma(c, pw[:, ch, :].sub(ch), c.pool_bd_d[l, ch])
        for tb in range(NB):
            ps = c.psb[tb % 2]
            proj_fm(c, w, c.hT, tb, ps)
            P.cp("act", upad[:, 16 + tb * 512:16 + (tb + 1) * 512], ps)
        src = upad
        nst = 2 if ch == 0 else 4
        for i in range(nst):
            sh = 1 << i
            P.tt("dve" if i % 2 == 0 else "pool", st[i][:, sh:W], src[:, sh:W], src[:, 0:W - sh], ALU.add)
            src = st[i]
        for gg in range(2):
            rows = slice(64 * gg, 64 * gg + 64)
            sw = st[(0 if ch == 0 else 2) + gg]
            P.tt("dve", d32[rows, :], sw[rows, 16:32], c.pool16[rows, ch, :], ALU.mult)
            P.stt("dve", dif[rows, :], sw[rows, 16:W], c.cst[rows, l, C_POOLINVW + ch:C_POOLINVW + ch + 1], upad[rows, 16:W],
                  ALU.mult, ALU.subtract)
            P.tt("dve", dif[rows, 0:16], d32[rows, :], upad[rows, 16:32], ALU.subtract)
        for tb in range(NB):
            ps = c.psb[2 + tb % 2]
            P.mm(ps, pw[:, ch, :].sub(ch), dif[:, blk(tb)])
            P.ts("dve", obr[:, 0, ch, blk(tb)], ps, c.cst[:, l, C_POOLSC + ch:C_POOLSC + ch + 1], ALU.mult)
    ar.release(m)


def tile_(t):
    return slice(t * 128, (t + 1) * 128)


def mixer_gla(c, l, obr):
    P, ar = c.P, c.ar
    m = ar.mark()
    rmask = ar.alloc("rmask", [T]); P.dma("sp", rmask, c.rmask_d)
    wlr = ar.alloc("wlr", [128]); P.dma("sp", wlr, c.wlr_d[l])
    tmp = ar.alloc("tmp", [T])
    tmp2 = ar.alloc("tmp2", [T])
    B = ar.alloc("B", [T])
    qe = ar.alloc("qe", [T], BF16)
    kpad = ar.alloc("kpad", [4, T], BF16)
    kupdT = ar.alloc("kupdT", [T])
    kupdtok = ar.alloc("kupdtok", [NT, 128], BF16)
    vtok = ar.alloc("vtok", [NT, 256], BF16)
    rs = ar.alloc("rs", [2, T], BF16)
    oT = ar.alloc("oT", [2, T])
    dec = ar.alloc("dec", [NT])
    negb = ar.alloc("negb", [1])
    wsl = ar.alloc("wsl", [3, KC, 128], BF16)
    wtm = ar.alloc("wtm", [KC, 256], BF16)
    S = ar.alloc("S", [256]); Sbf = ar.alloc("Sbf", [256], BF16); Stmp = ar.alloc("Stmp", [256])
    attm = ar.alloc("attm", [2, 4, 128], BF16)
    sqb = ar.alloc("sqb", [2, 512], BF16)
    SL = Slabs(c, [wsl[:, i].sub(i) for i in range(3)],
               [c.w_in_fm[l, i] for i in (FM_S3, FM_AQ, FM_AKP, FM_AKP + 1, FM_AKP + 2, FM_AKP + 3, FM_AK, FM_AR, FM_AR + 1)])
    wdma(c, wtm, c.w_in_tm[l, :, :, 0:256])

    def slab(idx):
        return SL.nxt()
    psn = [0]

    def nps():
        p = c.psb[psn[0] % 2]
        psn[0] += 1
        return p
    w = slab(FM_S3)
    for tb in range(NB):
        ps = nps()
        proj_fm(c, w, c.hT, tb, ps)
        P.cp("act", tmp[:, blk(tb)], ps)
    P.ts("pool", negb, c.cst[:, l, C_GLA_B:C_GLA_B + 1], -1.0, ALU.mult)
    for tb in range(NB):
        ps = nps()
        P.mm(ps, wlr[32:48, :], tmp[32:48, blk(tb)])
        P.act(tmp2[:, blk(tb)], ps, AF.Exp, bias=negb, scale=-1.0)
        P.act(tmp2[:, blk(tb)], tmp2[:, blk(tb)], AF.Ln, bias=c.onec, scale=1.0)
    P.ts("pool", tmp2, tmp2, -1.0 / 16.0, ALU.mult)
    P.scan(B, rmask, tmp2, 0.0, ALU.mult, ALU.add)
    P.act(tmp, B, AF.Exp)
    P.act(tmp2, B, AF.Exp, scale=-1.0)
    P.cp("pool", dec, tmp.re("p (a b) -> p a b", b=128)[:, :, 127])
    w = slab(FM_AQ)
    for tb in range(NB):
        ps = nps()
        proj_fm(c, w, c.hT, tb, ps)
        P.stt("dve", qe[:, blk(tb)], ps, 32.0 ** -0.5, tmp[:, blk(tb)], ALU.mult, ALU.mult)
    for h in range(4):
        w = slab(FM_AKP + h)
        for tb in range(NB):
            ps = nps()
            proj_fm(c, w, c.hT, tb, ps)
            P.tt("dve", kpad[:, h, blk(tb)], ps, tmp2[:, blk(tb)], ALU.mult)
    w = slab(FM_AK)
    for tb in range(NB):
        ps = nps()
        proj_fm(c, w, c.hT, tb, ps)
        for t4 in range(4):
            tl = tile_(tb * 4 + t4)
            last = tb * 512 + t4 * 128 + 127
            P.act(tmp[:, tl], B[:, tl], AF.Exp, bias=B[:, last:last + 1], scale=-1.0)
        P.tt("dve", kupdT[:, blk(tb)], ps, tmp[:, blk(tb)], ALU.mult)
    for t in range(NT):
        ps = nps()
        for k in range(KC):
            P.mm(ps[:, 0:256], c.hT[:, k, tile_(t)], wtm[:, k, :], start=(k == 0), stop=(k == KC - 1))
        P.cp("act", vtok[:, t, :], ps[:, 0:256])
    for ch in range(2):
        w = slab(FM_AR + ch)
        for tb in range(NB):
            ps = nps()
            proj_fm(c, w, c.hT, tb, ps)
            P.act(rs[:, ch, blk(tb)], ps, AF.Silu)
    for t in range(NT):
        ps = nps()
        P.tr(ps[:, 0:128], kupdT[:, tile_(t)], c.ident)
        P.cp("act", kupdtok[:, t, :], ps[:, 0:128])
    P.memset("pool", S, 0.0)
    for t in range(NT):
        tl = tile_(t)
        psA = c.psb[4 + t % 2]
        for h in range(4):
            P.mm(psA[:, h * 128:(h + 1) * 128], kpad[:, h, tl], qe[:, tl])
        am = attm[:, t % 2].sub(t % 2)
        P.tt("dve", am, psA.re("p (h f) -> p h f", h=4), c.tri_ui.re("p (o f) -> p o f", o=1).bc([128, 4, 128]), ALU.mult)
        psO = c.psb[6 + t % 2]
        for ch in range(2):
            for j in range(2):
                h = 2 * ch + j
                P.mm(psO[64 * j:64 * j + 64, ch * 128:(ch + 1) * 128], vtok[:, t, 64 * h:64 * h + 64], am[:, h, :],
                     start=True, stop=(t == 0))
            if t > 0:
                P.mm(psO[:, ch * 128:(ch + 1) * 128], Sbf[:, ch * 128:(ch + 1) * 128], qe[:, tl], start=False, stop=True)
        P.cp("act", oT[:, :, tl], psO[:, 0:256].re("p (c f) -> p c f", c=2))
        if t < NT - 1:
            psS = c.psb[2 + t % 2]
            P.mm(psS[:, 0:256], kupdtok[:, t, :], vtok[:, t, :])
            P.tt("dve", Stmp, psS[:, 0:256], c.bdmask, ALU.mult)
            P.stt("dve", S, S, dec[:, t:t + 1], Stmp, ALU.mult, ALU.add)
            P.cp("act", Sbf, S)
    for ch in range(2):
        for tb in range(NB):
            ps = nps()
            sv = sqb[:, tb % 2, :].sub(tb % 2)
            P.act(sv, oT[:, ch, blk(tb)], AF.Square)
            P.mm(ps, c.bd64bf, sv)
            rsqrt(c, tmp[:, blk(tb)], ps, 1.0 / 64.0)
            P.tt("dve", tmp[:, blk(tb)], oT[:, ch, blk(tb)], tmp[:, blk(tb)], ALU.mult)
            P.stt("dve", obr[:, 1, ch, blk(tb)], tmp[:, blk(tb)], c.cst[:, l, C_GLA_GN:C_GLA_GN + 1], rs[:, ch, blk(tb)],
                  ALU.mult, ALU.mult)
    ar.release(m)


def mixer_gdn(c, l, obr):
    P, ar = c.P, c.ar
    m = ar.mark()
    b0 = ar.alloc("b0", [3 + T])
    b1 = ar.alloc("b1", [T])
    b2 = ar.alloc("b2", [T])
    b3 = ar.alloc("b3", [T])
    qnT = ar.alloc("qnT", [T]); knT = ar.alloc("knT", [T])
    ktok = ar.alloc("ktok", [NT, 128]); vtok = ar.alloc("vtok", [NT, 128])
    S1tok = ar.alloc("S1tok", [NT, 96]); S2tok = ar.alloc("S2tok", [NT, 64])
    dgT = ar.alloc("dgT", [T], BF16)
    oT = ar.alloc("oT", [T])
    wsl = ar.alloc("wsl", [3, KC, 128], BF16)
    sqb = ar.alloc("sqb", [2, 512], BF16)
    Acol = ar.alloc("Acol", [1]); negA = ar.alloc("negA", [1])
    Sp = ar.alloc("Spair", [64])
    NCH = 4
    ch_t = []
    for i in range(NCH):
        d = {}
        for nm in ("ea", "eb", "egb", "Xa", "Xta", "Xb", "Xtb", "Tt", "Aqk", "qdec"):
            d[nm] = ar.alloc("%s%d" % (nm, i), [128])
        for nm in ("u", "vb", "kbe", "kdec", "vnew"):
            d[nm] = ar.alloc("%s%d" % (nm, i), [64])
        d["wT"] = ar.alloc("wT%d" % i, [128])
        ch_t.append(d)
    SL = Slabs(c, [wsl[:, i].sub(i) for i in range(3)],
               [c.w_in_fm[l, i] for i in (FM_S1, FM_S2, FM_DQ, FM_DK, FM_DV, FM_DG, FM_DQ + 1, FM_DK + 1, FM_DV + 1, FM_DG + 1)])

    def slab(idx):
        return SL.nxt()
    psn = [0]

    def nps():
        p = c.psb[psn[0] % 2]
        psn[0] += 1
        return p
    cs = c.cst
    P.dma("sp", b1, c.rmask_d)
    w = slab(FM_S1)
    for tb in range(NB):
        ps = nps()
        proj_fm(c, w, c.hT, tb, ps)
        P.cp("act", b0[:, blk(tb)], ps)
    P.act(Acol, cs[:, l, C_GDN_ALOG:C_GDN_ALOG + 1], AF.Exp)
    P.ts("pool", negA, Acol, -1.0, ALU.mult)
    z1 = b0[:, 0:T]
    P.act(z1, z1, AF.Exp, bias=cs[:, l, C_GDN_DTB:C_GDN_DTB + 1], scale=1.0)
    P.act(z1, z1, AF.Ln, bias=c.onec, scale=1.0)
    P.ts("dve", z1, z1, negA, ALU.mult)
    P.scan(b2, b1, z1, 0.0, ALU.mult, ALU.add)
    w = slab(FM_S2)
    for tb in range(NB):
        ps = nps()
        proj_fm(c, w, c.hT, tb, ps)
        P.act(b3[:, blk(tb)], ps, AF.Sigmoid)
    g3 = b2[32:64, :].re("p (a b) -> p a b", b=128)
    P.tt("dve", b0[32:64, 0:T].re("p (a b) -> p a b", b=128), g3[:, :, 127:128].bc([32, NT, 128]), g3, ALU.subtract)
    P.act(b2[32:64, :], b0[32:64, 0:T], AF.Exp)
    P.act(b2[64:96, :], b2[64:96, :], AF.Exp)
    P.tt("dve", b2[64:96, :], b2[64:96, :], b3[64:96, :], ALU.mult)
    P.ts("pool", b3[32:64, :], b3[32:64, :], -1.0, ALU.mult)
    for t in range(NT):
        ps = nps()
        P.tr(ps[:, 0:128], b2[:, tile_(t)], c.ident)
        P.cp("act", S1tok[:, t, :], ps[:, 0:96])
        ps = nps()
        P.tr(ps[:, 0:128], b3[:, tile_(t)], c.ident)
        P.cp("act", S2tok[:, t, :], ps[:, 0:64])
    P.memset("pool", b0[:, 0:3], 0.0)
    for pr in range(2):
        for which, fmi, dst in ((0, FM_DQ, qnT), (1, FM_DK, knT), (2, FM_DV, b3)):
            w = slab(fmi + pr)
            for tb in range(NB):
                ps = nps()
                proj_fm(c, w, c.hT, tb, ps)
                P.cp("act", b0[:, 3 + tb * 512:3 + (tb + 1) * 512], ps)
            cc = C_GDN_CONV + (which * 2 + pr) * 4
            P.ts("dve", b1, b0[:, 0:T], cs[:, l, cc:cc + 1], ALU.mult)
            for tap in range(1, 4):
                P.stt("dve", b1, b0[:, tap:tap + T], cs[:, l, cc + tap:cc + tap + 1], b1, ALU.mult, ALU.add)
            P.act(dst, b1, AF.Silu)
            if which < 2:
                for tb in range(NB):
                    ps = nps()
                    sv = sqb[:, tb % 2, :].sub(tb % 2)
                    P.act(sv, dst[:, blk(tb)], AF.Square)
                    P.mm(ps, c.bd64bf, sv)
                    rsqrt(c, b1[:, blk(tb)], ps, 1.0)
                    if which == 0:
                        P.stt("dve", dst[:, blk(tb)], dst[:, blk(tb)], 0.125, b1[:, blk(tb)], ALU.mult, ALU.mult)
                    else:
                        P.tt("dve", dst[:, blk(tb)], dst[:, blk(tb)], b1[:, blk(tb)], ALU.mult)
        w = slab(FM_DG + pr)
        for tb in range(NB):
            ps = nps()
            proj_fm(c, w, c.hT, tb, ps)
            P.act(dgT[:, blk(tb)], ps, AF.Silu)
        for t in range(NT):
            ps = nps()
            P.tr(ps[:, 0:128], knT[:, tile_(t)], c.ident)
            P.cp("act", ktok[:, t, :], ps[:, 0:128])
            ps = nps()
            P.tr(ps[:, 0:128], b3[:, tile_(t)], c.ident)
            P.cp("dve", vtok[:, t, :], ps[:, 0:128])
        P.memset("pool", Sp, 0.0)
        P.barrier()
        for t0 in range(0, NT, 2):
            chains = []
            for dt_ in range(2):
                for j in range(2):
                    ci = dt_ * 2 + j
                    chains.append((ci, t0 + dt_, j))

            def q_(ci, b, qd):
                bank = c.psb[(ci * 2 + b) % 8]
                return bank[:, qd * 128:(qd + 1) * 128].sub(qd)
            for (ci, t, j) in chains:
                h = 2 * pr + j
                P.mm(q_(ci, 0, 0), c.selA[0:4, h, :], b2[0:4, tile_(t)])
            for (ci, t, j) in chains:
                h = 2 * pr + j; d = ch_t[ci]
                gcol = S1tok[:, t, h:h + 1]
                P.ts("dve", d["ea"], q_(ci, 0, 0), gcol, ALU.subtract, 0.0, ALU.max)
                P.ts("dve", d["eb"], q_(ci, 0, 0), gcol, ALU.subtract, 0.0, ALU.min)
                P.act(d["egb"], q_(ci, 0, 0), AF.Exp)
                P.act(d["ea"], d["ea"], AF.Exp, scale=-1.0)
                P.act(d["eb"], d["eb"], AF.Exp)
            for (ci, t, j) in chains:
                hr = slice(64 * j, 64 * j + 64)
                P.mm(q_(ci, 0, 1), knT[hr, tile_(t)], knT[hr, tile_(t)])
                P.mm(q_(ci, 0, 2), knT[hr, tile_(t)], qnT[hr, tile_(t)])
            for (ci, t, j) in chains:
                h = 2 * pr + j; d = ch_t[ci]
                P.tt("dve", d["Xa"], q_(ci, 0, 1), d["ea"], ALU.mult)
                P.stt("dve", d["Xa"], d["Xa"], S2tok[:, t, 32 + h:33 + h], c.tri_sl, ALU.mult, ALU.mult)
                P.tt("dve", d["Aqk"], q_(ci, 0, 2), d["eb"], ALU.mult)
                P.tt("pool", d["Aqk"], d["Aqk"], c.tri_ui, ALU.mult)
            for (ci, t, j) in chains:
                d = ch_t[ci]
                P.tr(q_(ci, 0, 3), d["Xa"], c.ident)
            for (ci, t, j) in chains:
                d = ch_t[ci]
                P.cp("act", d["Xta"], q_(ci, 0, 3))
                P.tt("pool", d["Tt"], d["Xta"], c.ident, ALU.add)
            cur = {ci: ("Xa", "Xta") for ci in range(NCH)}
            for it in range(6):
                for (ci, t, j) in chains:
                    d = ch_t[ci]; xc, xtc = cur[ci]
                    P.mm(q_(ci, 1, 0), d[xtc], d[xc])
                    if it < 5:
                        P.mm(q_(ci, 1, 1), d[xc], d[xtc])
                for (ci, t, j) in chains:
                    d = ch_t[ci]; xc, xtc = cur[ci]
                    nx, nxt = ("Xb", "Xtb") if xc == "Xa" else ("Xa", "Xta")
                    P.cp("act", d[nx], q_(ci, 1, 0))
                    if it < 5:
                        P.cp("dve", d[nxt], q_(ci, 1, 1))
                    cur[ci] = (nx, nxt)
                for (ci, t, j) in chains:
                    d = ch_t[ci]; xc, xtc = cur[ci]
                    P.mm(q_(ci, 1, 2), d[xc], d["Tt"])
                for (ci, t, j) in chains:
                    d = ch_t[ci]
                    P.tt("dve", d["Tt"], d["Tt"], q_(ci, 1, 2), ALU.add)
            for (ci, t, j) in chains:
                h = 2 * pr + j; d = ch_t[ci]
                hc = slice(64 * j, 64 * j + 64)
                P.ts("pool", d["vb"], vtok[:, t, hc], S2tok[:, t, h:h + 1], ALU.mult)
                P.ts("pool", d["kbe"], ktok[:, t, hc], S1tok[:, t, 64 + h:65 + h], ALU.mult)
                P.ts("pool", d["kdec"], ktok[:, t, hc], S1tok[:, t, 32 + h:33 + h], ALU.mult)
                P.tt("pool", d["qdec"][hc, :], qnT[hc, tile_(t)], d["egb"][hc, :], ALU.mult)
            for (ci, t, j) in chains:
                d = ch_t[ci]
                hr = slice(64 * j, 64 * j + 64)
                P.mm(q_(ci, 1, 3)[:, 0:64], d["Tt"], d["vb"])
                P.mm(q_(ci, 0, 0)[hr, :], d["kbe"], d["Tt"])
            for (ci, t, j) in chains:
                d = ch_t[ci]
                hr = slice(64 * j, 64 * j + 64)
                P.cp("act", d["u"], q_(ci, 1, 3)[:, 0:64])
                P.cp("dve", d["wT"][hr, :], q_(ci, 0, 0)[hr, :])
            for (ci, t, j) in chains:
                d = ch_t[ci]
                hr = slice(64 * j, 64 * j + 64)
                P.mm(q_(ci, 0, 1)[:, 0:64], d["wT"][hr, :], Sp[hr, :].sub(j))
                P.tt("dve", d["vnew"], d["u"], q_(ci, 0, 1)[:, 0:64], ALU.subtract)
                P.mm(q_(ci, 0, 2)[hr, :], Sp[hr, :].sub(j), d["qdec"][hr, :], start=True, stop=False)
                P.mm(q_(ci, 0, 2)[hr, :], d["vnew"], d["Aqk"], start=False, stop=True)
                P.cp("act", oT[hr, tile_(t)].sub(j), q_(ci, 0, 2)[hr, :])
                P.mm(q_(ci, 0, 3)[hr, 0:64], d["kdec"], d["vnew"])
                P.stt("dve", Sp[hr, :].sub(j), Sp[hr, :].sub(j), d["egb"][hr, 127:128], q_(ci, 0, 3)[hr, 0:64], ALU.mult, ALU.add)
        P.barrier()
        for tb in range(NB):
            ps = nps()
            sv = sqb[:, tb % 2, :].sub(tb % 2)
            P.act(sv, oT[:, blk(tb)], AF.Square)
            P.mm(ps, c.bd64bf, sv)
            rsqrt(c, b1[:, blk(tb)], ps, 1.0 / 64.0)
            P.tt("dve", b1[:, blk(tb)], oT[:, blk(tb)], b1[:, blk(tb)], ALU.mult)
            P.stt("dve", obr[:, 2, pr, blk(tb)], b1[:, blk(tb)], cs[:, l, C_GDN_GN:C_GDN_GN + 1], dgT[:, blk(tb)],
                  ALU.mult, ALU.mult)
    ar.release(m)


TINY = 1e-30


def mixer_nsa(c, l, obr):
    P, ar = c.P, c.ar
    m = ar.mark()
    gT = ar.alloc("gT", [T])
    kcT = ar.alloc("kcT", [T], BF16); vcT = ar.alloc("vcT", [T], BF16)
    w1 = ar.alloc("w1", [32, 128], BF16)
    peT = ar.alloc("peT", [32, 8], BF16)
    kcR = ar.alloc("kcR", [16, 128], BF16)
    w2d = ar.alloc("w2d", [2, 128], BF16); w2v = ar.alloc("w2v", [128], BF16)
    b1c = ar.alloc("b1c", [1])
    gl = ar.alloc("gl", [128], BF16)
    ckdup = ar.alloc("ckdup", [2, 128], BF16)
    cvtok = ar.alloc("cvtok", [128], BF16)
    qT = ar.alloc("qT", [2, T], BF16)
    ksd = ar.alloc("ksd", [2, T], BF16); kwd = ar.alloc("kwd", [2, T], BF16)
    vstok = ar.alloc("vstok", [NT, 128], BF16); vwtok = ar.alloc("vwtok", [NT, 128], BF16)
    selT = ar.alloc("selT", [T], BF16)
    cmask = ar.alloc("cmask", [T], BF16); P.dma("pool", cmask, c.cmask_d)
    Esel = ar.alloc("Esel", [T], BF16); P.dma("pool", Esel, c.Esel_d)
    Wlong = ar.alloc("Wlong", [1408], BF16); P.dma("pool", Wlong, c.Wlong_d)
    cover1 = ar.alloc("cover1", [33], BF16); P.dma("pool", cover1, c.cover1_d)
    keepT = ar.alloc("keepT", [NT, 32]); P.dma("sp", keepT, c.keepT_d)
    addT = ar.alloc("addT", [NT, 32]); P.dma("sp", addT, c.addT_d)
    validT = ar.alloc("validT", [NT, 32]); P.dma("sp", validT, c.validT_d)
    selg = ar.alloc("selg", [2, 3, 128]); P.dma("sp", selg, c.selg_d)
    wsl = ar.alloc("wsl", [3, KC, 128], BF16)
    wtm = ar.alloc("wtm", [KC, 256], BF16)
    SL = Slabs(c, [wsl[:, i].sub(i) for i in range(3)],
               [c.w_in_fm[l, FM_S3], c.w_in_fm[l, FM_NKC], c.w_in_fm[l, FM_NVC], c.w_in_fm[l, FM_NQ], c.w_in_fm[l, FM_NQ + 1],
                c.nsadup_d[l, 0], c.nsadup_d[l, 1], c.nsadup_d[l, 2], c.nsadup_d[l, 3]])
    wdma(c, wtm, c.w_in_tm[l, :, :, 256:512])
    ee = ar.alloc("ee", [2, 2, 512], BF16)
    em = ar.alloc("em", [2, 2, 512], BF16)
    oacc = ar.alloc("oacc", [T])
    rt = ar.alloc("rt", [2, 512])
    im = {nm: ar.alloc(nm, [32]) for nm in ("imp", "vals", "wa", "wb", "sel")}
    m8 = ar.alloc("m8", [8]); rden = ar.alloc("rden", [2])
    nw = [0]

    def slab(src):
        return SL.nxt()
    psn = [0]

    def nps():
        p = c.psb[psn[0] % 2]
        psn[0] += 1
        return p
    w = slab(c.w_in_fm[l, FM_S3])
    for tb in range(NB):
        ps = nps()
        proj_fm(c, w, c.hT, tb, ps)
        P.act(gT[:, blk(tb)], ps, AF.Sigmoid)
    for fmi, dst in ((FM_NKC, kcT), (FM_NVC, vcT)):
        w = slab(c.w_in_fm[l, fmi])
        for tb in range(NB):
            ps = nps()
            proj_fm(c, w, c.hT, tb, ps)
            P.cp("act", dst[:, blk(tb)], ps)
    P.dma("pool", w2d, c.w2dup_d[l].re("g p m -> p g m"))
    P.dma("pool", w2v, c.w2vbd_d[l])
    P.memset("pool", gl, 0.0)
    P.memset("pool", ckdup, 0.0)
    for kv, src in ((0, kcT), (1, vcT)):
        P.dma("pool", w1, c.w1bd_d[l, kv])
        P.dma("pool", peT, c.peT_d[l, kv])
        P.cp("dve", kcR, src.re("p (c r) -> p r c", r=16))
        psC = nps()
        psB = nps()
        for li in range(32):
            s_, r_ = li // 16, li % 16
            P.mm(psC[:, 0:127], w1[:, li, :], kcR[:, r_, s_:s_ + 127], start=(li == 0), stop=(li == 31))
        for li in range(32):
            P.mm(psB[:, 0:8], w1[:, li, :], peT[:, li, :], start=(li == 0), stop=(li == 31))
        P.cp("dve", b1c, psB[:, 0:1])
        P.act(gl[:, 0:127], psC[:, 0:127], AF.Gelu_apprx_tanh, bias=b1c, scale=1.0)
        if kv == 0:
            for g in range(2):
                ps = nps()
                P.mm(ps[:, 0:128], w2d[:, g, :], gl)
                P.cp("act", ckdup[:, g, :], ps[:, 0:128])
        else:
            ps = nps()
            P.mm(ps[:, 0:128], gl, w2v)
            P.cp("act", cvtok, ps[:, 0:128])
    for g in range(2):
        w = slab(c.w_in_fm[l, FM_NQ + g])
        for tb in range(NB):
            ps = nps()
            proj_fm(c, w, c.hT, tb, ps)
            P.ts("dve", qT[:, g, blk(tb)], ps, 0.125, ALU.mult)
    for i, dst in ((0, ksd), (2, kwd)):
        for g in range(2):
            w = slab(c.nsadup_d[l, i + g])
            for tb in range(NB):
                ps = nps()
                proj_fm(c, w, c.hT, tb, ps)
                P.cp("act", dst[:, g, blk(tb)], ps)
    for t in range(NT):
        ps = nps()
        for k in range(KC):
            P.mm(ps[:, 0:256], c.hT[:, k, tile_(t)], wtm[:, k, :], start=(k == 0), stop=(k == KC - 1))
        P.cp("act", vstok[:, t, :], ps[:, 0:128])
        P.cp("dve", vwtok[:, t, :], ps[:, 128:256])
    P.barrier()
    lvl = getattr(c, "nsa_level", 9)
    if "nsa1" in c.dbg_d:
        P.dma("pool", c.dbg_d["nsa1"][:, 0:256], ckdup.re("p g n -> p (g n)"))
        P.dma("pool", c.dbg_d["nsa1"][:, 256:384], cvtok)
    if lvl < 2:
        ar.release(m)
        return
    psb = c.psb
    nrot = [0]

    def combine(g, tb, br, psO, psD, first):
        psG = psb[4]
        P.mm(psG, selg[0:12, g, br, :], gT[0:12, blk(tb)])
        r = rt[:, 0, :].sub(0)
        r2 = rt[:, 1, :].sub(1)
        P.ts("dve", r, psD, TINY, ALU.max)
        P.op("dve", (lambda oa: lambda e: e.reciprocal(oa, oa))(r.ap), _k(r), _k(r))
        P.tt("dve", r, r, psG, ALU.mult)
        if first:
            P.tt("dve", oacc[:, blk(tb)], psO, r, ALU.mult)
        else:
            P.tt("dve", r2, psO, r, ALU.mult)
            P.tt("pool", oacc[:, blk(tb)], oacc[:, blk(tb)], r2, ALU.add)

    for g in range(2):
        for tb in range(NB):
            psO, psD = psb[6], psb[7]
            rot = nrot[0] % 2
            nrot[0] += 1
            for j in range(2):
                hr = slice(64 * j, 64 * j + 64)
                psS = psb[j]
                P.mm(psS, ckdup[hr, g, :], qT[hr, g, blk(tb)])
                e = ee[:, rot, j, :].sub((rot, j))
                P.act(e, psS, AF.Exp)
                P.tt("pool" if j else "dve", e, e, cmask[:, blk(tb)], ALU.mult)
                P.mm(psO[hr, :], cvtok[:, 64 * g:64 * g + 64], e)
                P.mm(psD[hr, :], c.onesbf[:, 0:64], e)
            for t4 in range(4):
                qt = tb * 4 + t4
                psI = psb[2 + t4 % 2]
                for j in range(2):
                    e = ee[:, rot, j, t4 * 128:(t4 + 1) * 128].sub((rot, j))
                    P.mm(psI[:, j * 64:j * 64 + 33], e, cover1)
                pI = psI[:, 0:128].re("p (j f) -> p j f", j=2)
                P.ts("dve", rden, pI[:, :, 32], TINY, ALU.max)
                P.op("dve", (lambda oa: lambda e_: e_.reciprocal(oa, oa))(rden.ap), _k(rden), _k(rden))
                P.ts("dve", im["imp"], psI[:, 0:32], rden[:, 0:1], ALU.mult)
                P.stt("dve", im["imp"], psI[:, 64:96], rden[:, 1:2], im["imp"], ALU.mult, ALU.add)
                P.tt("dve", im["vals"], im["imp"], keepT[:, qt, :], ALU.mult)
                P.tt("dve", im["vals"], im["vals"], addT[:, qt, :], ALU.add)
                va, wa, wb = im["vals"].ap, im["wa"].ap, im["wb"].ap
                m8a = m8.ap
                P.op("dve", lambda e_: e_.max(out=m8a, in_=va), _k(im["vals"]), _k(m8))
                P.op("dve", lambda e_: e_.match_replace(out=wa, in_to_replace=m8a, in_values=va, imm_value=-2.0),
                     _k(im["vals"], m8), _k(im["wa"]))
                P.op("dve", lambda e_: e_.max(out=m8a, in_=wa), _k(im["wa"]), _k(m8))
                P.op("dve", lambda e_: e_.match_replace(out=wb, in_to_replace=m8a, in_values=wa, imm_value=-2.0),
                     _k(im["wa"], m8), _k(im["wb"]))
                P.tt("dve", im["wb"], im["vals"], im["wb"], ALU.subtract)
                P.stt("dve", im["sel"], im["wb"], 0.0, validT[:, qt, :], ALU.is_gt, ALU.mult)
                psT = psb[4 + t4 % 2]
                P.tr(psT[0:32, 0:128], im["sel"], c.ident)
                P.cp("act", selT[0:32, tile_(qt)], psT[0:32, 0:128])
            combine(g, tb, 0, psO, psD, True)
        P.barrier()
        if "nsa2" in c.dbg_d and g == 0:
            P.dma("pool", c.dbg_d["nsa2"], selT[0:32, :])
        if lvl < 3:
            continue
        for br, kd, vtok_ in ((1, ksd, vstok), (2, kwd, vwtok)):
            for tb in range(NB):
                psO, psD = psb[6], psb[7]
                kts = list(range(0, 4 * tb + 4)) if br == 1 else list(range(max(0, 4 * tb - 4), 4 * tb + 4))
                nk = len(kts)
                rots = []
                for ki in range(nk):
                    rots.append(nrot[0] % 2)
                    nrot[0] += 1

                def stage1(ki):
                    kt = kts[ki]
                    rot = rots[ki]
                    rel = kt - 4 * tb
                    if br == 1:
                        psM = psb[4 + rot]
                        P.mm(psM, Esel[0:32, tile_(kt)], selT[0:32, blk(tb)])
                    for j in range(2):
                        hr = slice(64 * j, 64 * j + 64)
                        psS = psb[2 * rot + j]
                        P.mm(psS, kd[hr, g, tile_(kt)], qT[hr, g, blk(tb)])
                        e = ee[:, rot, j, :].sub((rot, j))
                        P.act(e, psS, AF.Exp)
                        e2 = em[:, rot, j, :].sub((rot, j))
                        if br == 1:
                            P.tt("dve", e2, e, psM, ALU.mult)
                            if rel >= 0:
                                off = 384 - 128 * rel
                                P.tt("pool", e2, e2, Wlong[:, off:off + 512], ALU.mult)
                        else:
                            off = 384 - 128 * rel
                            P.tt("pool" if j else "dve", e2, e, Wlong[:, off:off + 512], ALU.mult)

                def stage2(ki):
                    kt = kts[ki]
                    rot = rots[ki]
                    for j in range(2):
                        hr = slice(64 * j, 64 * j + 64)
                        e2 = em[:, rot, j, :].sub((rot, j))
                        P.mm(psO[hr, :], vtok_[:, kt, 64 * g:64 * g + 64], e2, start=(ki == 0), stop=(ki == nk - 1))
                        P.mm(psD[hr, :], c.onesbf[:, 0:64], e2, start=(ki == 0), stop=(ki == nk - 1))
                for step in range(nk + 1):
                    if step < nk:
                        stage1(step)
                    if step >= 1:
                        stage2(step - 1)
                combine(g, tb, br, psO, psD, False)
        P.barrier()
        for tb in range(NB):
            P.cp("act", obr[:, 3, g, blk(tb)], oacc[:, blk(tb)])
    ar.release(m)


NCORES = 8
SEQ_PER_CORE = 4


def _to_fm(a):
    n, t, d = a.shape
    return np.ascontiguousarray(a.transpose(0, 2, 1).reshape(n, KC, 128, t).transpose(0, 2, 1, 3))


def kernel(**inputs):
    inp = {k: np.asarray(v) for k, v in inputs.items()}
    w = host_prep(inp)
    nc, c = build(nseq=SEQ_PER_CORE, nlayers=L)
    x = inp["x"].astype(np.float32, copy=False)
    mem = inp["mem"].astype(np.float32, copy=False)
    in_maps = []
    for core in range(NCORES):
        sl = slice(core * SEQ_PER_CORE, (core + 1) * SEQ_PER_CORE)
        m = dict(w)
        m["xT"] = _to_fm(x[sl])
        m["memT"] = _to_fm(mem[sl])
        in_maps.append(m)
    res = run_bass_kernel_spmd(nc, in_maps, core_ids=list(range(NCORES)))
    out = np.empty((NCORES * SEQ_PER_CORE, T, D), np.float32)
    for core in range(NCORES):
        oT = res.results[core]["outT"]
        for s in range(SEQ_PER_CORE):
            out[core * SEQ_PER_CORE + s] = oT[s].transpose(2, 1, 0).reshape(T, D)
    return out
```

```python
from contextlib import ExitStack
import numpy as np
import concourse.bass as bass
import concourse.mybir as mybir
from concourse.bass_utils import run_bass_kernel_spmd

F32 = mybir.dt.float32
BF16 = mybir.dt.bfloat16
AF = mybir.ActivationFunctionType
ALU = mybir.AluOpType

ENGS = ("pe", "act", "dve", "pool", "sp")
NDMASEM = 8

T = 2048
D = 1024
KC = 8
NB = 4
NT = 16
L = 2
DFF = 2816
NF = 22
EPS = 1e-6


class Op:
    __slots__ = ("eng", "fn", "reads", "writes", "dma", "deps", "signal", "sigval", "sem", "prewait", "bar")

    def __init__(self, eng, fn, reads, writes, dma):
        self.eng, self.fn, self.reads, self.writes, self.dma = eng, fn, reads, writes, dma
        self.deps = []
        self.signal = False
        self.sigval = None
        self.sem = None
        self.prewait = None
        self.bar = False


class V:
    __slots__ = ("ap", "key")

    def __init__(self, ap, key):
        self.ap, self.key = ap, key

    def __getitem__(self, idx):
        return V(self.ap[idx], self.key)

    def sub(self, k):
        return V(self.ap, (self.key, k))

    def bc(self, shape):
        return V(self.ap.to_broadcast(list(shape)), self.key)

    def re(self, s, **kw):
        return V(self.ap.rearrange(s, **kw), self.key)


def _k(*vs):
    out = []
    for v in vs:
        if isinstance(v, V):
            out.append(v.key)
    return out


def _a(v):
    return v.ap if isinstance(v, V) else v


class Prog:
    def __init__(self, nc):
        self.nc = nc
        self.ops = []
        self.es = ExitStack()
        self.uid = 0
        self.excl = set()

    def sb(self, name, shape, dt=F32):
        t = self.es.enter_context(self.nc.sbuf_tensor("sb_" + name, list(shape), dt))
        return t

    def ps(self, name, shape, dt=F32):
        return self.es.enter_context(self.nc.psum_tensor(name, list(shape), dt))

    def op(self, eng, fn, reads=(), writes=(), dma=False):
        o = Op(eng, fn, tuple(reads), tuple(writes), dma)
        self.ops.append(o)
        return o

    def barrier(self):
        o = Op("sp", None, (), (), False)
        o.bar = True
        self.ops.append(o)

    def dma(self, q, out, in_, **kw):
        oa, ia = _a(out), _a(in_)
        return self.op(q, lambda e: e.dma_start(out=oa, in_=ia, **kw), _k(in_), _k(out), dma=True)

    def mm(self, out, lhsT, rhs, start=True, stop=True):
        oa, la, ra = out.ap, lhsT.ap, rhs.ap
        return self.op("pe", lambda e: e.matmul(oa, lhsT=la, rhs=ra, start=start, stop=stop),
                       _k(lhsT, rhs) + ([] if start else _k(out)), _k(out))

    def tr(self, out, in_, ident):
        oa, ia, da = out.ap, in_.ap, ident.ap
        return self.op("pe", lambda e: e.transpose(oa, ia, da), _k(in_, ident), _k(out))

    def act(self, out, in_, func, bias=0.0, scale=1.0, accum=None):
        oa, ia, ba, sa = out.ap, in_.ap, _a(bias), _a(scale)
        ca = _a(accum) if accum is not None else None

        def fn(e):
            if ca is not None:
                return e.activation(out=oa, in_=ia, func=func, bias=ba, scale=sa, accum_out=ca)
            return e.activation(out=oa, in_=ia, func=func, bias=ba, scale=sa)
        return self.op("act", fn, _k(in_, bias, scale), _k(out) + (_k(accum) if accum is not None else []))

    def tt(self, eng, out, a, b, op):
        oa, aa, ba = out.ap, a.ap, b.ap
        return self.op(eng, lambda e: e.tensor_tensor(out=oa, in0=aa, in1=ba, op=op), _k(a, b), _k(out))

    def ts(self, eng, out, a, s1, op0, s2=None, op1=None):
        oa, aa, s1a, s2a = out.ap, a.ap, _a(s1), _a(s2)

        def fn(e):
            if op1 is None:
                return e.tensor_scalar(out=oa, in0=aa, scalar1=s1a, scalar2=None, op0=op0)
            return e.tensor_scalar(out=oa, in0=aa, scalar1=s1a, scalar2=s2a, op0=op0, op1=op1)
        return self.op(eng, fn, _k(a, s1, s2), _k(out))

    def stt(self, eng, out, a, scalar, b, op0, op1):
        oa, aa, sa, ba = out.ap, a.ap, _a(scalar), b.ap
        eng = "dve"
        return self.op(eng, lambda e: e.scalar_tensor_tensor(out=oa, in0=aa, scalar=sa, in1=ba, op0=op0, op1=op1),
                       _k(a, scalar, b), _k(out))

    def cp(self, eng, out, in_):
        oa, ia = out.ap, in_.ap
        if eng == "act":
            return self.op(eng, lambda e: e.copy(oa, ia), _k(in_), _k(out))
        return self.op(eng, lambda e: e.tensor_copy(oa, ia), _k(in_), _k(out))

    def memset(self, eng, out, val):
        oa = out.ap
        return self.op(eng, lambda e: e.memset(oa, val), (), _k(out))

    def scan(self, out, d0, d1, init, op0, op1):
        oa, a0, a1 = out.ap, d0.ap, d1.ap
        return self.op("dve", lambda e: e.tensor_tensor_scan(out=oa, data0=a0, data1=a1, initial=init, op0=op0, op1=op1),
                       _k(d0, d1), _k(out))

    def finish(self):
        nc = self.nc
        last_w = {}
        readers = {}
        pend = {e: [] for e in ENGS}
        seg = []
        excl = self.excl

        def _bank(k):
            b = k
            while isinstance(b, tuple):
                b = b[0]
            return b if b in excl else None
        for o in self.ops:
            if o.bar:
                lastc = {}
                dm = []
                for p in seg:
                    if p.dma:
                        dm.append(p)
                    else:
                        lastc[p.eng] = p
                newl = [p for p in list(lastc.values()) + dm if p.fn is not None]
                for e in ENGS:
                    pend[e].extend(newl)
                seg = []
                last_w = {}
                readers = {}
                continue
            rr, ww = [], []
            for k in o.reads:
                b = _bank(k)
                if b is None:
                    rr.append(k)
                else:
                    ww.append(b)
            for k in o.writes:
                b = _bank(k)
                ww.append(k if b is None else b)
            o.reads, o.writes = tuple(rr), tuple(dict.fromkeys(ww))
            seg.append(o)
            deps = set()
            for k in o.reads:
                w = last_w.get(k)
                if w is not None:
                    deps.add(w)
            for k in o.writes:
                w = last_w.get(k)
                if w is not None:
                    deps.add(w)
                for r in readers.get(k, ()):
                    deps.add(r)
            deps.discard(o)
            if o.eng == "pe":
                deps = {d for d in deps if not (d.eng == "pe" and not d.dma)}
            if pend[o.eng]:
                deps.update(pend[o.eng])
                pend[o.eng] = []
            deps.discard(o)
            o.deps = list(deps)
            for d in deps:
                d.signal = True
            for k in o.reads:
                readers.setdefault(k, []).append(o)
            for k in o.writes:
                last_w[k] = o
                readers[k] = []
        self.ops = [o for o in self.ops if not o.bar]
        streams = {e: [] for e in ENGS}
        for o in self.ops:
            streams[o.eng].append(o)
        sems = {e: self.es.enter_context(nc.semaphore("s_" + e)) for e in ENGS}
        dsems = {e: [self.es.enter_context(nc.semaphore("d_%s%d" % (e, i))) for i in range(NDMASEM)] for e in ENGS
                 if any(o.dma for o in streams[e])}
        for e in ENGS:
            c = 0
            nd = 0
            for o in streams[e]:
                if o.dma:
                    o.sem = dsems[e][nd % NDMASEM]
                    o.sigval = 16 * (nd // NDMASEM + 1)
                    if nd >= NDMASEM:
                        o.prewait = (o.sem, 16 * (nd // NDMASEM))
                    nd += 1
                    o.signal = True
                elif o.signal:
                    c += 1
                    o.sem = sems[e]
                    o.sigval = c
        self.stats = {e: len(streams[e]) for e in ENGS}
        nwaits = [0]

        def emit_stream(eng, e):
            known = {}
            for o in streams[e]:
                if o.prewait is not None:
                    s, v = o.prewait
                    if known.get(id(s), 0) < v:
                        eng.wait_ge(s, v)
                        known[id(s)] = v
                        nwaits[0] += 1
                need = {}
                for d in o.deps:
                    s = d.sem
                    if s is None:
                        continue
                    if need.get(id(s), (None, 0))[1] < d.sigval:
                        need[id(s)] = (s, d.sigval)
                for sid, (s, v) in need.items():
                    if known.get(sid, 0) < v:
                        eng.wait_ge(s, v)
                        known[sid] = v
                        nwaits[0] += 1
                ins = o.fn(eng)
                if o.signal and ins is not None:
                    ins.then_inc(o.sem, 16 if o.dma else 1)

        block = self.es.enter_context(nc.Block())

        @block.tensor
        def _(eng):
            emit_stream(eng, "pe")

        @block.scalar
        def _(eng):
            emit_stream(eng, "act")

        @block.vector
        def _(eng):
            emit_stream(eng, "dve")

        @block.gpsimd
        def _(eng):
            emit_stream(eng, "pool")

        @block.sync
        def _(eng):
            emit_stream(eng, "sp")

        self.stats["waits"] = nwaits[0]
        self.es.close()


class Arena:
    def __init__(self, P, nbytes):
        self.P = P
        self.nbytes = nbytes
        self.t16 = P.sb("arena", [128, nbytes // 2], BF16)
        self.t32 = self.t16.bitcast(F32)
        self.off = 0
        self.gen = 0
        self.peak = 0
        self.top_live = False
        self.reserved_top = 0

    def reset(self):
        self.P.barrier()
        self.off = 0
        self.gen += 1

    def mark(self):
        return self.off

    def release(self, m):
        self.P.barrier()
        self.off = m
        self.gen += 1

    def top_view(self, name, free_shape, nbytes):
        e0 = (self.nbytes - nbytes) // 4
        n = nbytes // 4
        ap = self.t32[:, e0:e0 + n].rearrange("p (a b) -> p a b", a=free_shape[0])
        return V(ap, name)

    def alloc(self, name, free_shape, dt=F32):
        n = 1
        for s in free_shape:
            n *= s
        esz = 4 if dt == F32 else 2
        nb = (n * esz + 63) // 64 * 64
        lim = self.nbytes - (self.reserved_top if self.top_live else 0)
        assert self.off + nb <= lim, "arena overflow %s: %d + %d > %d" % (name, self.off, nb, lim)
        base = self.t32 if dt == F32 else self.t16
        e0 = self.off // esz
        ap = base[:, e0:e0 + n]
        self.off += nb
        self.peak = max(self.peak, self.off)
        if len(free_shape) == 2:
            ap = ap.rearrange("p (a b) -> p a b", a=free_shape[0])
        elif len(free_shape) == 3:
            ap = ap.rearrange("p (a b c) -> p a b c", a=free_shape[0], b=free_shape[1])
        return V(ap, "%s#%d" % (name, self.gen))


POOL_WINDOWS = (2, 4, 8, 16)
NFM = 27
(FM_PIN, FM_AQ, FM_AKP, FM_AK, FM_AR, FM_S3, FM_S1, FM_S2, FM_DQ, FM_DK, FM_DV, FM_DG, FM_NQ, FM_NKC, FM_NVC, FM_NKS,
 FM_NKW) = (0, 2, 3, 7, 8, 10, 11, 12, 13, 15, 17, 19, 21, 23, 24, 25, 26)
C_GMIX, C_GCROSS, C_GFFN, C_GMEM, C_BGATE, C_POOLSC, C_POOLINVW, C_GLA_B, C_GLA_GN = 0, 8, 16, 24, 32, 64, 66, 68, 69
C_GDN_CONV, C_GDN_ALOG, C_GDN_DTB, C_GDN_GN, C_FFN_CONV, C_FFN_B, NCST = 70, 94, 95, 96, 100, 166, 192
_off = np.cumsum([0, 256, 128, 128, 256, 256, 16, 256, 256, 256, 4, 4, 256, 256, 128, 128, 128, 128, 128, 128, 12])
(O_PIN, O_AQ, O_AK, O_AV, O_AR, O_ALR, O_DQ, O_DK, O_DV, O_DB, O_DA, O_DG, O_NQ, O_NKC, O_NVC, O_NKS, O_NVS, O_NKW,
 O_NVW, O_NG) = [int(v) for v in _off[:-1]]


def _fm(W):
    K, n = W.shape
    return np.ascontiguousarray(W.reshape(K // 128, 128, n // 128, 128).transpose(2, 1, 0, 3))


def _col(v):
    return np.ascontiguousarray(v.reshape(-1, 128).T)


def host_prep(inp):
    f32 = np.float32
    w = {}
    w_in = inp["w_in"]
    fm = np.zeros((L, NFM, 128, KC, 128), f32)
    tm = np.zeros((L, 128, KC, 512), f32)
    cst = np.zeros((128, L, NCST), f32)
    poolbd = np.zeros((L, 2, 128, 128), f32)
    for l in range(L):
        W = w_in[l]

        def cols(o, n):
            return W[:, o:o + n]

        def put(idx, Wc):
            n = Wc.shape[1]
            pad = np.zeros((D, 128), f32)
            pad[:, :n] = Wc
            fm[l, idx] = _fm(pad)[0]
        put(FM_PIN, cols(O_PIN, 128)); put(FM_PIN + 1, cols(O_PIN + 128, 128))
        put(FM_AQ, cols(O_AQ, 128))
        for h in range(4):
            pad = np.zeros((D, 128), f32)
            pad[:, 32 * h:32 * h + 32] = cols(O_AK + 32 * h, 32)
            put(FM_AKP + h, pad)
        put(FM_AK, cols(O_AK, 128))
        put(FM_AR, cols(O_AR, 128)); put(FM_AR + 1, cols(O_AR + 128, 128))
        s3 = np.zeros((D, 128), f32); s3[:, 0:12] = cols(O_NG, 12); s3[:, 32:48] = cols(O_ALR, 16); put(FM_S3, s3)
        s1 = np.zeros((D, 128), f32)
        s2 = np.zeros((D, 128), f32)
        for r in (0, 32, 64):
            s1[:, r:r + 4] = cols(O_DA, 4)
            s2[:, r:r + 4] = cols(O_DB, 4)
        put(FM_S1, s1); put(FM_S2, s2)
        for j in range(2):
            put(FM_DQ + j, cols(O_DQ + 128 * j, 128)); put(FM_DK + j, cols(O_DK + 128 * j, 128))
            put(FM_DV + j, cols(O_DV + 128 * j, 128)); put(FM_DG + j, cols(O_DG + 128 * j, 128))
            put(FM_NQ + j, cols(O_NQ + 128 * j, 128))
        put(FM_NKC, cols(O_NKC, 128)); put(FM_NVC, cols(O_NVC, 128)); put(FM_NKS, cols(O_NKS, 128)); put(FM_NKW, cols(O_NKW, 128))
        tmw = np.concatenate([cols(O_AV, 256), cols(O_NVS, 128), cols(O_NVW, 128)], axis=1)
        tm[l] = tmw.reshape(KC, 128, 512).transpose(1, 0, 2)
        cst[:, l, C_GMIX:C_GMIX + 8] = _col(inp["g_mix"][l])
        cst[:, l, C_GCROSS:C_GCROSS + 8] = _col(inp["g_cross"][l])
        cst[:, l, C_GFFN:C_GFFN + 8] = _col(inp["g_ffn"][l])
        cst[:, l, C_GMEM:C_GMEM + 8] = _col(inp["g_mem"][l])
        for i in range(4):
            cst[:, l, C_BGATE + 8 * i:C_BGATE + 8 * i + 8] = _col(inp["b_gate"][l, i])
        cst[:, l, C_POOLSC:C_POOLSC + 2] = _col(inp["pool_scale"][l])
        cst[:, l, C_GLA_B] = inp["gla_b_lr"][l]
        cst[:, l, C_GLA_GN] = np.tile(inp["gla_g_norm"][l], 2)
        gc = inp["gdn_conv"][l]
        for which in range(3):
            for pr in range(2):
                for tap in range(4):
                    cst[:, l, C_GDN_CONV + (which * 2 + pr) * 4 + tap] = gc[tap, which * 256 + pr * 128: which * 256 + pr * 128 + 128]
        for r in (0, 32, 64):
            cst[r:r + 4, l, C_GDN_ALOG] = inp["gdn_a_log"][l]
            cst[r:r + 4, l, C_GDN_DTB] = inp["gdn_dt_bias"][l]
        cst[:, l, C_GDN_GN] = np.tile(inp["gdn_g_norm"][l], 2)
        fcv = inp["ffn_conv"][l]
        for tap in range(3):
            cst[:, l, C_FFN_CONV + tap:C_FFN_CONV + 66:3] = _col(fcv[tap])
        cst[:, l, C_FFN_B:C_FFN_B + 22] = _col(inp["ffn_conv_b"][l])
        for c in range(2):
            for gg in range(2):
                poolbd[l, c, 64 * gg:64 * gg + 64, 64 * gg:64 * gg + 64] = inp["pool_w"][l, 2 * c + gg]
    for c in range(2):
        for gg in range(2):
            cst[64 * gg:64 * gg + 64, :, C_POOLINVW + c] = 1.0 / POOL_WINDOWS[2 * c + gg]
    w["w_in_fm"] = fm
    wg = np.zeros((L, 4, 8, 128, KC, 128), f32); wbr = np.zeros((L, 4, 8, 128, 2, 128), f32)
    wo = np.zeros((L, 8, 128, KC, 128), f32); wxq = np.zeros((L, 4, 128, KC, 128), f32)
    wkk = np.zeros((L, 4, 128, KC, 128), f32); wvt = np.zeros((L, 128, KC, 512), f32)
    wxo = np.zeros((L, 8, 128, 4, 128), f32); wup = np.zeros((L, NF, 128, KC, 256), f32)
    for l in range(L):
        for i in range(4):
            wg[l, i] = _fm(inp["w_gate"][l, i])
            wbr[l, i] = _fm(inp["w_branch"][l, i])
        wo[l] = _fm(inp["w_out"][l])
        wxq[l] = _fm(inp["w_xq"][l])
        wkk[l] = _fm(inp["w_mem_kv"][l][:, 0:512])
        wvt[l] = inp["w_mem_kv"][l][:, 512:1024].reshape(KC, 128, 512).transpose(1, 0, 2)
        wxo[l] = _fm(inp["w_xo"][l])
        wu = _fm(inp["w_up"][l][:, 0:DFF]); wv_ = _fm(inp["w_up"][l][:, DFF:2 * DFF])
        wup[l, :, :, :, 0:128] = wu
        wup[l, :, :, :, 128:256] = wv_
    w["w_gate_fm"] = wg; w["w_branch_fm"] = wbr; w["w_out_fm"] = wo; w["w_xq_fm"] = wxq; w["w_kk_fm"] = wkk
    w["w_v_tm"] = wvt; w["w_xo_fm"] = wxo; w["w_up_fm"] = wup
    w["w_down_r"] = np.ascontiguousarray(inp["w_down"].reshape(L, NF, 128, D))
    w["w_in_tm"] = tm
    w["cst"] = cst
    w["pool_bd"] = poolbd
    pool16 = np.zeros((128, 2, 16), f32)
    for c in range(2):
        for gg in range(2):
            wdw = POOL_WINDOWS[2 * c + gg]
            pool16[64 * gg:64 * gg + 64, c, :] = 1.0 / np.minimum(np.arange(16) + 1, wdw)
    w["pool16"] = pool16
    w["ident"] = np.eye(128, dtype=f32)
    w["ones"] = np.ones((128, 128), f32)
    bd64 = np.zeros((128, 128), f32); bd64[:64, :64] = 1; bd64[64:, 64:] = 1
    w["bd64"] = bd64
    w["gfin"] = _col(inp["g_final"])
    wlr = np.zeros((L, 128, 128), f32)
    for l in range(L):
        wlr[l, 32:48, :] = inp["gla_w_lr"][l]
    w["wlr"] = wlr
    rmask = np.ones((128, T), f32); rmask[:, ::128] = 0.0
    w["rmask"] = rmask
    pp = np.arange(128)[:, None]; ff = np.arange(128)[None, :]
    w["tri_ui"] = (pp <= ff).astype(f32)
    w["tri_sl"] = (pp > ff).astype(f32)
    w["bdmask"] = ((np.arange(128)[:, None] // 32) == (np.arange(256)[None, :] // 64)).astype(f32)
    w1bd = np.zeros((L, 2, 128, 32, 128), f32)
    peT = np.zeros((L, 2, 128, 32, 8), f32)
    w2dup = np.zeros((L, 2, 128, 128), f32)
    w2vbd = np.zeros((L, 128, 128), f32)
    for l in range(L):
        for kv in range(2):
            w1 = inp["nsa_cmp_w1"][l, kv].reshape(32, 64, 64)
            for g in range(2):
                w1bd[l, kv, 64 * g:64 * g + 64, :, 64 * g:64 * g + 64] = w1.transpose(1, 0, 2)
                peT[l, kv, 64 * g:64 * g + 64, :, :] = inp["nsa_pe"][l, kv].T[:, :, None]
        w2k = inp["nsa_cmp_w2"][l, 0]
        w2v = inp["nsa_cmp_w2"][l, 1]
        for g in range(2):
            w2dup[l, g, 64 * g:64 * g + 64, 0:64] = w2k
            w2dup[l, g, 64 * g:64 * g + 64, 64:128] = w2k
            w2vbd[l, 64 * g:64 * g + 64, 64 * g:64 * g + 64] = w2v
    w["w1bd"] = w1bd; w["peT"] = peT; w["w2dup"] = w2dup; w["w2vbd"] = w2vbd
    nsadup = np.zeros((L, 4, 128, KC, 128), f32)
    for l in range(L):
        for i, o in enumerate((O_NKS, O_NKS + 64, O_NKW, O_NKW + 64)):
            Wc = np.concatenate([inp["w_in"][l][:, o:o + 64]] * 2, axis=1)
            nsadup[l, i] = _fm(Wc)[0]
    w["nsadup"] = nsadup
    n_i = np.arange(128)[:, None]; q_i = np.arange(T)[None, :]
    w["cmask"] = ((16 * n_i + 31 <= q_i) & (n_i < 127)).astype(f32)
    cover1 = np.zeros((128, 33), f32)
    for n in range(127):
        for m_ in range(32):
            if 16 * n <= 64 * m_ + 63 and 16 * n + 31 >= 64 * m_:
                cover1[n, m_] = 1.0
        cover1[n, 32] = 1.0
    w["cover1"] = cover1
    keepT = np.zeros((128, NT, 32), f32); addT = np.zeros((128, NT, 32), f32); validT = np.zeros((128, NT, 32), f32)
    for qt in range(NT):
        for p in range(128):
            cur = (128 * qt + p) // 64
            for m_ in range(32):
                if m_ > cur:
                    addT[p, qt, m_] = -1.0
                elif m_ == cur:
                    addT[p, qt, m_] = 1e4; validT[p, qt, m_] = 1
                elif m_ == cur - 1:
                    addT[p, qt, m_] = 2e4; validT[p, qt, m_] = 1
                elif m_ == 0:
                    addT[p, qt, m_] = 3e4; validT[p, qt, m_] = 1
                else:
                    keepT[p, qt, m_] = 1; validT[p, qt, m_] = 1
    w["keepT"] = keepT; w["addT"] = addT; w["validT"] = validT
    w["Esel"] = (np.arange(128)[:, None] == (np.arange(T)[None, :] // 64)).astype(f32)
    cc = np.arange(1408)[None, :]; pq = np.arange(128)[:, None]
    w["Wlong"] = ((pq <= cc - 384) & (pq > cc - 896)).astype(f32)
    selg = np.zeros((128, 2, 3, 128), f32)
    for g in range(2):
        for b in range(3):
            for m_ in range(128):
                selg[(g * 2 + m_ // 64) * 3 + b, g, b, m_] = 1.0
    w["selg"] = selg
    selA = np.zeros((128, 4, 128), f32)
    for h in range(4):
        selA[h, h, :] = 1.0
    w["selA"] = selA
    return w


class Ctx:
    pass


def build(nseq=4, nlayers=L, stages=("A", "pool", "gla", "gdn", "nsa", "G", "X", "F"), dbg=(), nsa_level=9):
    nc = bass.Bass("TRN2", target_bir_lowering=False)
    P = Prog(nc)
    c = Ctx()
    c.nsa_level = nsa_level
    c.nc, c.P, c.stages, c.dbg = nc, P, stages, dbg

    def din(name, shape):
        return V(nc.dram_tensor(name, list(shape), F32, kind="ExternalInput").ap(), "dram_" + name)

    c.xT_d = din("xT", [nseq, 128, KC, T])
    c.memT_d = din("memT", [nseq, 128, KC, 256])
    c.w_in_fm = din("w_in_fm", [L, NFM, 128, KC, 128])
    c.w_in_tm = din("w_in_tm", [L, 128, KC, 512])
    c.cst_d = din("cst", [128, L, NCST])
    c.pool_bd_d = din("pool_bd", [L, 2, 128, 128])
    c.pool16_d = din("pool16", [128, 2, 16])
    c.ident_d = din("ident", [128, 128])
    c.ones_d = din("ones", [128, 128])
    c.bd64_d = din("bd64", [128, 128])
    c.gfin_d = din("gfin", [128, KC])
    c.wlr_d = din("wlr", [L, 128, 128])
    c.rmask_d = din("rmask", [128, T])
    c.tri_ui_d = din("tri_ui", [128, 128])
    c.tri_sl_d = din("tri_sl", [128, 128])
    c.bdmask_d = din("bdmask", [128, 256])
    c.selA_d = din("selA", [128, 4, 128])
    c.w_gate_d = din("w_gate_fm", [L, 4, 8, 128, KC, 128])
    c.w_branch_d = din("w_branch_fm", [L, 4, 8, 128, 2, 128])
    c.w_out_d = din("w_out_fm", [L, 8, 128, KC, 128])
    c.w_xq_d = din("w_xq_fm", [L, 4, 128, KC, 128])
    c.w_kk_d = din("w_kk_fm", [L, 4, 128, KC, 128])
    c.w_v_d = din("w_v_tm", [L, 128, KC, 512])
    c.w_xo_d = din("w_xo_fm", [L, 8, 128, 4, 128])
    c.w_up_d = din("w_up_fm", [L, NF, 128, KC, 256])
    c.w_down_d = din("w_down_r", [L, NF, 128, D])
    c.w1bd_d = din("w1bd", [L, 2, 128, 32, 128])
    c.peT_d = din("peT", [L, 2, 128, 32, 8])
    c.w2dup_d = din("w2dup", [L, 2, 128, 128])
    c.w2vbd_d = din("w2vbd", [L, 128, 128])
    c.nsadup_d = din("nsadup", [L, 4, 128, KC, 128])
    c.cmask_d = din("cmask", [128, T])
    c.cover1_d = din("cover1", [128, 33])
    c.keepT_d = din("keepT", [128, NT, 32])
    c.addT_d = din("addT", [128, NT, 32])
    c.validT_d = din("validT", [128, NT, 32])
    c.Esel_d = din("Esel", [128, T])
    c.Wlong_d = din("Wlong", [128, 1408])
    c.selg_d = din("selg", [128, 2, 3, 128])
    c.out_d = V(nc.dram_tensor("outT", [nseq, 128, KC, T], F32, kind="ExternalOutput").ap(), "dram_out")
    c.xs_d = V(nc.dram_tensor("xs", [128, KC, T], F32, kind="Internal").ap(), "dram_xs")
    c.dbg_d = {}
    for name, shape in dbg:
        c.dbg_d[name] = V(nc.dram_tensor("dbg_" + name, list(shape), F32, kind="ExternalOutput").ap(), "dram_dbg_" + name)

    c.cst = V(P.sb("cst", [128, L, NCST])[:], "cst")
    c.ident = V(P.sb("ident", [128, 128])[:], "ident")
    c.ones = V(P.sb("ones", [128, 128])[:], "ones")
    c.bd64 = V(P.sb("bd64", [128, 128])[:], "bd64")
    c.gfin = V(P.sb("gfin", [128, KC])[:], "gfin")
    c.pool16 = V(P.sb("pool16", [128, 2, 16])[:], "pool16")
    c.hT = V(P.sb("hT", [128, KC, T], BF16)[:], "hT")
    c.tri_ui = V(P.sb("tri_ui", [128, 128])[:], "tri_ui")
    c.tri_sl = V(P.sb("tri_sl", [128, 128])[:], "tri_sl")
    c.bdmask = V(P.sb("bdmask", [128, 256])[:], "bdmask")
    c.onec = V(P.sb("onec", [128, 1])[:], "onec")
    c.onesbf = V(P.sb("onesbf", [128, 128], BF16)[:], "onesbf")
    c.bd64bf = V(P.sb("bd64bf", [128, 128], BF16)[:], "bd64bf")
    P.dma("pool", c.bd64bf, c.bd64_d)
    P.memset("pool", c.onesbf, 1.0)
    c.selA = V(P.sb("selA", [128, 4, 128])[:], "selA")
    P.dma("sp", c.selA, c.selA_d)
    P.memset("pool", c.onec, 1.0)
    for dst, src in ((c.tri_ui, c.tri_ui_d), (c.tri_sl, c.tri_sl_d), (c.bdmask, c.bdmask_d)):
        P.dma("sp", dst, src)
    c.epsc = V(P.sb("epsc", [128, 1])[:], "epsc")
    P.memset("pool", c.epsc, EPS)
    c.ar = Arena(P, 156 * 1024)
    c.psb = [V(P.ps("ps%d" % i, [128, 512])[:], "ps%d" % i) for i in range(8)]
    c.ar.reserved_top = KC * T * 4
    c.xT = c.ar.top_view("xT", [KC, T], KC * T * 4)
    P.excl = set("ps%d" % i for i in range(8))
    for dst, src in ((c.cst, c.cst_d), (c.ident, c.ident_d), (c.ones, c.ones_d), (c.bd64, c.bd64_d), (c.gfin, c.gfin_d),
                     (c.pool16, c.pool16_d)):
        P.dma("sp", dst, src)
    c.wq = 0

    for s in range(nseq):
        for l in range(nlayers):
            seq_layer(c, s, l, first=(l == 0), last=(l == nlayers - 1))
    P.barrier()
    P.op("sp", lambda e: None)
    P.finish()
    c.stats = P.stats
    return nc, c


def wdma(c, dst, src):
    c.P.dma("pool", dst, src)


def blk(tb):
    return slice(tb * 512, (tb + 1) * 512)


class Slabs:
    def __init__(self, c, bufs, srcs):
        self.c, self.bufs, self.srcs = c, bufs, srcs
        self.issued = 0
        self.cur = 0

    def _issue(self):
        i = self.issued
        b = self.bufs[i % len(self.bufs)]
        src = self.srcs[i]
        if isinstance(src, (list, tuple)):
            for dst_sel, sv in src:
                wdma(self.c, dst_sel(b), sv)
        else:
            wdma(self.c, b, src)
        self.issued += 1

    def nxt(self):
        i = self.cur
        while self.issued <= min(i + 1, len(self.srcs) - 1):
            self._issue()
        self.cur += 1
        return self.bufs[i % len(self.bufs)]


def proj_fm(c, wsl, rhsT, tb, out_ps, M=128, kc=KC):
    for k in range(kc):
        c.P.mm(out_ps[0:M, :], wsl[:, k, 0:M], rhsT[:, k, blk(tb)], start=(k == 0), stop=(k == kc - 1))


def rsqrt(c, out, in_, scale=1.0, eps=EPS):
    c.P.act(out, in_, AF.Sqrt, bias=c.epsc[0:out.ap.shape[0], :], scale=scale)
    c.P.op("dve", (lambda oa: lambda e: e.reciprocal(oa, oa))(out.ap), _k(out), _k(out))


def phase_norm(c, xT, gcol, out):
    P, ar = c.P, c.ar
    sq = ar.alloc("sq", [4, 512], BF16)
    rstd = ar.alloc("rstd", [2, 512])
    for tb in range(NB):
        ps = c.psb[tb % 2]
        for k in range(KC):
            s = sq[:, k % 4, :].sub(k % 4)
            P.act(s, xT[:, k, blk(tb)].sub(k), AF.Square)
            P.mm(ps, c.onesbf, s, start=(k == 0), stop=(k == KC - 1))
        r = rstd[:, tb % 2, :].sub(tb % 2)
        rsqrt(c, r, ps, 1.0 / D)
        for k in range(KC):
            eng = "dve" if k % 2 == 0 else "pool"
            P.stt(eng, out[:, k, blk(tb)], xT[:, k, blk(tb)].sub(k), gcol[:, k:k + 1], r, ALU.mult, ALU.mult)


def seq_layer(c, s, l, first, last):
    P, ar = c.P, c.ar
    ar.reset()
    xT = c.xT
    ar.top_live = True
    if first:
        for k in range(KC):
            P.dma("sp", xT[:, k, :].sub(k), c.xT_d[s, :, k, :])
    phase_norm(c, xT, c.cst[:, l, C_GMIX:C_GMIX + 8], c.hT)
    if not first:
        for k in range(KC):
            P.dma("sp", c.xs_d[:, k, :].sub(k), xT[:, k, :].sub(k))
    if "hT" in c.dbg_d:
        P.dma("pool", c.dbg_d["hT"], c.hT)
    ar.reset()
    ar.top_live = False
    obr = ar.alloc("obr", [4, 2, T], BF16)
    if "pool" in c.stages:
        mixer_pool(c, l, obr)
    if "gla" in c.stages:
        mixer_gla(c, l, obr)
    if "gdn" in c.stages:
        mixer_gdn(c, l, obr)
    if "nsa" in c.stages:
        mixer_nsa(c, l, obr)
    if "obr" in c.dbg_d:
        P.dma("pool", c.dbg_d["obr"], obr)
    if "G" not in c.stages:
        return
    ar.top_live = True
    xsrc = c.xT_d[s] if first else c.xs_d
    for k in range(KC):
        P.dma("sp", xT[:, k, :].sub(k), xsrc[:, k, :].sub(k) if not first else xsrc[:, k, :])
    phase_gate(c, l, obr, xT)
    dump_x(c, "x1", xT)
    ar.reset()
    if "X" in c.stages:
        phase_cross(c, s, l, xT)
        dump_x(c, "x2", xT)
        ar.reset()
    if "F" in c.stages:
        phase_ffn(c, l, xT)
        dump_x(c, "x3", xT)
        ar.reset()
    if last:
        phase_final(c, s, xT)


def dump_x(c, name, xT):
    if name in c.dbg_d:
        for k in range(KC):
            c.P.dma("sp", c.dbg_d[name][:, k, :], xT[:, k, :].sub(k))


def add_resid(c, xT, m, tb, ps, i):
    xv = xT[:, m, blk(tb)].sub(m)
    c.P.tt("dve", xv, xv, ps, ALU.add)


def phase_gate(c, l, obr, xT):
    P, ar = c.P, c.ar
    yT = ar.alloc("yT", [KC, T], BF16)
    yacc = ar.alloc("yacc", [T])
    sg = ar.alloc("sg", [2, 512])
    tm = ar.alloc("tm", [2, 512])
    wsl = ar.alloc("wsl", [3, KC, 128], BF16)
    wbl = ar.alloc("wbl", [3, 2, 128], BF16)
    srcs = []
    for m in range(8):
        for i in range(4):
            srcs.append(((lambda b: b[0]), c.w_gate_d[l, i, m]))
    bufs = [(wsl[:, i].sub(i), wbl[:, i].sub(i)) for i in range(3)]
    SLg = Slabs(c, bufs, [[((lambda b: b[0]), c.w_gate_d[l, i, m]), ((lambda b: b[1]), c.w_branch_d[l, i, m])]
                          for m in range(8) for i in range(4)])
    for m in range(8):
        for i in range(4):
            wg, wb = SLg.nxt()
            for tb in range(NB):
                psg = c.psb[(2 * tb) % 4]
                psr = c.psb[(2 * tb) % 4 + 1]
                proj_fm(c, wg, c.hT, tb, psg)
                for k2 in range(2):
                    P.mm(psr, wb[:, k2, :], obr[:, i, k2, blk(tb)], start=(k2 == 0), stop=(k2 == 1))
                sgv = sg[:, tb % 2, :].sub(tb % 2)
                P.act(sgv, psg, AF.Sigmoid, bias=c.cst[:, l, C_BGATE + 8 * i + m:C_BGATE + 8 * i + m + 1], scale=1.0)
                if i == 0:
                    P.tt("dve", yacc[:, blk(tb)].sub(tb), sgv, psr, ALU.mult)
                else:
                    tv = tm[:, tb % 2, :].sub(tb % 2)
                    P.tt("dve", tv, sgv, psr, ALU.mult)
                    P.tt("dve", yacc[:, blk(tb)].sub(tb), yacc[:, blk(tb)].sub(tb), tv, ALU.add)
        for tb in range(NB):
            P.cp("act", yT[:, m, blk(tb)].sub(m), yacc[:, blk(tb)].sub(tb))
    SLo = Slabs(c, [wsl[:, i].sub(i) for i in range(3)], [c.w_out_d[l, m] for m in range(8)])
    for m in range(8):
        wo = SLo.nxt()
        for tb in range(NB):
            ps = c.psb[4 + tb % 4]
            for k in range(KC):
                P.mm(ps, wo[:, k, :], yT[:, k, blk(tb)].sub(k), start=(k == 0), stop=(k == KC - 1))
            add_resid(c, xT, m, tb, ps, tb)


def phase_cross(c, s, l, xT):
    P, ar = c.P, c.ar
    phase_norm(c, xT, c.cst[:, l, C_GCROSS:C_GCROSS + 8], c.hT)
    memT = ar.alloc("memT", [KC, 256])
    memn = ar.alloc("memn", [KC, 256], BF16)
    sq = ar.alloc("msq", [2, 256], BF16)
    rstd = ar.alloc("mrstd", [256])
    KT = ar.alloc("KT", [4, 256], BF16)
    Vtok = ar.alloc("Vtok", [2, 512], BF16)
    wv = ar.alloc("wv", [KC, 512], BF16)
    qh = ar.alloc("qh", [2, 512], BF16)
    ee = ar.alloc("xe", [2, 2, 512], BF16)
    oX = ar.alloc("oX", [4, T], BF16)
    rr = ar.alloc("xr", [2, 512])
    wsl = ar.alloc("wsl", [3, KC, 128], BF16)
    wxo = ar.alloc("wxo", [3, 4, 128], BF16)
    SL = Slabs(c, [wsl[:, i].sub(i) for i in range(3)], [c.w_kk_d[l, hh] for hh in range(4)] + [c.w_xq_d[l, hh] for hh in range(4)])
    SLo = Slabs(c, [wxo[:, i].sub(i) for i in range(3)], [c.w_xo_d[l, m] for m in range(8)])
    wdma(c, wv, c.w_v_d[l])
    P.dma("sp", memT, c.memT_d[s])
    ps = c.psb[0]
    for k in range(KC):
        sv = sq[:, k % 2, :].sub(k % 2)
        P.act(sv, memT[:, k, :], AF.Square)
        P.mm(ps[:, 0:256], c.onesbf, sv, start=(k == 0), stop=(k == KC - 1))
    rsqrt(c, rstd, ps[:, 0:256], 1.0 / D)
    for k in range(KC):
        P.stt("dve", memn[:, k, :], memT[:, k, :], c.cst[:, l, C_GMEM + k:C_GMEM + k + 1], rstd, ALU.mult, ALU.mult)
    n = 0
    for hh in range(4):
        w = SL.nxt()
        ps = c.psb[1 + hh % 2]
        for k in range(KC):
            P.mm(ps[:, 0:256], w[:, k, :], memn[:, k, :], start=(k == 0), stop=(k == KC - 1))
        P.cp("act", KT[:, hh, :], ps[:, 0:256])
    for mt in range(2):
        ps = c.psb[3 + mt]
        for k in range(KC):
            P.mm(ps, memn[:, k, mt * 128:(mt + 1) * 128], wv[:, k, :], start=(k == 0), stop=(k == KC - 1))
        P.cp("act", Vtok[:, mt, :], ps)
    it = 0
    for hh in range(4):
        w = SL.nxt()
        for tb in range(NB):
            r2 = it % 2
            it += 1
            psq = c.psb[r2]
            proj_fm(c, w, c.hT, tb, psq)
            qv = qh[:, r2, :].sub(r2)
            P.ts("dve", qv, psq, 128.0 ** -0.5, ALU.mult)
            psO, psD = c.psb[4 + r2], c.psb[6 + r2]
            for mt in range(2):
                psS = c.psb[2 + mt]
                P.mm(psS, KT[:, hh, mt * 128:(mt + 1) * 128], qv)
                ev = ee[:, r2, mt, :].sub((r2, mt))
                P.act(ev, psS, AF.Exp)
                P.mm(psO, Vtok[:, mt, hh * 128:(hh + 1) * 128], ev, start=(mt == 0), stop=(mt == 1))
                P.mm(psD, c.onesbf, ev, start=(mt == 0), stop=(mt == 1))
            rv = rr[:, r2, :].sub(r2)
            P.op("dve", (lambda oa, ia: lambda e: e.reciprocal(oa, ia))(rv.ap, psD.ap), _k(psD), _k(rv))
            P.tt("dve", oX[:, hh, blk(tb)].sub(hh), psO, rv, ALU.mult)
    for m in range(8):
        w = SLo.nxt()
        for tb in range(NB):
            ps = c.psb[tb % 4]
            for k in range(4):
                P.mm(ps, w[:, k, :], oX[:, k, blk(tb)].sub(k), start=(k == 0), stop=(k == 3))
            add_resid(c, xT, m, tb, ps, tb)


FGROUPS = ((0, 6), (6, 12), (12, 17), (17, 22))


def phase_ffn(c, l, xT):
    P, ar = c.P, c.ar
    phase_norm(c, xT, c.cst[:, l, C_GFFN:C_GFFN + 8], c.hT)
    aT = ar.alloc("aT", [6, T], BF16)
    upad = ar.alloc("upad", [2, 2 + T])
    c1 = ar.alloc("c1", [2, 512])
    gb = ar.alloc("gb", [2, 512], BF16)
    wu = ar.alloc("wu", [3, KC, 256], BF16)
    wd = ar.alloc("wd", [2, 6, D], BF16)
    SLu = Slabs(c, [wu[:, i].sub(i) for i in range(3)], [c.w_up_d[l, f] for f in range(NF)])
    P.memset("pool", upad[:, 0, 0:2].sub(0), 0.0)
    P.memset("pool", upad[:, 1, 0:2].sub(1), 0.0)
    cs = c.cst
    n = 0
    for gi, (f0, f1) in enumerate(FGROUPS):
        wdg = wd[:, gi % 2].sub(gi % 2)
        for fi, f in enumerate(range(f0, f1)):
            P.dma("pool", wdg[:, fi, :], c.w_down_d[l, f])
        for fi, f in enumerate(range(f0, f1)):
            w = SLu.nxt()
            up = upad[:, n % 2].sub(n % 2)
            n += 1
            cw = C_FFN_CONV + 3 * f
            for tb in range(NB):
                psu, psv = c.psb[(2 * tb) % 4], c.psb[(2 * tb) % 4 + 1]
                for k in range(KC):
                    P.mm(psu, w[:, k, 0:128], c.hT[:, k, blk(tb)], start=(k == 0), stop=(k == KC - 1))
                for k in range(KC):
                    P.mm(psv, w[:, k, 128:256], c.hT[:, k, blk(tb)], start=(k == 0), stop=(k == KC - 1))
                b0 = tb * 512
                P.cp("act", up[:, 2 + b0:2 + b0 + 512], psu)
                cv = c1[:, tb % 2, :].sub(tb % 2)
                P.ts("dve", cv, up[:, b0:b0 + 512], cs[:, l, cw:cw + 1], ALU.mult, cs[:, l, C_FFN_B + f:C_FFN_B + f + 1], ALU.add)
                P.stt("dve", cv, up[:, b0 + 1:b0 + 513], cs[:, l, cw + 1:cw + 2], cv, ALU.mult, ALU.add)
                P.stt("dve", cv, up[:, b0 + 2:b0 + 514], cs[:, l, cw + 2:cw + 3], cv, ALU.mult, ALU.add)
                gv = gb[:, tb % 2, :].sub(tb % 2)
                P.act(gv, cv, AF.Gelu_apprx_tanh)
                P.tt("dve", aT[:, fi, blk(tb)].sub(fi), gv, psv, ALU.mult)
        nf = f1 - f0
        for m in range(8):
            for tb in range(NB):
                ps = c.psb[4 + tb % 4]
                for fi in range(nf):
                    P.mm(ps, wdg[:, fi, m * 128:(m + 1) * 128], aT[:, fi, blk(tb)].sub(fi), start=(fi == 0), stop=(fi == nf - 1))
                add_resid(c, xT, m, tb, ps, tb)


def phase_final(c, s, xT):
    P, ar = c.P, c.ar
    sq = ar.alloc("fsq", [4, 512], BF16)
    rstd = ar.alloc("frstd", [2, 512])
    ob = ar.alloc("fob", [4, 512])
    n = 0
    for tb in range(NB):
        ps = c.psb[tb % 2]
        for k in range(KC):
            sv = sq[:, k % 4, :].sub(k % 4)
            P.act(sv, xT[:, k, blk(tb)].sub(k), AF.Square)
            P.mm(ps, c.onesbf, sv, start=(k == 0), stop=(k == KC - 1))
        r = rstd[:, tb % 2, :].sub(tb % 2)
        rsqrt(c, r, ps, 1.0 / D)
        for k in range(KC):
            o = ob[:, n % 4, :].sub(n % 4)
            n += 1
            P.stt("dve", o, xT[:, k, blk(tb)].sub(k), c.gfin[:, k:k + 1], r, ALU.mult, ALU.mult)
            P.dma("sp", c.out_d[s, :, k, blk(tb)], o)
    ar.reset()


def mixer_pool(c, l, obr):
    P, ar = c.P, c.ar
    m = ar.mark()
    W = 16 + T
    upad = ar.alloc("upad", [W])
    st = [ar.alloc("s%d" % i, [W]) for i in range(4)]
    dif = ar.alloc("dif", [T], BF16)
    d32 = ar.alloc("d32", [16])
    wsl = ar.alloc("wsl", [2, KC, 128], BF16)
    pw = ar.alloc("pw", [2, 128], BF16)
    SL = Slabs(c, [wsl[:, i].sub(i) for i in range(2)], [c.w_in_fm[l, FM_PIN], c.w_in_fm[l, FM_PIN + 1]])
    P.memset("pool", upad[:, 0:16], 0.0)
    for i in range(4):
        P.memset("pool", st[i][:, 0:16], 0.0)
    for ch in range(2):
        w = SL.nxt()
        wdma(c, pw[:, ch, :].sub(ch), c.pool_bd_d[l, ch])
        for tb in range(NB):
            ps = c.psb[tb % 2]
            proj_fm(c, w, c.hT, tb, ps)
            P.cp("act", upad[:, 16 + tb * 512:16 + (tb + 1) * 512], ps)
        src = upad
        nst = 2 if ch == 0 else 4
        for i in range(nst):
            sh = 1 << i
            P.tt("dve" if i % 2 == 0 else "pool", st[i][:, sh:W], src[:, sh:W], src[:, 0:W - sh], ALU.add)
            src = st[i]
        for gg in range(2):
            rows = slice(64 * gg, 64 * gg + 64)
            sw = st[(0 if ch == 0 else 2) + gg]
            P.tt("dve", d32[rows, :], sw[rows, 16:32], c.pool16[rows, ch, :], ALU.mult)
            P.stt("dve", dif[rows, :], sw[rows, 16:W], c.cst[rows, l, C_POOLINVW + ch:C_POOLINVW + ch + 1], upad[rows, 16:W],
                  ALU.mult, ALU.subtract)
            P.tt("dve", dif[rows, 0:16], d32[rows, :], upad[rows, 16:32], ALU.subtract)
        for tb in range(NB):
            ps = c.psb[2 + tb % 2]
            P.mm(ps, pw[:, ch, :].sub(ch), dif[:, blk(tb)])
            P.ts("dve", obr[:, 0, ch, blk(tb)], ps, c.cst[:, l, C_POOLSC + ch:C_POOLSC + ch + 1], ALU.mult)
    ar.release(m)


def tile_(t):
    return slice(t * 128, (t + 1) * 128)


def mixer_gla(c, l, obr):
    P, ar = c.P, c.ar
    m = ar.mark()
    rmask = ar.alloc("rmask", [T]); P.dma("sp", rmask, c.rmask_d)
    wlr = ar.alloc("wlr", [128]); P.dma("sp", wlr, c.wlr_d[l])
    tmp = ar.alloc("tmp", [T])
    tmp2 = ar.alloc("tmp2", [T])
    B = ar.alloc("B", [T])
    qe = ar.alloc("qe", [T], BF16)
    kpad = ar.alloc("kpad", [4, T], BF16)
    kupdT = ar.alloc("kupdT", [T])
    kupdtok = ar.alloc("kupdtok", [NT, 128], BF16)
    vtok = ar.alloc("vtok", [NT, 256], BF16)
    rs = ar.alloc("rs", [2, T], BF16)
    oT = ar.alloc("oT", [2, T])
    dec = ar.alloc("dec", [NT])
    negb = ar.alloc("negb", [1])
    wsl = ar.alloc("wsl", [3, KC, 128], BF16)
    wtm = ar.alloc("wtm", [KC, 256], BF16)
    S = ar.alloc("S", [256]); Sbf = ar.alloc("Sbf", [256], BF16); Stmp = ar.alloc("Stmp", [256])
    attm = ar.alloc("attm", [2, 4, 128], BF16)
    sqb = ar.alloc("sqb", [2, 512], BF16)
    SL = Slabs(c, [wsl[:, i].sub(i) for i in range(3)],
               [c.w_in_fm[l, i] for i in (FM_S3, FM_AQ, FM_AKP, FM_AKP + 1, FM_AKP + 2, FM_AKP + 3, FM_AK, FM_AR, FM_AR + 1)])
    wdma(c, wtm, c.w_in_tm[l, :, :, 0:256])

    def slab(idx):
        return SL.nxt()
    psn = [0]

    def nps():
        p = c.psb[psn[0] % 2]
        psn[0] += 1
        return p
    w = slab(FM_S3)
    for tb in range(NB):
        ps = nps()
        proj_fm(c, w, c.hT, tb, ps)
        P.cp("act", tmp[:, blk(tb)], ps)
    P.ts("pool", negb, c.cst[:, l, C_GLA_B:C_GLA_B + 1], -1.0, ALU.mult)
    for tb in range(NB):
        ps = nps()
        P.mm(ps, wlr[32:48, :], tmp[32:48, blk(tb)])
        P.act(tmp2[:, blk(tb)], ps, AF.Exp, bias=negb, scale=-1.0)
        P.act(tmp2[:, blk(tb)], tmp2[:, blk(tb)], AF.Ln, bias=c.onec, scale=1.0)
    P.ts("pool", tmp2, tmp2, -1.0 / 16.0, ALU.mult)
    P.scan(B, rmask, tmp2, 0.0, ALU.mult, ALU.add)
    P.act(tmp, B, AF.Exp)
    P.act(tmp2, B, AF.Exp, scale=-1.0)
    P.cp("pool", dec, tmp.re("p (a b) -> p a b", b=128)[:, :, 127])
    w = slab(FM_AQ)
    for tb in range(NB):
        ps = nps()
        proj_fm(c, w, c.hT, tb, ps)
        P.stt("dve", qe[:, blk(tb)], ps, 32.0 ** -0.5, tmp[:, blk(tb)], ALU.mult, ALU.mult)
    for h in range(4):
        w = slab(FM_AKP + h)
        for tb in range(NB):
            ps = nps()
            proj_fm(c, w, c.hT, tb, ps)
            P.tt("dve", kpad[:, h, blk(tb)], ps, tmp2[:, blk(tb)], ALU.mult)
    w = slab(FM_AK)
    for tb in range(NB):
        ps = nps()
        proj_fm(c, w, c.hT, tb, ps)
        for t4 in range(4):
            tl = tile_(tb * 4 + t4)
            last = tb * 512 + t4 * 128 + 127
            P.act(tmp[:, tl], B[:, tl], AF.Exp, bias=B[:, last:last + 1], scale=-1.0)
        P.tt("dve", kupdT[:, blk(tb)], ps, tmp[:, blk(tb)], ALU.mult)
    for t in range(NT):
        ps = nps()
        for k in range(KC):
            P.mm(ps[:, 0:256], c.hT[:, k, tile_(t)], wtm[:, k, :], start=(k == 0), stop=(k == KC - 1))
        P.cp("act", vtok[:, t, :], ps[:, 0:256])
    for ch in range(2):
        w = slab(FM_AR + ch)
        for tb in range(NB):
            ps = nps()
            proj_fm(c, w, c.hT, tb, ps)
            P.act(rs[:, ch, blk(tb)], ps, AF.Silu)
    for t in range(NT):
        ps = nps()
        P.tr(ps[:, 0:128], kupdT[:, tile_(t)], c.ident)
        P.cp("act", kupdtok[:, t, :], ps[:, 0:128])
    P.memset("pool", S, 0.0)
    for t in range(NT):
        tl = tile_(t)
        psA = c.psb[4 + t % 2]
        for h in range(4):
            P.mm(psA[:, h * 128:(h + 1) * 128], kpad[:, h, tl], qe[:, tl])
        am = attm[:, t % 2].sub(t % 2)
        P.tt("dve", am, psA.re("p (h f) -> p h f", h=4), c.tri_ui.re("p (o f) -> p o f", o=1).bc([128, 4, 128]), ALU.mult)
        psO = c.psb[6 + t % 2]
        for ch in range(2):
            for j in range(2):
                h = 2 * ch + j
                P.mm(psO[64 * j:64 * j + 64, ch * 128:(ch + 1) * 128], vtok[:, t, 64 * h:64 * h + 64], am[:, h, :],
                     start=True, stop=(t == 0))
            if t > 0:
                P.mm(psO[:, ch * 128:(ch + 1) * 128], Sbf[:, ch * 128:(ch + 1) * 128], qe[:, tl], start=False, stop=True)
        P.cp("act", oT[:, :, tl], psO[:, 0:256].re("p (c f) -> p c f", c=2))
        if t < NT - 1:
            psS = c.psb[2 + t % 2]
            P.mm(psS[:, 0:256], kupdtok[:, t, :], vtok[:, t, :])
            P.tt("dve", Stmp, psS[:, 0:256], c.bdmask, ALU.mult)
            P.stt("dve", S, S, dec[:, t:t + 1], Stmp, ALU.mult, ALU.add)
            P.cp("act", Sbf, S)
    for ch in range(2):
        for tb in range(NB):
            ps = nps()
            sv = sqb[:, tb % 2, :].sub(tb % 2)
            P.act(sv, oT[:, ch, blk(tb)], AF.Square)
            P.mm(ps, c.bd64bf, sv)
            rsqrt(c, tmp[:, blk(tb)], ps, 1.0 / 64.0)
            P.tt("dve", tmp[:, blk(tb)], oT[:, ch, blk(tb)], tmp[:, blk(tb)], ALU.mult)
            P.stt("dve", obr[:, 1, ch, blk(tb)], tmp[:, blk(tb)], c.cst[:, l, C_GLA_GN:C_GLA_GN + 1], rs[:, ch, blk(tb)],
                  ALU.mult, ALU.mult)
    ar.release(m)


def mixer_gdn(c, l, obr):
    P, ar = c.P, c.ar
    m = ar.mark()
    b0 = ar.alloc("b0", [3 + T])
    b1 = ar.alloc("b1", [T])
    b2 = ar.alloc("b2", [T])
    b3 = ar.alloc("b3", [T])
    qnT = ar.alloc("qnT", [T]); knT = ar.alloc("knT", [T])
    ktok = ar.alloc("ktok", [NT, 128]); vtok = ar.alloc("vtok", [NT, 128])
    S1tok = ar.alloc("S1tok", [NT, 96]); S2tok = ar.alloc("S2tok", [NT, 64])
    dgT = ar.alloc("dgT", [T], BF16)
    oT = ar.alloc("oT", [T])
    wsl = ar.alloc("wsl", [3, KC, 128], BF16)
    sqb = ar.alloc("sqb", [2, 512], BF16)
    Acol = ar.alloc("Acol", [1]); negA = ar.alloc("negA", [1])
    Sp = ar.alloc("Spair", [64])
    NCH = 4
    ch_t = []
    for i in range(NCH):
        d = {}
        for nm in ("ea", "eb", "egb", "Xf"):
            d[nm] = ar.alloc("%s%d" % (nm, i), [128])
        for nm in ("Xa", "Xta", "Xb", "Xtb", "Tt", "Aqk", "qdec", "wT"):
            d[nm] = ar.alloc("%s%d" % (nm, i), [128], BF16)
        d["u"] = ar.alloc("u%d" % i, [64])
        for nm in ("vb", "kbe", "kdec", "vnew"):
            d[nm] = ar.alloc("%s%d" % (nm, i), [64], BF16)
        ch_t.append(d)
    knTb = ar.alloc("knTb", [T], BF16); qnTb = ar.alloc("qnTb", [T], BF16)
    Spb = ar.alloc("Spb", [64], BF16)
    SL = Slabs(c, [wsl[:, i].sub(i) for i in range(3)],
               [c.w_in_fm[l, i] for i in (FM_S1, FM_S2, FM_DQ, FM_DK, FM_DV, FM_DG, FM_DQ + 1, FM_DK + 1, FM_DV + 1, FM_DG + 1)])

    def slab(idx):
        return SL.nxt()
    psn = [0]

    def nps():
        p = c.psb[psn[0] % 2]
        psn[0] += 1
        return p
    cs = c.cst
    P.dma("sp", b1, c.rmask_d)
    w = slab(FM_S1)
    for tb in range(NB):
        ps = nps()
        proj_fm(c, w, c.hT, tb, ps)
        P.cp("act", b0[:, blk(tb)], ps)
    P.act(Acol, cs[:, l, C_GDN_ALOG:C_GDN_ALOG + 1], AF.Exp)
    P.ts("pool", negA, Acol, -1.0, ALU.mult)
    z1 = b0[:, 0:T]
    P.act(z1, z1, AF.Exp, bias=cs[:, l, C_GDN_DTB:C_GDN_DTB + 1], scale=1.0)
    P.act(z1, z1, AF.Ln, bias=c.onec, scale=1.0)
    P.ts("dve", z1, z1, negA, ALU.mult)
    P.scan(b2, b1, z1, 0.0, ALU.mult, ALU.add)
    w = slab(FM_S2)
    for tb in range(NB):
        ps = nps()
        proj_fm(c, w, c.hT, tb, ps)
        P.act(b3[:, blk(tb)], ps, AF.Sigmoid)
    g3 = b2[32:64, :].re("p (a b) -> p a b", b=128)
    P.tt("dve", b0[32:64, 0:T].re("p (a b) -> p a b", b=128), g3[:, :, 127:128].bc([32, NT, 128]), g3, ALU.subtract)
    P.act(b2[32:64, :], b0[32:64, 0:T], AF.Exp)
    P.act(b2[64:96, :], b2[64:96, :], AF.Exp)
    P.tt("dve", b2[64:96, :], b2[64:96, :], b3[64:96, :], ALU.mult)
    P.ts("pool", b3[32:64, :], b3[32:64, :], -1.0, ALU.mult)
    for t in range(NT):
        ps = nps()
        P.tr(ps[:, 0:128], b2[:, tile_(t)], c.ident)
        P.cp("act", S1tok[:, t, :], ps[:, 0:96])
        ps = nps()
        P.tr(ps[:, 0:128], b3[:, tile_(t)], c.ident)
        P.cp("act", S2tok[:, t, :], ps[:, 0:64])
    P.memset("pool", b0[:, 0:3], 0.0)
    for pr in range(2):
        for which, fmi, dst in ((0, FM_DQ, qnT), (1, FM_DK, knT), (2, FM_DV, b3)):
            w = slab(fmi + pr)
            for tb in range(NB):
                ps = nps()
                proj_fm(c, w, c.hT, tb, ps)
                P.cp("act", b0[:, 3 + tb * 512:3 + (tb + 1) * 512], ps)
            cc = C_GDN_CONV + (which * 2 + pr) * 4
            P.ts("dve", b1, b0[:, 0:T], cs[:, l, cc:cc + 1], ALU.mult)
            for tap in range(1, 4):
                P.stt("dve", b1, b0[:, tap:tap + T], cs[:, l, cc + tap:cc + tap + 1], b1, ALU.mult, ALU.add)
            P.act(dst, b1, AF.Silu)
            if which < 2:
                for tb in range(NB):
                    ps = nps()
                    sv = sqb[:, tb % 2, :].sub(tb % 2)
                    P.act(sv, dst[:, blk(tb)], AF.Square)
                    P.mm(ps, c.bd64bf, sv)
                    rsqrt(c, b1[:, blk(tb)], ps, 1.0)
                    if which == 0:
                        P.stt("dve", dst[:, blk(tb)], dst[:, blk(tb)], 0.125, b1[:, blk(tb)], ALU.mult, ALU.mult)
                    else:
                        P.tt("dve", dst[:, blk(tb)], dst[:, blk(tb)], b1[:, blk(tb)], ALU.mult)
        P.cp("pool", qnTb, qnT)
        P.cp("pool", knTb, knT)
        w = slab(FM_DG + pr)
        for tb in range(NB):
            ps = nps()
            proj_fm(c, w, c.hT, tb, ps)
            P.act(dgT[:, blk(tb)], ps, AF.Silu)
        for t in range(NT):
            ps = nps()
            P.tr(ps[:, 0:128], knT[:, tile_(t)], c.ident)
            P.cp("act", ktok[:, t, :], ps[:, 0:128])
            ps = nps()
            P.tr(ps[:, 0:128], b3[:, tile_(t)], c.ident)
            P.cp("dve", vtok[:, t, :], ps[:, 0:128])
        P.memset("pool", Sp, 0.0)
        P.memset("pool", Spb, 0.0)
        P.barrier()
        for t0 in range(0, NT, 2):
            chains = []
            for dt_ in range(2):
                for j in range(2):
                    ci = dt_ * 2 + j
                    chains.append((ci, t0 + dt_, j))

            def q_(ci, b, qd):
                bank = c.psb[(ci * 2 + b) % 8]
                return bank[:, qd * 128:(qd + 1) * 128].sub(qd)
            for (ci, t, j) in chains:
                h = 2 * pr + j
                P.mm(q_(ci, 0, 0), c.selA[0:4, h, :], b2[0:4, tile_(t)])
            for (ci, t, j) in chains:
                h = 2 * pr + j; d = ch_t[ci]
                gcol = S1tok[:, t, h:h + 1]
                P.ts("dve", d["ea"], q_(ci, 0, 0), gcol, ALU.subtract, 0.0, ALU.max)
                P.ts("dve", d["eb"], q_(ci, 0, 0), gcol, ALU.subtract, 0.0, ALU.min)
                P.act(d["egb"], q_(ci, 0, 0), AF.Exp)
                P.act(d["ea"], d["ea"], AF.Exp, scale=-1.0)
                P.act(d["eb"], d["eb"], AF.Exp)
            for (ci, t, j) in chains:
                hr = slice(64 * j, 64 * j + 64)
                P.mm(q_(ci, 0, 1), knTb[hr, tile_(t)], knTb[hr, tile_(t)])
                P.mm(q_(ci, 0, 2), knTb[hr, tile_(t)], qnTb[hr, tile_(t)])
            for (ci, t, j) in chains:
                h = 2 * pr + j; d = ch_t[ci]
                P.tt("dve", d["Xf"], q_(ci, 0, 1), d["ea"], ALU.mult)
                P.stt("dve", d["Xf"], d["Xf"], S2tok[:, t, 32 + h:33 + h], c.tri_sl, ALU.mult, ALU.mult)
                P.cp("pool", d["Xa"], d["Xf"])
                P.tt("dve", d["eb"], q_(ci, 0, 2), d["eb"], ALU.mult)
                P.tt("pool", d["Aqk"], d["eb"], c.tri_ui, ALU.mult)
            for (ci, t, j) in chains:
                d = ch_t[ci]
                P.tr(q_(ci, 0, 3), d["Xf"], c.ident)
            for (ci, t, j) in chains:
                d = ch_t[ci]
                P.cp("act", d["Xta"], q_(ci, 0, 3))
                P.tt("dve", d["Tt"], q_(ci, 0, 3), c.ident, ALU.add)
            cur = {ci: ("Xa", "Xta") for ci in range(NCH)}
            for it in range(6):
                for (ci, t, j) in chains:
                    d = ch_t[ci]; xc, xtc = cur[ci]
                    P.mm(q_(ci, 1, 0), d[xtc], d[xc])
                    if it < 5:
                        P.mm(q_(ci, 1, 1), d[xc], d[xtc])
                for (ci, t, j) in chains:
                    d = ch_t[ci]; xc, xtc = cur[ci]
                    nx, nxt = ("Xb", "Xtb") if xc == "Xa" else ("Xa", "Xta")
                    P.cp("act", d[nx], q_(ci, 1, 0))
                    if it < 5:
                        P.cp("dve", d[nxt], q_(ci, 1, 1))
                    cur[ci] = (nx, nxt)
                for (ci, t, j) in chains:
                    d = ch_t[ci]; xc, xtc = cur[ci]
                    P.mm(q_(ci, 1, 2), d[xc], d["Tt"])
                for (ci, t, j) in chains:
                    d = ch_t[ci]
                    P.tt("dve", d["Tt"], d["Tt"], q_(ci, 1, 2), ALU.add)
            for (ci, t, j) in chains:
                h = 2 * pr + j; d = ch_t[ci]
                hc = slice(64 * j, 64 * j + 64)
                P.ts("pool", d["vb"], vtok[:, t, hc], S2tok[:, t, h:h + 1], ALU.mult)
                P.ts("pool", d["kbe"], ktok[:, t, hc], S1tok[:, t, 64 + h:65 + h], ALU.mult)
                P.ts("pool", d["kdec"], ktok[:, t, hc], S1tok[:, t, 32 + h:33 + h], ALU.mult)
                P.tt("pool", d["qdec"][hc, :], qnT[hc, tile_(t)], d["egb"][hc, :], ALU.mult)
            for (ci, t, j) in chains:
                d = ch_t[ci]
                hr = slice(64 * j, 64 * j + 64)
                P.mm(q_(ci, 1, 3)[:, 0:64], d["Tt"], d["vb"])
                P.mm(q_(ci, 0, 0)[hr, :], d["kbe"], d["Tt"])
            for (ci, t, j) in chains:
                d = ch_t[ci]
                hr = slice(64 * j, 64 * j + 64)
                P.cp("act", d["u"], q_(ci, 1, 3)[:, 0:64])
                P.cp("dve", d["wT"][hr, :], q_(ci, 0, 0)[hr, :])
            for (ci, t, j) in chains:
                d = ch_t[ci]
                hr = slice(64 * j, 64 * j + 64)
                P.mm(q_(ci, 0, 1)[:, 0:64], d["wT"][hr, :], Spb[hr, :].sub(j))
                P.tt("dve", d["vnew"], d["u"], q_(ci, 0, 1)[:, 0:64], ALU.subtract)
                P.mm(q_(ci, 0, 2)[hr, :], Spb[hr, :].sub(j), d["qdec"][hr, :], start=True, stop=False)
                P.mm(q_(ci, 0, 2)[hr, :], d["vnew"], d["Aqk"], start=False, stop=True)
                P.cp("act", oT[hr, tile_(t)].sub(j), q_(ci, 0, 2)[hr, :])
                P.mm(q_(ci, 0, 3)[hr, 0:64], d["kdec"], d["vnew"])
                P.stt("dve", Sp[hr, :].sub(j), Sp[hr, :].sub(j), d["egb"][hr, 127:128], q_(ci, 0, 3)[hr, 0:64], ALU.mult, ALU.add)
                P.cp("act", Spb[hr, :].sub(j), Sp[hr, :].sub(j))
        P.barrier()
        for tb in range(NB):
            ps = nps()
            sv = sqb[:, tb % 2, :].sub(tb % 2)
            P.act(sv, oT[:, blk(tb)], AF.Square)
            P.mm(ps, c.bd64bf, sv)
            rsqrt(c, b1[:, blk(tb)], ps, 1.0 / 64.0)
            P.tt("dve", b1[:, blk(tb)], oT[:, blk(tb)], b1[:, blk(tb)], ALU.mult)
            P.stt("dve", obr[:, 2, pr, blk(tb)], b1[:, blk(tb)], cs[:, l, C_GDN_GN:C_GDN_GN + 1], dgT[:, blk(tb)],
                  ALU.mult, ALU.mult)
    ar.release(m)


TINY = 1e-30


def mixer_nsa(c, l, obr):
    P, ar = c.P, c.ar
    m = ar.mark()
    gT = ar.alloc("gT", [T])
    kcT = ar.alloc("kcT", [T], BF16); vcT = ar.alloc("vcT", [T], BF16)
    w1 = ar.alloc("w1", [32, 128], BF16)
    peT = ar.alloc("peT", [32, 8], BF16)
    kcR = ar.alloc("kcR", [16, 128], BF16)
    w2d = ar.alloc("w2d", [2, 128], BF16); w2v = ar.alloc("w2v", [128], BF16)
    b1c = ar.alloc("b1c", [1])
    gl = ar.alloc("gl", [128], BF16)
    ckdup = ar.alloc("ckdup", [2, 128], BF16)
    cvtok = ar.alloc("cvtok", [128], BF16)
    qT = ar.alloc("qT", [2, T], BF16)
    ksd = ar.alloc("ksd", [2, T], BF16); kwd = ar.alloc("kwd", [2, T], BF16)
    vstok = ar.alloc("vstok", [NT, 128], BF16); vwtok = ar.alloc("vwtok", [NT, 128], BF16)
    selT = ar.alloc("selT", [T], BF16)
    cmask = ar.alloc("cmask", [T], BF16); P.dma("pool", cmask, c.cmask_d)
    Esel = ar.alloc("Esel", [T], BF16); P.dma("pool", Esel, c.Esel_d)
    Wlong = ar.alloc("Wlong", [1408], BF16); P.dma("pool", Wlong, c.Wlong_d)
    cover1 = ar.alloc("cover1", [33], BF16); P.dma("pool", cover1, c.cover1_d)
    keepT = ar.alloc("keepT", [NT, 32]); P.dma("sp", keepT, c.keepT_d)
    addT = ar.alloc("addT", [NT, 32]); P.dma("sp", addT, c.addT_d)
    validT = ar.alloc("validT", [NT, 32]); P.dma("sp", validT, c.validT_d)
    selg = ar.alloc("selg", [2, 3, 128]); P.dma("sp", selg, c.selg_d)
    wsl = ar.alloc("wsl", [3, KC, 128], BF16)
    wtm = ar.alloc("wtm", [KC, 256], BF16)
    SL = Slabs(c, [wsl[:, i].sub(i) for i in range(3)],
               [c.w_in_fm[l, FM_S3], c.w_in_fm[l, FM_NKC], c.w_in_fm[l, FM_NVC], c.w_in_fm[l, FM_NQ], c.w_in_fm[l, FM_NQ + 1],
                c.nsadup_d[l, 0], c.nsadup_d[l, 1], c.nsadup_d[l, 2], c.nsadup_d[l, 3]])
    wdma(c, wtm, c.w_in_tm[l, :, :, 256:512])
    ee = ar.alloc("ee", [2, 2, 512], BF16)
    em = ar.alloc("em", [2, 2, 512], BF16)
    oacc = ar.alloc("oacc", [T])
    rt = ar.alloc("rt", [2, 512])
    im = {nm: ar.alloc(nm, [32]) for nm in ("imp", "vals", "wa", "wb", "sel")}
    m8 = ar.alloc("m8", [8]); rden = ar.alloc("rden", [2])
    nw = [0]

    def slab(src):
        return SL.nxt()
    psn = [0]

    def nps():
        p = c.psb[psn[0] % 2]
        psn[0] += 1
        return p
    w = slab(c.w_in_fm[l, FM_S3])
    for tb in range(NB):
        ps = nps()
        proj_fm(c, w, c.hT, tb, ps)
        P.act(gT[:, blk(tb)], ps, AF.Sigmoid)
    for fmi, dst in ((FM_NKC, kcT), (FM_NVC, vcT)):
        w = slab(c.w_in_fm[l, fmi])
        for tb in range(NB):
            ps = nps()
            proj_fm(c, w, c.hT, tb, ps)
            P.cp("act", dst[:, blk(tb)], ps)
    P.dma("pool", w2d, c.w2dup_d[l].re("g p m -> p g m"))
    P.dma("pool", w2v, c.w2vbd_d[l])
    P.memset("pool", gl, 0.0)
    P.memset("pool", ckdup, 0.0)
    for kv, src in ((0, kcT), (1, vcT)):
        P.dma("pool", w1, c.w1bd_d[l, kv])
        P.dma("pool", peT, c.peT_d[l, kv])
        P.cp("dve", kcR, src.re("p (c r) -> p r c", r=16))
        psC = nps()
        psB = nps()
        for li in range(32):
            s_, r_ = li // 16, li % 16
            P.mm(psC[:, 0:127], w1[:, li, :], kcR[:, r_, s_:s_ + 127], start=(li == 0), stop=(li == 31))
        for li in range(32):
            P.mm(psB[:, 0:8], w1[:, li, :], peT[:, li, :], start=(li == 0), stop=(li == 31))
        P.cp("dve", b1c, psB[:, 0:1])
        P.act(gl[:, 0:127], psC[:, 0:127], AF.Gelu_apprx_tanh, bias=b1c, scale=1.0)
        if kv == 0:
            for g in range(2):
                ps = nps()
                P.mm(ps[:, 0:128], w2d[:, g, :], gl)
                P.cp("act", ckdup[:, g, :], ps[:, 0:128])
        else:
            ps = nps()
            P.mm(ps[:, 0:128], gl, w2v)
            P.cp("act", cvtok, ps[:, 0:128])
    for g in range(2):
        w = slab(c.w_in_fm[l, FM_NQ + g])
        for tb in range(NB):
            ps = nps()
            proj_fm(c, w, c.hT, tb, ps)
            P.ts("dve", qT[:, g, blk(tb)], ps, 0.125, ALU.mult)
    for i, dst in ((0, ksd), (2, kwd)):
        for g in range(2):
            w = slab(c.nsadup_d[l, i + g])
            for tb in range(NB):
                ps = nps()
                proj_fm(c, w, c.hT, tb, ps)
                P.cp("act", dst[:, g, blk(tb)], ps)
    for t in range(NT):
        ps = nps()
        for k in range(KC):
            P.mm(ps[:, 0:256], c.hT[:, k, tile_(t)], wtm[:, k, :], start=(k == 0), stop=(k == KC - 1))
        P.cp("act", vstok[:, t, :], ps[:, 0:128])
        P.cp("dve", vwtok[:, t, :], ps[:, 128:256])
    P.barrier()
    lvl = getattr(c, "nsa_level", 9)
    if "nsa1" in c.dbg_d:
        P.dma("pool", c.dbg_d["nsa1"][:, 0:256], ckdup.re("p g n -> p (g n)"))
        P.dma("pool", c.dbg_d["nsa1"][:, 256:384], cvtok)
    if lvl < 2:
        ar.release(m)
        return
    psb = c.psb
    nrot = [0]

    def combine(g, tb, br, psO, psD, first):
        psG = psb[4]
        P.mm(psG, selg[0:12, g, br, :], gT[0:12, blk(tb)])
        r = rt[:, 0, :].sub(0)
        r2 = rt[:, 1, :].sub(1)
        P.ts("dve", r, psD, TINY, ALU.max)
        P.op("dve", (lambda oa: lambda e: e.reciprocal(oa, oa))(r.ap), _k(r), _k(r))
        P.tt("dve", r, r, psG, ALU.mult)
        if first:
            P.tt("dve", oacc[:, blk(tb)], psO, r, ALU.mult)
        else:
            P.tt("dve", r2, psO, r, ALU.mult)
            P.tt("pool", oacc[:, blk(tb)], oacc[:, blk(tb)], r2, ALU.add)

    for g in range(2):
        for tb in range(NB):
            psO, psD = psb[6], psb[7]
            rot = nrot[0] % 2
            nrot[0] += 1
            for j in range(2):
                hr = slice(64 * j, 64 * j + 64)
                psS = psb[j]
                P.mm(psS, ckdup[hr, g, :], qT[hr, g, blk(tb)])
                e = ee[:, rot, j, :].sub((rot, j))
                P.act(e, psS, AF.Exp)
                P.tt("pool" if j else "dve", e, e, cmask[:, blk(tb)], ALU.mult)
                P.mm(psO[hr, :], cvtok[:, 64 * g:64 * g + 64], e)
                P.mm(psD[hr, :], c.onesbf[:, 0:64], e)
            for t4 in range(4):
                qt = tb * 4 + t4
                psI = psb[2 + t4 % 2]
                for j in range(2):
                    e = ee[:, rot, j, t4 * 128:(t4 + 1) * 128].sub((rot, j))
                    P.mm(psI[:, j * 64:j * 64 + 33], e, cover1)
                pI = psI[:, 0:128].re("p (j f) -> p j f", j=2)
                P.ts("dve", rden, pI[:, :, 32], TINY, ALU.max)
                P.op("dve", (lambda oa: lambda e_: e_.reciprocal(oa, oa))(rden.ap), _k(rden), _k(rden))
                P.ts("dve", im["imp"], psI[:, 0:32], rden[:, 0:1], ALU.mult)
                P.stt("dve", im["imp"], psI[:, 64:96], rden[:, 1:2], im["imp"], ALU.mult, ALU.add)
                P.tt("dve", im["vals"], im["imp"], keepT[:, qt, :], ALU.mult)
                P.tt("dve", im["vals"], im["vals"], addT[:, qt, :], ALU.add)
                va, wa, wb = im["vals"].ap, im["wa"].ap, im["wb"].ap
                m8a = m8.ap
                P.op("dve", lambda e_: e_.max(out=m8a, in_=va), _k(im["vals"]), _k(m8))
                P.op("dve", lambda e_: e_.match_replace(out=wa, in_to_replace=m8a, in_values=va, imm_value=-2.0),
                     _k(im["vals"], m8), _k(im["wa"]))
                P.op("dve", lambda e_: e_.max(out=m8a, in_=wa), _k(im["wa"]), _k(m8))
                P.op("dve", lambda e_: e_.match_replace(out=wb, in_to_replace=m8a, in_values=wa, imm_value=-2.0),
                     _k(im["wa"], m8), _k(im["wb"]))
                P.tt("dve", im["wb"], im["vals"], im["wb"], ALU.subtract)
                P.stt("dve", im["sel"], im["wb"], 0.0, validT[:, qt, :], ALU.is_gt, ALU.mult)
                psT = psb[4 + t4 % 2]
                P.tr(psT[0:32, 0:128], im["sel"], c.ident)
                P.cp("act", selT[0:32, tile_(qt)], psT[0:32, 0:128])
            combine(g, tb, 0, psO, psD, True)
        P.barrier()
        if "nsa2" in c.dbg_d and g == 0:
            P.dma("pool", c.dbg_d["nsa2"], selT[0:32, :])
        if lvl < 3:
            continue
        for br, kd, vtok_ in ((1, ksd, vstok), (2, kwd, vwtok)):
            for tb in range(NB):
                psO, psD = psb[6], psb[7]
                kts = list(range(0, 4 * tb + 4)) if br == 1 else list(range(max(0, 4 * tb - 4), 4 * tb + 4))
                nk = len(kts)
                rots = []
                for ki in range(nk):
                    rots.append(nrot[0] % 2)
                    nrot[0] += 1

                def stage1(ki):
                    kt = kts[ki]
                    rot = rots[ki]
                    rel = kt - 4 * tb
                    if br == 1:
                        psM = psb[4 + rot]
                        P.mm(psM, Esel[0:32, tile_(kt)], selT[0:32, blk(tb)])
                    for j in range(2):
                        hr = slice(64 * j, 64 * j + 64)
                        psS = psb[2 * rot + j]
                        P.mm(psS, kd[hr, g, tile_(kt)], qT[hr, g, blk(tb)])
                        e = ee[:, rot, j, :].sub((rot, j))
                        P.act(e, psS, AF.Exp)
                        e2 = em[:, rot, j, :].sub((rot, j))
                        if br == 1:
                            P.tt("dve", e2, e, psM, ALU.mult)
                            if rel >= 0:
                                off = 384 - 128 * rel
                                P.tt("pool", e2, e2, Wlong[:, off:off + 512], ALU.mult)
                        else:
                            off = 384 - 128 * rel
                            P.tt("pool" if j else "dve", e2, e, Wlong[:, off:off + 512], ALU.mult)

                def stage2(ki):
                    kt = kts[ki]
                    rot = rots[ki]
                    for j in range(2):
                        hr = slice(64 * j, 64 * j + 64)
                        e2 = em[:, rot, j, :].sub((rot, j))
                        P.mm(psO[hr, :], vtok_[:, kt, 64 * g:64 * g + 64], e2, start=(ki == 0), stop=(ki == nk - 1))
                        P.mm(psD[hr, :], c.onesbf[:, 0:64], e2, start=(ki == 0), stop=(ki == nk - 1))
                for step in range(nk + 1):
                    if step < nk:
                        stage1(step)
                    if step >= 1:
                        stage2(step - 1)
                combine(g, tb, br, psO, psD, False)
        P.barrier()
        for tb in range(NB):
            P.cp("act", obr[:, 3, g, blk(tb)], oacc[:, blk(tb)])
    ar.release(m)


NCORES = 8
SEQ_PER_CORE = 4


def _to_fm(a):
    n, t, d = a.shape
    return np.ascontiguousarray(a.transpose(0, 2, 1).reshape(n, KC, 128, t).transpose(0, 2, 1, 3))


def kernel(**inputs):
    inp = {k: np.asarray(v) for k, v in inputs.items()}
    w = host_prep(inp)
    nc, c = build(nseq=SEQ_PER_CORE, nlayers=L)
    x = inp["x"].astype(np.float32, copy=False)
    mem = inp["mem"].astype(np.float32, copy=False)
    in_maps = []
    for core in range(NCORES):
        sl = slice(core * SEQ_PER_CORE, (core + 1) * SEQ_PER_CORE)
        m = dict(w)
        m["xT"] = _to_fm(x[sl])
        m["memT"] = _to_fm(mem[sl])
        in_maps.append(m)
    res = run_bass_kernel_spmd(nc, in_maps, core_ids=list(range(NCORES)))
    out = np.empty((NCORES * SEQ_PER_CORE, T, D), np.float32)
    for core in range(NCORES):
        oT = res.results[core]["outT"]
        for s in range(SEQ_PER_CORE):
            out[core * SEQ_PER_CORE + s] = oT[s].transpose(2, 1, 0).reshape(T, D)
    return out
```

```python
from contextlib import ExitStack
import numpy as np
import concourse.bass as bass
import concourse.mybir as mybir
from concourse.bass_utils import run_bass_kernel_spmd

F32 = mybir.dt.float32
BF16 = mybir.dt.bfloat16
AF = mybir.ActivationFunctionType
ALU = mybir.AluOpType

ENGS = ("pe", "act", "dve", "pool", "sp")
NDMASEM = 8

T = 2048
D = 1024
KC = 8
NB = 4
NT = 16
L = 2
DFF = 2816
NF = 22
EPS = 1e-6


class Op:
    __slots__ = ("eng", "fn", "reads", "writes", "dma", "deps", "signal", "sigval", "sem", "prewait", "bar")

    def __init__(self, eng, fn, reads, writes, dma):
        self.eng, self.fn, self.reads, self.writes, self.dma = eng, fn, reads, writes, dma
        self.deps = []
        self.signal = False
        self.sigval = None
        self.sem = None
        self.prewait = None
        self.bar = False


class V:
    __slots__ = ("ap", "key")

    def __init__(self, ap, key):
        self.ap, self.key = ap, key

    def __getitem__(self, idx):
        return V(self.ap[idx], self.key)

    def sub(self, k):
        return V(self.ap, (self.key, k))

    def bc(self, shape):
        return V(self.ap.to_broadcast(list(shape)), self.key)

    def re(self, s, **kw):
        return V(self.ap.rearrange(s, **kw), self.key)


def _k(*vs):
    out = []
    for v in vs:
        if isinstance(v, V):
            out.append(v.key)
    return out


def _a(v):
    return v.ap if isinstance(v, V) else v


class Prog:
    def __init__(self, nc):
        self.nc = nc
        self.ops = []
        self.es = ExitStack()
        self.uid = 0
        self.excl = set()

    def sb(self, name, shape, dt=F32):
        t = self.es.enter_context(self.nc.sbuf_tensor("sb_" + name, list(shape), dt))
        return t

    def ps(self, name, shape, dt=F32):
        return self.es.enter_context(self.nc.psum_tensor(name, list(shape), dt))

    def op(self, eng, fn, reads=(), writes=(), dma=False):
        o = Op(eng, fn, tuple(reads), tuple(writes), dma)
        self.ops.append(o)
        return o

    def barrier(self):
        o = Op("sp", None, (), (), False)
        o.bar = True
        self.ops.append(o)

    def dma(self, q, out, in_, **kw):
        oa, ia = _a(out), _a(in_)
        return self.op(q, lambda e: e.dma_start(out=oa, in_=ia, **kw), _k(in_), _k(out), dma=True)

    def mm(self, out, lhsT, rhs, start=True, stop=True):
        oa, la, ra = out.ap, lhsT.ap, rhs.ap
        return self.op("pe", lambda e: e.matmul(oa, lhsT=la, rhs=ra, start=start, stop=stop),
                       _k(lhsT, rhs) + ([] if start else _k(out)), _k(out))

    def tr(self, out, in_, ident):
        oa, ia, da = out.ap, in_.ap, ident.ap
        return self.op("pe", lambda e: e.transpose(oa, ia, da), _k(in_, ident), _k(out))

    def act(self, out, in_, func, bias=0.0, scale=1.0, accum=None):
        oa, ia, ba, sa = out.ap, in_.ap, _a(bias), _a(scale)
        ca = _a(accum) if accum is not None else None

        def fn(e):
            if ca is not None:
                return e.activation(out=oa, in_=ia, func=func, bias=ba, scale=sa, accum_out=ca)
            return e.activation(out=oa, in_=ia, func=func, bias=ba, scale=sa)
        return self.op("act", fn, _k(in_, bias, scale), _k(out) + (_k(accum) if accum is not None else []))

    def tt(self, eng, out, a, b, op):
        oa, aa, ba = out.ap, a.ap, b.ap
        return self.op(eng, lambda e: e.tensor_tensor(out=oa, in0=aa, in1=ba, op=op), _k(a, b), _k(out))

    def ts(self, eng, out, a, s1, op0, s2=None, op1=None):
        oa, aa, s1a, s2a = out.ap, a.ap, _a(s1), _a(s2)

        def fn(e):
            if op1 is None:
                return e.tensor_scalar(out=oa, in0=aa, scalar1=s1a, scalar2=None, op0=op0)
            return e.tensor_scalar(out=oa, in0=aa, scalar1=s1a, scalar2=s2a, op0=op0, op1=op1)
        return self.op(eng, fn, _k(a, s1, s2), _k(out))

    def stt(self, eng, out, a, scalar, b, op0, op1):
        oa, aa, sa, ba = out.ap, a.ap, _a(scalar), b.ap
        eng = "dve"
        return self.op(eng, lambda e: e.scalar_tensor_tensor(out=oa, in0=aa, scalar=sa, in1=ba, op0=op0, op1=op1),
                       _k(a, scalar, b), _k(out))

    def cp(self, eng, out, in_):
        oa, ia = out.ap, in_.ap
        if eng == "act":
            return self.op(eng, lambda e: e.copy(oa, ia), _k(in_), _k(out))
        return self.op(eng, lambda e: e.tensor_copy(oa, ia), _k(in_), _k(out))

    def memset(self, eng, out, val):
        oa = out.ap
        return self.op(eng, lambda e: e.memset(oa, val), (), _k(out))

    def scan(self, out, d0, d1, init, op0, op1):
        oa, a0, a1 = out.ap, d0.ap, d1.ap
        return self.op("dve", lambda e: e.tensor_tensor_scan(out=oa, data0=a0, data1=a1, initial=init, op0=op0, op1=op1),
                       _k(d0, d1), _k(out))

    def finish(self):
        nc = self.nc
        last_w = {}
        readers = {}
        pend = {e: [] for e in ENGS}
        seg = []
        excl = self.excl

        def _bank(k):
            b = k
            while isinstance(b, tuple):
                b = b[0]
            return b if b in excl else None
        for o in self.ops:
            if o.bar:
                lastc = {}
                dm = []
                for p in seg:
                    if p.dma:
                        dm.append(p)
                    else:
                        lastc[p.eng] = p
                newl = [p for p in list(lastc.values()) + dm if p.fn is not None]
                for e in ENGS:
                    pend[e].extend(newl)
                seg = []
                last_w = {}
                readers = {}
                continue
            rr, ww = [], []
            for k in o.reads:
                b = _bank(k)
                if b is None:
                    rr.append(k)
                else:
                    ww.append(b)
            for k in o.writes:
                b = _bank(k)
                ww.append(k if b is None else b)
            o.reads, o.writes = tuple(rr), tuple(dict.fromkeys(ww))
            seg.append(o)
            deps = set()
            for k in o.reads:
                w = last_w.get(k)
                if w is not None:
                    deps.add(w)
            for k in o.writes:
                w = last_w.get(k)
                if w is not None:
                    deps.add(w)
                for r in readers.get(k, ()):
                    deps.add(r)
            deps.discard(o)
            if o.eng == "pe":
                deps = {d for d in deps if not (d.eng == "pe" and not d.dma)}
            if pend[o.eng]:
                deps.update(pend[o.eng])
                pend[o.eng] = []
            deps.discard(o)
            o.deps = list(deps)
            for d in deps:
                d.signal = True
            for k in o.reads:
                readers.setdefault(k, []).append(o)
            for k in o.writes:
                last_w[k] = o
                readers[k] = []
        self.ops = [o for o in self.ops if not o.bar]
        streams = {e: [] for e in ENGS}
        for o in self.ops:
            streams[o.eng].append(o)
        sems = {e: self.es.enter_context(nc.semaphore("s_" + e)) for e in ENGS}
        dsems = {e: [self.es.enter_context(nc.semaphore("d_%s%d" % (e, i))) for i in range(NDMASEM)] for e in ENGS
                 if any(o.dma for o in streams[e])}
        for e in ENGS:
            c = 0
            nd = 0
            for o in streams[e]:
                if o.dma:
                    o.sem = dsems[e][nd % NDMASEM]
                    o.sigval = 16 * (nd // NDMASEM + 1)
                    if nd >= NDMASEM:
                        o.prewait = (o.sem, 16 * (nd // NDMASEM))
                    nd += 1
                    o.signal = True
                elif o.signal:
                    c += 1
                    o.sem = sems[e]
                    o.sigval = c
        self.stats = {e: len(streams[e]) for e in ENGS}
        nwaits = [0]

        def emit_stream(eng, e):
            known = {}
            for o in streams[e]:
                if o.prewait is not None:
                    s, v = o.prewait
                    if known.get(id(s), 0) < v:
                        eng.wait_ge(s, v)
                        known[id(s)] = v
                        nwaits[0] += 1
                need = {}
                for d in o.deps:
                    s = d.sem
                    if s is None:
                        continue
                    if need.get(id(s), (None, 0))[1] < d.sigval:
                        need[id(s)] = (s, d.sigval)
                for sid, (s, v) in need.items():
                    if known.get(sid, 0) < v:
                        eng.wait_ge(s, v)
                        known[sid] = v
                        nwaits[0] += 1
                ins = o.fn(eng)
                if o.signal and ins is not None:
                    ins.then_inc(o.sem, 16 if o.dma else 1)

        block = self.es.enter_context(nc.Block())

        @block.tensor
        def _(eng):
            emit_stream(eng, "pe")

        @block.scalar
        def _(eng):
            emit_stream(eng, "act")

        @block.vector
        def _(eng):
            emit_stream(eng, "dve")

        @block.gpsimd
        def _(eng):
            emit_stream(eng, "pool")

        @block.sync
        def _(eng):
            emit_stream(eng, "sp")

        self.stats["waits"] = nwaits[0]
        self.es.close()


class Arena:
    def __init__(self, P, nbytes):
        self.P = P
        self.nbytes = nbytes
        self.t16 = P.sb("arena", [128, nbytes // 2], BF16)
        self.t32 = self.t16.bitcast(F32)
        self.off = 0
        self.gen = 0
        self.peak = 0
        self.top_live = False
        self.reserved_top = 0

    def reset(self):
        self.P.barrier()
        self.off = 0
        self.gen += 1

    def mark(self):
        return self.off

    def release(self, m):
        self.P.barrier()
        self.off = m
        self.gen += 1

    def top_view(self, name, free_shape, nbytes):
        e0 = (self.nbytes - nbytes) // 4
        n = nbytes // 4
        ap = self.t32[:, e0:e0 + n].rearrange("p (a b) -> p a b", a=free_shape[0])
        return V(ap, name)

    def alloc(self, name, free_shape, dt=F32):
        n = 1
        for s in free_shape:
            n *= s
        esz = 4 if dt == F32 else 2
        nb = (n * esz + 63) // 64 * 64
        lim = self.nbytes - (self.reserved_top if self.top_live else 0)
        assert self.off + nb <= lim, "arena overflow %s: %d + %d > %d" % (name, self.off, nb, lim)
        base = self.t32 if dt == F32 else self.t16
        e0 = self.off // esz
        ap = base[:, e0:e0 + n]
        self.off += nb
        self.peak = max(self.peak, self.off)
        if len(free_shape) == 2:
            ap = ap.rearrange("p (a b) -> p a b", a=free_shape[0])
        elif len(free_shape) == 3:
            ap = ap.rearrange("p (a b c) -> p a b c", a=free_shape[0], b=free_shape[1])
        return V(ap, "%s#%d" % (name, self.gen))


POOL_WINDOWS = (2, 4, 8, 16)
NFM = 27
(FM_PIN, FM_AQ, FM_AKP, FM_AK, FM_AR, FM_S3, FM_S1, FM_S2, FM_DQ, FM_DK, FM_DV, FM_DG, FM_NQ, FM_NKC, FM_NVC, FM_NKS,
 FM_NKW) = (0, 2, 3, 7, 8, 10, 11, 12, 13, 15, 17, 19, 21, 23, 24, 25, 26)
C_GMIX, C_GCROSS, C_GFFN, C_GMEM, C_BGATE, C_POOLSC, C_POOLINVW, C_GLA_B, C_GLA_GN = 0, 8, 16, 24, 32, 64, 66, 68, 69
C_GDN_CONV, C_GDN_ALOG, C_GDN_DTB, C_GDN_GN, C_FFN_CONV, C_FFN_B, NCST = 70, 94, 95, 96, 100, 166, 192
_off = np.cumsum([0, 256, 128, 128, 256, 256, 16, 256, 256, 256, 4, 4, 256, 256, 128, 128, 128, 128, 128, 128, 12])
(O_PIN, O_AQ, O_AK, O_AV, O_AR, O_ALR, O_DQ, O_DK, O_DV, O_DB, O_DA, O_DG, O_NQ, O_NKC, O_NVC, O_NKS, O_NVS, O_NKW,
 O_NVW, O_NG) = [int(v) for v in _off[:-1]]


def _fm(W):
    K, n = W.shape
    return np.ascontiguousarray(W.reshape(K // 128, 128, n // 128, 128).transpose(2, 1, 0, 3))


def _col(v):
    return np.ascontiguousarray(v.reshape(-1, 128).T)


def host_prep(inp):
    f32 = np.float32
    w = {}
    w_in = inp["w_in"]
    fm = np.zeros((L, NFM, 128, KC, 128), f32)
    tm = np.zeros((L, 128, KC, 512), f32)
    cst = np.zeros((128, L, NCST), f32)
    poolbd = np.zeros((L, 2, 128, 128), f32)
    for l in range(L):
        W = w_in[l]

        def cols(o, n):
            return W[:, o:o + n]

        def put(idx, Wc):
            n = Wc.shape[1]
            pad = np.zeros((D, 128), f32)
            pad[:, :n] = Wc
            fm[l, idx] = _fm(pad)[0]
        put(FM_PIN, cols(O_PIN, 128)); put(FM_PIN + 1, cols(O_PIN + 128, 128))
        put(FM_AQ, cols(O_AQ, 128))
        for h in range(4):
            pad = np.zeros((D, 128), f32)
            pad[:, 32 * h:32 * h + 32] = cols(O_AK + 32 * h, 32)
            put(FM_AKP + h, pad)
        put(FM_AK, cols(O_AK, 128))
        put(FM_AR, cols(O_AR, 128)); put(FM_AR + 1, cols(O_AR + 128, 128))
        s3 = np.zeros((D, 128), f32); s3[:, 0:12] = cols(O_NG, 12); s3[:, 32:48] = cols(O_ALR, 16); put(FM_S3, s3)
        s1 = np.zeros((D, 128), f32)
        s2 = np.zeros((D, 128), f32)
        for r in (0, 32, 64):
            s1[:, r:r + 4] = cols(O_DA, 4)
            s2[:, r:r + 4] = cols(O_DB, 4)
        put(FM_S1, s1); put(FM_S2, s2)
        for j in range(2):
            put(FM_DQ + j, cols(O_DQ + 128 * j, 128)); put(FM_DK + j, cols(O_DK + 128 * j, 128))
            put(FM_DV + j, cols(O_DV + 128 * j, 128)); put(FM_DG + j, cols(O_DG + 128 * j, 128))
            put(FM_NQ + j, cols(O_NQ + 128 * j, 128))
        put(FM_NKC, cols(O_NKC, 128)); put(FM_NVC, cols(O_NVC, 128)); put(FM_NKS, cols(O_NKS, 128)); put(FM_NKW, cols(O_NKW, 128))
        tmw = np.concatenate([cols(O_AV, 256), cols(O_NVS, 128), cols(O_NVW, 128)], axis=1)
        tm[l] = tmw.reshape(KC, 128, 512).transpose(1, 0, 2)
        cst[:, l, C_GMIX:C_GMIX + 8] = _col(inp["g_mix"][l])
        cst[:, l, C_GCROSS:C_GCROSS + 8] = _col(inp["g_cross"][l])
        cst[:, l, C_GFFN:C_GFFN + 8] = _col(inp["g_ffn"][l])
        cst[:, l, C_GMEM:C_GMEM + 8] = _col(inp["g_mem"][l])
        for i in range(4):
            cst[:, l, C_BGATE + 8 * i:C_BGATE + 8 * i + 8] = _col(inp["b_gate"][l, i])
        cst[:, l, C_POOLSC:C_POOLSC + 2] = _col(inp["pool_scale"][l])
        cst[:, l, C_GLA_B] = inp["gla_b_lr"][l]
        cst[:, l, C_GLA_GN] = np.tile(inp["gla_g_norm"][l], 2)
        gc = inp["gdn_conv"][l]
        for which in range(3):
            for pr in range(2):
                for tap in range(4):
                    cst[:, l, C_GDN_CONV + (which * 2 + pr) * 4 + tap] = gc[tap, which * 256 + pr * 128: which * 256 + pr * 128 + 128]
        for r in (0, 32, 64):
            cst[r:r + 4, l, C_GDN_ALOG] = inp["gdn_a_log"][l]
            cst[r:r + 4, l, C_GDN_DTB] = inp["gdn_dt_bias"][l]
        cst[:, l, C_GDN_GN] = np.tile(inp["gdn_g_norm"][l], 2)
        fcv = inp["ffn_conv"][l]
        for tap in range(3):
            cst[:, l, C_FFN_CONV + tap:C_FFN_CONV + 66:3] = _col(fcv[tap])
        cst[:, l, C_FFN_B:C_FFN_B + 22] = _col(inp["ffn_conv_b"][l])
        for c in range(2):
            for gg in range(2):
                poolbd[l, c, 64 * gg:64 * gg + 64, 64 * gg:64 * gg + 64] = inp["pool_w"][l, 2 * c + gg]
    for c in range(2):
        for gg in range(2):
            cst[64 * gg:64 * gg + 64, :, C_POOLINVW + c] = 1.0 / POOL_WINDOWS[2 * c + gg]
    w["w_in_fm"] = fm
    wg = np.zeros((L, 4, 8, 128, KC, 128), f32); wbr = np.zeros((L, 4, 8, 128, 2, 128), f32)
    wo = np.zeros((L, 8, 128, KC, 128), f32); wxq = np.zeros((L, 4, 128, KC, 128), f32)
    wkk = np.zeros((L, 4, 128, KC, 128), f32); wvt = np.zeros((L, 128, KC, 512), f32)
    wxo = np.zeros((L, 8, 128, 4, 128), f32); wup = np.zeros((L, NF, 128, KC, 256), f32)
    for l in range(L):
        for i in range(4):
            wg[l, i] = _fm(inp["w_gate"][l, i])
            wbr[l, i] = _fm(inp["w_branch"][l, i])
        wo[l] = _fm(inp["w_out"][l])
        wxq[l] = _fm(inp["w_xq"][l])
        wkk[l] = _fm(inp["w_mem_kv"][l][:, 0:512])
        wvt[l] = inp["w_mem_kv"][l][:, 512:1024].reshape(KC, 128, 512).transpose(1, 0, 2)
        wxo[l] = _fm(inp["w_xo"][l])
        wu = _fm(inp["w_up"][l][:, 0:DFF]); wv_ = _fm(inp["w_up"][l][:, DFF:2 * DFF])
        wup[l, :, :, :, 0:128] = wu
        wup[l, :, :, :, 128:256] = wv_
    w["w_gate_fm"] = wg; w["w_branch_fm"] = wbr; w["w_out_fm"] = wo; w["w_xq_fm"] = wxq; w["w_kk_fm"] = wkk
    w["w_v_tm"] = wvt; w["w_xo_fm"] = wxo; w["w_up_fm"] = wup
    w["w_down_r"] = np.ascontiguousarray(inp["w_down"].reshape(L, NF, 128, D))
    w["w_in_tm"] = tm
    w["cst"] = cst
    w["pool_bd"] = poolbd
    pool16 = np.zeros((128, 2, 16), f32)
    for c in range(2):
        for gg in range(2):
            wdw = POOL_WINDOWS[2 * c + gg]
            pool16[64 * gg:64 * gg + 64, c, :] = 1.0 / np.minimum(np.arange(16) + 1, wdw)
    w["pool16"] = pool16
    w["ident"] = np.eye(128, dtype=f32)
    w["ones"] = np.ones((128, 128), f32)
    bd64 = np.zeros((128, 128), f32); bd64[:64, :64] = 1; bd64[64:, 64:] = 1
    w["bd64"] = bd64
    w["gfin"] = _col(inp["g_final"])
    wlr = np.zeros((L, 128, 128), f32)
    for l in range(L):
        wlr[l, 32:48, :] = inp["gla_w_lr"][l]
    w["wlr"] = wlr
    rmask = np.ones((128, T), f32); rmask[:, ::128] = 0.0
    w["rmask"] = rmask
    pp = np.arange(128)[:, None]; ff = np.arange(128)[None, :]
    w["tri_ui"] = (pp <= ff).astype(f32)
    w["tri_sl"] = (pp > ff).astype(f32)
    w["bdmask"] = ((np.arange(128)[:, None] // 32) == (np.arange(256)[None, :] // 64)).astype(f32)
    w1bd = np.zeros((L, 2, 128, 32, 128), f32)
    peT = np.zeros((L, 2, 128, 32, 8), f32)
    w2dup = np.zeros((L, 2, 128, 128), f32)
    w2vbd = np.zeros((L, 128, 128), f32)
    for l in range(L):
        for kv in range(2):
            w1 = inp["nsa_cmp_w1"][l, kv].reshape(32, 64, 64)
            for g in range(2):
                w1bd[l, kv, 64 * g:64 * g + 64, :, 64 * g:64 * g + 64] = w1.transpose(1, 0, 2)
                peT[l, kv, 64 * g:64 * g + 64, :, :] = inp["nsa_pe"][l, kv].T[:, :, None]
        w2k = inp["nsa_cmp_w2"][l, 0]
        w2v = inp["nsa_cmp_w2"][l, 1]
        for g in range(2):
            w2dup[l, g, 64 * g:64 * g + 64, 0:64] = w2k
            w2dup[l, g, 64 * g:64 * g + 64, 64:128] = w2k
            w2vbd[l, 64 * g:64 * g + 64, 64 * g:64 * g + 64] = w2v
    w["w1bd"] = w1bd; w["peT"] = peT; w["w2dup"] = w2dup; w["w2vbd"] = w2vbd
    nsadup = np.zeros((L, 4, 128, KC, 128), f32)
    for l in range(L):
        for i, o in enumerate((O_NKS, O_NKS + 64, O_NKW, O_NKW + 64)):
            Wc = np.concatenate([inp["w_in"][l][:, o:o + 64]] * 2, axis=1)
            nsadup[l, i] = _fm(Wc)[0]
    w["nsadup"] = nsadup
    n_i = np.arange(128)[:, None]; q_i = np.arange(T)[None, :]
    w["cmask"] = ((16 * n_i + 31 <= q_i) & (n_i < 127)).astype(f32)
    cover1 = np.zeros((128, 33), f32)
    for n in range(127):
        for m_ in range(32):
            if 16 * n <= 64 * m_ + 63 and 16 * n + 31 >= 64 * m_:
                cover1[n, m_] = 1.0
        cover1[n, 32] = 1.0
    w["cover1"] = cover1
    keepT = np.zeros((128, NT, 32), f32); addT = np.zeros((128, NT, 32), f32); validT = np.zeros((128, NT, 32), f32)
    for qt in range(NT):
        for p in range(128):
            cur = (128 * qt + p) // 64
            for m_ in range(32):
                if m_ > cur:
                    addT[p, qt, m_] = -1.0
                elif m_ == cur:
                    addT[p, qt, m_] = 1e4; validT[p, qt, m_] = 1
                elif m_ == cur - 1:
                    addT[p, qt, m_] = 2e4; validT[p, qt, m_] = 1
                elif m_ == 0:
                    addT[p, qt, m_] = 3e4; validT[p, qt, m_] = 1
                else:
                    keepT[p, qt, m_] = 1; validT[p, qt, m_] = 1
    w["keepT"] = keepT; w["addT"] = addT; w["validT"] = validT
    w["Esel"] = (np.arange(128)[:, None] == (np.arange(T)[None, :] // 64)).astype(f32)
    cc = np.arange(1408)[None, :]; pq = np.arange(128)[:, None]
    w["Wlong"] = ((pq <= cc - 384) & (pq > cc - 896)).astype(f32)
    selg = np.zeros((128, 2, 3, 128), f32)
    for g in range(2):
        for b in range(3):
            for m_ in range(128):
                selg[(g * 2 + m_ // 64) * 3 + b, g, b, m_] = 1.0
    w["selg"] = selg
    selA = np.zeros((128, 4, 128), f32)
    for h in range(4):
        selA[h, h, :] = 1.0
    w["selA"] = selA
    return w


class Ctx:
    pass


def build(nseq=4, nlayers=L, stages=("A", "pool", "gla", "gdn", "nsa", "G", "X", "F"), dbg=(), nsa_level=9):
    nc = bass.Bass("TRN2", target_bir_lowering=False)
    P = Prog(nc)
    c = Ctx()
    c.nsa_level = nsa_level
    c.nc, c.P, c.stages, c.dbg = nc, P, stages, dbg

    def din(name, shape):
        return V(nc.dram_tensor(name, list(shape), F32, kind="ExternalInput").ap(), "dram_" + name)

    c.xT_d = din("xT", [nseq, 128, KC, T])
    c.memT_d = din("memT", [nseq, 128, KC, 256])
    c.w_in_fm = din("w_in_fm", [L, NFM, 128, KC, 128])
    c.w_in_tm = din("w_in_tm", [L, 128, KC, 512])
    c.cst_d = din("cst", [128, L, NCST])
    c.pool_bd_d = din("pool_bd", [L, 2, 128, 128])
    c.pool16_d = din("pool16", [128, 2, 16])
    c.ident_d = din("ident", [128, 128])
    c.ones_d = din("ones", [128, 128])
    c.bd64_d = din("bd64", [128, 128])
    c.gfin_d = din("gfin", [128, KC])
    c.wlr_d = din("wlr", [L, 128, 128])
    c.rmask_d = din("rmask", [128, T])
    c.tri_ui_d = din("tri_ui", [128, 128])
    c.tri_sl_d = din("tri_sl", [128, 128])
    c.bdmask_d = din("bdmask", [128, 256])
    c.selA_d = din("selA", [128, 4, 128])
    c.w_gate_d = din("w_gate_fm", [L, 4, 8, 128, KC, 128])
    c.w_branch_d = din("w_branch_fm", [L, 4, 8, 128, 2, 128])
    c.w_out_d = din("w_out_fm", [L, 8, 128, KC, 128])
    c.w_xq_d = din("w_xq_fm", [L, 4, 128, KC, 128])
    c.w_kk_d = din("w_kk_fm", [L, 4, 128, KC, 128])
    c.w_v_d = din("w_v_tm", [L, 128, KC, 512])
    c.w_xo_d = din("w_xo_fm", [L, 8, 128, 4, 128])
    c.w_up_d = din("w_up_fm", [L, NF, 128, KC, 256])
    c.w_down_d = din("w_down_r", [L, NF, 128, D])
    c.w1bd_d = din("w1bd", [L, 2, 128, 32, 128])
    c.peT_d = din("peT", [L, 2, 128, 32, 8])
    c.w2dup_d = din("w2dup", [L, 2, 128, 128])
    c.w2vbd_d = din("w2vbd", [L, 128, 128])
    c.nsadup_d = din("nsadup", [L, 4, 128, KC, 128])
    c.cmask_d = din("cmask", [128, T])
    c.cover1_d = din("cover1", [128, 33])
    c.keepT_d = din("keepT", [128, NT, 32])
    c.addT_d = din("addT", [128, NT, 32])
    c.validT_d = din("validT", [128, NT, 32])
    c.Esel_d = din("Esel", [128, T])
    c.Wlong_d = din("Wlong", [128, 1408])
    c.selg_d = din("selg", [128, 2, 3, 128])
    c.out_d = V(nc.dram_tensor("outT", [nseq, 128, KC, T], F32, kind="ExternalOutput").ap(), "dram_out")
    c.xs_d = V(nc.dram_tensor("xs", [128, KC, T], F32, kind="Internal").ap(), "dram_xs")
    c.dbg_d = {}
    for name, shape in dbg:
        c.dbg_d[name] = V(nc.dram_tensor("dbg_" + name, list(shape), F32, kind="ExternalOutput").ap(), "dram_dbg_" + name)

    c.cst = V(P.sb("cst", [128, L, NCST])[:], "cst")
    c.ident = V(P.sb("ident", [128, 128])[:], "ident")
    c.ones = V(P.sb("ones", [128, 128])[:], "ones")
    c.bd64 = V(P.sb("bd64", [128, 128])[:], "bd64")
    c.gfin = V(P.sb("gfin", [128, KC])[:], "gfin")
    c.pool16 = V(P.sb("pool16", [128, 2, 16])[:], "pool16")
    c.hT = V(P.sb("hT", [128, KC, T], BF16)[:], "hT")
    c.tri_ui = V(P.sb("tri_ui", [128, 128])[:], "tri_ui")
    c.tri_sl = V(P.sb("tri_sl", [128, 128])[:], "tri_sl")
    c.bdmask = V(P.sb("bdmask", [128, 256])[:], "bdmask")
    c.onec = V(P.sb("onec", [128, 1])[:], "onec")
    c.onesbf = V(P.sb("onesbf", [128, 128], BF16)[:], "onesbf")
    c.bd64bf = V(P.sb("bd64bf", [128, 128], BF16)[:], "bd64bf")
    P.dma("pool", c.bd64bf, c.bd64_d)
    P.memset("pool", c.onesbf, 1.0)
    c.selA = V(P.sb("selA", [128, 4, 128])[:], "selA")
    P.dma("sp", c.selA, c.selA_d)
    P.memset("pool", c.onec, 1.0)
    for dst, src in ((c.tri_ui, c.tri_ui_d), (c.tri_sl, c.tri_sl_d), (c.bdmask, c.bdmask_d)):
        P.dma("sp", dst, src)
    c.epsc = V(P.sb("epsc", [128, 1])[:], "epsc")
    P.memset("pool", c.epsc, EPS)
    c.ar = Arena(P, 156 * 1024)
    c.psb = [V(P.ps("ps%d" % i, [128, 512])[:], "ps%d" % i) for i in range(8)]
    c.ar.reserved_top = KC * T * 4
    c.xT = c.ar.top_view("xT", [KC, T], KC * T * 4)
    P.excl = set("ps%d" % i for i in range(8))
    for dst, src in ((c.cst, c.cst_d), (c.ident, c.ident_d), (c.ones, c.ones_d), (c.bd64, c.bd64_d), (c.gfin, c.gfin_d),
                     (c.pool16, c.pool16_d)):
        P.dma("sp", dst, src)
    c.wq = 0

    for s in range(nseq):
        for l in range(nlayers):
            seq_layer(c, s, l, first=(l == 0), last=(l == nlayers - 1))
    P.barrier()
    P.op("sp", lambda e: None)
    P.finish()
    c.stats = P.stats
    return nc, c


def wdma(c, dst, src):
    c.P.dma("pool", dst, src)


def blk(tb):
    return slice(tb * 512, (tb + 1) * 512)


class Slabs:
    def __init__(self, c, bufs, srcs):
        self.c, self.bufs, self.srcs = c, bufs, srcs
        self.issued = 0
        self.cur = 0

    def _issue(self):
        i = self.issued
        b = self.bufs[i % len(self.bufs)]
        src = self.srcs[i]
        if isinstance(src, (list, tuple)):
            for dst_sel, sv in src:
                wdma(self.c, dst_sel(b), sv)
        else:
            wdma(self.c, b, src)
        self.issued += 1

    def nxt(self):
        i = self.cur
        while self.issued <= min(i + 1, len(self.srcs) - 1):
            self._issue()
        self.cur += 1
        return self.bufs[i % len(self.bufs)]


def proj_fm(c, wsl, rhsT, tb, out_ps, M=128, kc=KC):
    for k in range(kc):
        c.P.mm(out_ps[0:M, :], wsl[:, k, 0:M], rhsT[:, k, blk(tb)], start=(k == 0), stop=(k == kc - 1))


def rsqrt(c, out, in_, scale=1.0, eps=EPS):
    c.P.act(out, in_, AF.Sqrt, bias=c.epsc[0:out.ap.shape[0], :], scale=scale)
    c.P.op("dve", (lambda oa: lambda e: e.reciprocal(oa, oa))(out.ap), _k(out), _k(out))


def phase_norm(c, xT, gcol, out):
    P, ar = c.P, c.ar
    sq = ar.alloc("sq", [4, 512], BF16)
    rstd = ar.alloc("rstd", [2, 512])
    for tb in range(NB):
        ps = c.psb[tb % 2]
        for k in range(KC):
            s = sq[:, k % 4, :].sub(k % 4)
            P.act(s, xT[:, k, blk(tb)].sub(k), AF.Square)
            P.mm(ps, c.onesbf, s, start=(k == 0), stop=(k == KC - 1))
        r = rstd[:, tb % 2, :].sub(tb % 2)
        rsqrt(c, r, ps, 1.0 / D)
        for k in range(KC):
            eng = "dve" if k % 2 == 0 else "pool"
            P.stt(eng, out[:, k, blk(tb)], xT[:, k, blk(tb)].sub(k), gcol[:, k:k + 1], r, ALU.mult, ALU.mult)


def seq_layer(c, s, l, first, last):
    P, ar = c.P, c.ar
    ar.reset()
    xT = c.xT
    ar.top_live = True
    if first:
        for k in range(KC):
            P.dma("sp", xT[:, k, :].sub(k), c.xT_d[s, :, k, :])
    phase_norm(c, xT, c.cst[:, l, C_GMIX:C_GMIX + 8], c.hT)
    if not first:
        for k in range(KC):
            P.dma("sp", c.xs_d[:, k, :].sub(k), xT[:, k, :].sub(k))
    if "hT" in c.dbg_d:
        P.dma("pool", c.dbg_d["hT"], c.hT)
    ar.reset()
    ar.top_live = False
    obr = ar.alloc("obr", [4, 2, T], BF16)
    if "pool" in c.stages:
        mixer_pool(c, l, obr)
    if "gla" in c.stages:
        mixer_gla(c, l, obr)
    if "gdn" in c.stages:
        mixer_gdn(c, l, obr)
    if "nsa" in c.stages:
        mixer_nsa(c, l, obr)
    if "obr" in c.dbg_d:
        P.dma("pool", c.dbg_d["obr"], obr)
    if "G" not in c.stages:
        return
    ar.top_live = True
    xsrc = c.xT_d[s] if first else c.xs_d
    for k in range(KC):
        P.dma("sp", xT[:, k, :].sub(k), xsrc[:, k, :].sub(k) if not first else xsrc[:, k, :])
    phase_gate(c, l, obr, xT)
    dump_x(c, "x1", xT)
    ar.reset()
    if "X" in c.stages:
        phase_cross(c, s, l, xT)
        dump_x(c, "x2", xT)
        ar.reset()
    if "F" in c.stages:
        phase_ffn(c, l, xT)
        dump_x(c, "x3", xT)
        ar.reset()
    if last:
        phase_final(c, s, xT)


def dump_x(c, name, xT):
    if name in c.dbg_d:
        for k in range(KC):
            c.P.dma("sp", c.dbg_d[name][:, k, :], xT[:, k, :].sub(k))


def add_resid(c, xT, m, tb, ps, i):
    xv = xT[:, m, blk(tb)].sub(m)
    c.P.tt("dve", xv, xv, ps, ALU.add)


def phase_gate(c, l, obr, xT):
    P, ar = c.P, c.ar
    yT = ar.alloc("yT", [KC, T], BF16)
    yacc = ar.alloc("yacc", [T])
    sg = ar.alloc("sg", [2, 512])
    tm = ar.alloc("tm", [2, 512])
    wsl = ar.alloc("wsl", [3, KC, 128], BF16)
    wbl = ar.alloc("wbl", [3, 2, 128], BF16)
    srcs = []
    for m in range(8):
        for i in range(4):
            srcs.append(((lambda b: b[0]), c.w_gate_d[l, i, m]))
    bufs = [(wsl[:, i].sub(i), wbl[:, i].sub(i)) for i in range(3)]
    SLg = Slabs(c, bufs, [[((lambda b: b[0]), c.w_gate_d[l, i, m]), ((lambda b: b[1]), c.w_branch_d[l, i, m])]
                          for m in range(8) for i in range(4)])
    for m in range(8):
        for i in range(4):
            wg, wb = SLg.nxt()
            for tb in range(NB):
                psg = c.psb[(2 * tb) % 4]
                psr = c.psb[(2 * tb) % 4 + 1]
                proj_fm(c, wg, c.hT, tb, psg)
                for k2 in range(2):
                    P.mm(psr, wb[:, k2, :], obr[:, i, k2, blk(tb)], start=(k2 == 0), stop=(k2 == 1))
                sgv = sg[:, tb % 2, :].sub(tb % 2)
                P.act(sgv, psg, AF.Sigmoid, bias=c.cst[:, l, C_BGATE + 8 * i + m:C_BGATE + 8 * i + m + 1], scale=1.0)
                if i == 0:
                    P.tt("dve", yacc[:, blk(tb)].sub(tb), sgv, psr, ALU.mult)
                else:
                    tv = tm[:, tb % 2, :].sub(tb % 2)
                    P.tt("dve", tv, sgv, psr, ALU.mult)
                    P.tt("dve", yacc[:, blk(tb)].sub(tb), yacc[:, blk(tb)].sub(tb), tv, ALU.add)
        for tb in range(NB):
            P.cp("act", yT[:, m, blk(tb)].sub(m), yacc[:, blk(tb)].sub(tb))
    SLo = Slabs(c, [wsl[:, i].sub(i) for i in range(3)], [c.w_out_d[l, m] for m in range(8)])
    for m in range(8):
        wo = SLo.nxt()
        for tb in range(NB):
            ps = c.psb[4 + tb % 4]
            for k in range(KC):
                P.mm(ps, wo[:, k, :], yT[:, k, blk(tb)].sub(k), start=(k == 0), stop=(k == KC - 1))
            add_resid(c, xT, m, tb, ps, tb)


def phase_cross(c, s, l, xT):
    P, ar = c.P, c.ar
    phase_norm(c, xT, c.cst[:, l, C_GCROSS:C_GCROSS + 8], c.hT)
    memT = ar.alloc("memT", [KC, 256])
    memn = ar.alloc("memn", [KC, 256], BF16)
    sq = ar.alloc("msq", [2, 256], BF16)
    rstd = ar.alloc("mrstd", [256])
    KT = ar.alloc("KT", [4, 256], BF16)
    Vtok = ar.alloc("Vtok", [2, 512], BF16)
    wv = ar.alloc("wv", [KC, 512], BF16)
    qh = ar.alloc("qh", [2, 512], BF16)
    ee = ar.alloc("xe", [2, 2, 512], BF16)
    oX = ar.alloc("oX", [4, T], BF16)
    rr = ar.alloc("xr", [2, 512])
    wsl = ar.alloc("wsl", [3, KC, 128], BF16)
    wxo = ar.alloc("wxo", [3, 4, 128], BF16)
    SL = Slabs(c, [wsl[:, i].sub(i) for i in range(3)], [c.w_kk_d[l, hh] for hh in range(4)] + [c.w_xq_d[l, hh] for hh in range(4)])
    SLo = Slabs(c, [wxo[:, i].sub(i) for i in range(3)], [c.w_xo_d[l, m] for m in range(8)])
    wdma(c, wv, c.w_v_d[l])
    P.dma("sp", memT, c.memT_d[s])
    ps = c.psb[0]
    for k in range(KC):
        sv = sq[:, k % 2, :].sub(k % 2)
        P.act(sv, memT[:, k, :], AF.Square)
        P.mm(ps[:, 0:256], c.onesbf, sv, start=(k == 0), stop=(k == KC - 1))
    rsqrt(c, rstd, ps[:, 0:256], 1.0 / D)
    for k in range(KC):
        P.stt("dve", memn[:, k, :], memT[:, k, :], c.cst[:, l, C_GMEM + k:C_GMEM + k + 1], rstd, ALU.mult, ALU.mult)
    n = 0
    for hh in range(4):
        w = SL.nxt()
        ps = c.psb[1 + hh % 2]
        for k in range(KC):
            P.mm(ps[:, 0:256], w[:, k, :], memn[:, k, :], start=(k == 0), stop=(k == KC - 1))
        P.cp("act", KT[:, hh, :], ps[:, 0:256])
    for mt in range(2):
        ps = c.psb[3 + mt]
        for k in range(KC):
            P.mm(ps, memn[:, k, mt * 128:(mt + 1) * 128], wv[:, k, :], start=(k == 0), stop=(k == KC - 1))
        P.cp("act", Vtok[:, mt, :], ps)
    iters = [(hh, tb) for hh in range(4) for tb in range(NB)]
    wq = {}

    def xs1(i):
        hh, tb = iters[i]
        r2 = i % 2
        if tb == 0:
            wq[hh] = SL.nxt()
        psq = c.psb[r2]
        proj_fm(c, wq[hh], c.hT, tb, psq)
        qv = qh[:, r2, :].sub(r2)
        P.ts("dve", qv, psq, 128.0 ** -0.5, ALU.mult)
        for mt in range(2):
            psS = c.psb[2 + 2 * r2 + mt]
            P.mm(psS, KT[:, hh, mt * 128:(mt + 1) * 128], qv)
            ev = ee[:, r2, mt, :].sub((r2, mt))
            P.act(ev, psS, AF.Exp)

    def xs2(i):
        hh, tb = iters[i]
        r2 = i % 2
        psO, psD = c.psb[6], c.psb[7]
        for mt in range(2):
            ev = ee[:, r2, mt, :].sub((r2, mt))
            P.mm(psO, Vtok[:, mt, hh * 128:(hh + 1) * 128], ev, start=(mt == 0), stop=(mt == 1))
            P.mm(psD, c.onesbf, ev, start=(mt == 0), stop=(mt == 1))
        rv = rr[:, r2, :].sub(r2)
        P.op("dve", (lambda oa, ia: lambda e: e.reciprocal(oa, ia))(rv.ap, psD.ap), _k(psD), _k(rv))
        P.tt("dve", oX[:, hh, blk(tb)].sub(hh), psO, rv, ALU.mult)
    for i in range(len(iters) + 1):
        if i < len(iters):
            xs1(i)
        if i >= 1:
            xs2(i - 1)
    for m in range(8):
        w = SLo.nxt()
        for tb in range(NB):
            ps = c.psb[tb % 4]
            for k in range(4):
                P.mm(ps, w[:, k, :], oX[:, k, blk(tb)].sub(k), start=(k == 0), stop=(k == 3))
            add_resid(c, xT, m, tb, ps, tb)


FGROUPS = ((0, 6), (6, 12), (12, 17), (17, 22))


def phase_ffn(c, l, xT):
    P, ar = c.P, c.ar
    phase_norm(c, xT, c.cst[:, l, C_GFFN:C_GFFN + 8], c.hT)
    aT = ar.alloc("aT", [6, T], BF16)
    upad = ar.alloc("upad", [2, 2 + T])
    c1 = ar.alloc("c1", [2, 512])
    gb = ar.alloc("gb", [2, 512], BF16)
    wu = ar.alloc("wu", [3, KC, 256], BF16)
    wd = ar.alloc("wd", [2, 6, D], BF16)
    SLu = Slabs(c, [wu[:, i].sub(i) for i in range(3)], [c.w_up_d[l, f] for f in range(NF)])
    P.memset("pool", upad[:, 0, 0:2].sub(0), 0.0)
    P.memset("pool", upad[:, 1, 0:2].sub(1), 0.0)
    cs = c.cst
    n = 0
    for gi, (f0, f1) in enumerate(FGROUPS):
        wdg = wd[:, gi % 2].sub(gi % 2)
        for fi, f in enumerate(range(f0, f1)):
            P.dma("pool", wdg[:, fi, :], c.w_down_d[l, f])
        for fi, f in enumerate(range(f0, f1)):
            w = SLu.nxt()
            up = upad[:, n % 2].sub(n % 2)
            n += 1
            cw = C_FFN_CONV + 3 * f
            for tb in range(NB):
                psu, psv = c.psb[(2 * tb) % 4], c.psb[(2 * tb) % 4 + 1]
                for k in range(KC):
                    P.mm(psu, w[:, k, 0:128], c.hT[:, k, blk(tb)], start=(k == 0), stop=(k == KC - 1))
                for k in range(KC):
                    P.mm(psv, w[:, k, 128:256], c.hT[:, k, blk(tb)], start=(k == 0), stop=(k == KC - 1))
                b0 = tb * 512
                P.cp("act", up[:, 2 + b0:2 + b0 + 512], psu)
                cv = c1[:, tb % 2, :].sub(tb % 2)
                P.ts("dve", cv, up[:, b0:b0 + 512], cs[:, l, cw:cw + 1], ALU.mult, cs[:, l, C_FFN_B + f:C_FFN_B + f + 1], ALU.add)
                P.stt("dve", cv, up[:, b0 + 1:b0 + 513], cs[:, l, cw + 1:cw + 2], cv, ALU.mult, ALU.add)
                P.stt("dve", cv, up[:, b0 + 2:b0 + 514], cs[:, l, cw + 2:cw + 3], cv, ALU.mult, ALU.add)
                gv = gb[:, tb % 2, :].sub(tb % 2)
                P.act(gv, cv, AF.Gelu_apprx_tanh)
                P.tt("dve", aT[:, fi, blk(tb)].sub(fi), gv, psv, ALU.mult)
        nf = f1 - f0
        for m in range(8):
            for tb in range(NB):
                ps = c.psb[4 + tb % 4]
                for fi in range(nf):
                    P.mm(ps, wdg[:, fi, m * 128:(m + 1) * 128], aT[:, fi, blk(tb)].sub(fi), start=(fi == 0), stop=(fi == nf - 1))
                add_resid(c, xT, m, tb, ps, tb)


def phase_final(c, s, xT):
    P, ar = c.P, c.ar
    sq = ar.alloc("fsq", [4, 512], BF16)
    rstd = ar.alloc("frstd", [2, 512])
    ob = ar.alloc("fob", [4, 512])
    n = 0
    for tb in range(NB):
        ps = c.psb[tb % 2]
        for k in range(KC):
            sv = sq[:, k % 4, :].sub(k % 4)
            P.act(sv, xT[:, k, blk(tb)].sub(k), AF.Square)
            P.mm(ps, c.onesbf, sv, start=(k == 0), stop=(k == KC - 1))
        r = rstd[:, tb % 2, :].sub(tb % 2)
        rsqrt(c, r, ps, 1.0 / D)
        for k in range(KC):
            o = ob[:, n % 4, :].sub(n % 4)
            n += 1
            P.stt("dve", o, xT[:, k, blk(tb)].sub(k), c.gfin[:, k:k + 1], r, ALU.mult, ALU.mult)
            P.dma("sp", c.out_d[s, :, k, blk(tb)], o)
    ar.reset()


def mixer_pool(c, l, obr):
    P, ar = c.P, c.ar
    m = ar.mark()
    W = 16 + T
    upad = ar.alloc("upad", [W])
    st = [ar.alloc("s%d" % i, [W]) for i in range(4)]
    dif = ar.alloc("dif", [T], BF16)
    d32 = ar.alloc("d32", [16])
    wsl = ar.alloc("wsl", [2, KC, 128], BF16)
    pw = ar.alloc("pw", [2, 128], BF16)
    SL = Slabs(c, [wsl[:, i].sub(i) for i in range(2)], [c.w_in_fm[l, FM_PIN], c.w_in_fm[l, FM_PIN + 1]])
    P.memset("pool", upad[:, 0:16], 0.0)
    for i in range(4):
        P.memset("pool", st[i][:, 0:16], 0.0)
    for ch in range(2):
        w = SL.nxt()
        wdma(c, pw[:, ch, :].sub(ch), c.pool_bd_d[l, ch])
        for tb in range(NB):
            ps = c.psb[tb % 2]
            proj_fm(c, w, c.hT, tb, ps)
            P.cp("act", upad[:, 16 + tb * 512:16 + (tb + 1) * 512], ps)
        src = upad
        nst = 2 if ch == 0 else 4
        for i in range(nst):
            sh = 1 << i
            P.tt("dve" if i % 2 == 0 else "pool", st[i][:, sh:W], src[:, sh:W], src[:, 0:W - sh], ALU.add)
            src = st[i]
        for gg in range(2):
            rows = slice(64 * gg, 64 * gg + 64)
            sw = st[(0 if ch == 0 else 2) + gg]
            P.tt("dve", d32[rows, :], sw[rows, 16:32], c.pool16[rows, ch, :], ALU.mult)
            P.stt("dve", dif[rows, :], sw[rows, 16:W], c.cst[rows, l, C_POOLINVW + ch:C_POOLINVW + ch + 1], upad[rows, 16:W],
                  ALU.mult, ALU.subtract)
            P.tt("dve", dif[rows, 0:16], d32[rows, :], upad[rows, 16:32], ALU.subtract)
        for tb in range(NB):
            ps = c.psb[2 + tb % 2]
            P.mm(ps, pw[:, ch, :].sub(ch), dif[:, blk(tb)])
            P.ts("dve", obr[:, 0, ch, blk(tb)], ps, c.cst[:, l, C_POOLSC + ch:C_POOLSC + ch + 1], ALU.mult)
    ar.release(m)


def tile_(t):
    return slice(t * 128, (t + 1) * 128)


def mixer_gla(c, l, obr):
    P, ar = c.P, c.ar
    m = ar.mark()
    rmask = ar.alloc("rmask", [T]); P.dma("sp", rmask, c.rmask_d)
    wlr = ar.alloc("wlr", [128]); P.dma("sp", wlr, c.wlr_d[l])
    tmp = ar.alloc("tmp", [T])
    tmp2 = ar.alloc("tmp2", [T])
    B = ar.alloc("B", [T])
    qe = ar.alloc("qe", [T], BF16)
    kpad = ar.alloc("kpad", [4, T], BF16)
    kupdT = ar.alloc("kupdT", [T])
    kupdtok = ar.alloc("kupdtok", [NT, 128], BF16)
    vtok = ar.alloc("vtok", [NT, 256], BF16)
    rs = ar.alloc("rs", [2, T], BF16)
    oT = ar.alloc("oT", [2, T])
    dec = ar.alloc("dec", [NT])
    negb = ar.alloc("negb", [1])
    wsl = ar.alloc("wsl", [3, KC, 128], BF16)
    wtm = ar.alloc("wtm", [KC, 256], BF16)
    S = ar.alloc("S", [256]); Sbf = ar.alloc("Sbf", [256], BF16); Stmp = ar.alloc("Stmp", [256])
    Sbf2 = ar.alloc("Sbf2", [256], BF16)
    attm = ar.alloc("attm", [2, 4, 128], BF16)
    sqb = ar.alloc("sqb", [2, 512], BF16)
    SL = Slabs(c, [wsl[:, i].sub(i) for i in range(3)],
               [c.w_in_fm[l, i] for i in (FM_S3, FM_AR, FM_AR + 1, FM_AQ, FM_AKP, FM_AKP + 1, FM_AKP + 2, FM_AKP + 3, FM_AK)])
    wdma(c, wtm, c.w_in_tm[l, :, :, 0:256])

    def slab(idx):
        return SL.nxt()
    psn = [0]

    def nps():
        p = c.psb[psn[0] % 2]
        psn[0] += 1
        return p
    w = slab(FM_S3)
    for tb in range(NB):
        ps = nps()
        proj_fm(c, w, c.hT, tb, ps)
        P.cp("act", tmp[:, blk(tb)], ps)
    P.ts("pool", negb, c.cst[:, l, C_GLA_B:C_GLA_B + 1], -1.0, ALU.mult)
    for t in range(NT):
        ps = nps()
        for k in range(KC):
            P.mm(ps[:, 0:256], c.hT[:, k, tile_(t)], wtm[:, k, :], start=(k == 0), stop=(k == KC - 1))
        P.cp("act", vtok[:, t, :], ps[:, 0:256])
    for ch in range(2):
        w = slab(FM_AR + ch)
        for tb in range(NB):
            ps = nps()
            proj_fm(c, w, c.hT, tb, ps)
            P.act(rs[:, ch, blk(tb)], ps, AF.Silu)
    for tb in range(NB):
        ps = nps()
        P.mm(ps, wlr[32:48, :], tmp[32:48, blk(tb)])
        P.act(tmp2[:, blk(tb)], ps, AF.Exp, bias=negb, scale=-1.0)
        P.act(tmp2[:, blk(tb)], tmp2[:, blk(tb)], AF.Ln, bias=c.onec, scale=1.0)
    P.ts("pool", tmp2, tmp2, -1.0 / 16.0, ALU.mult)
    P.scan(B, rmask, tmp2, 0.0, ALU.mult, ALU.add)
    P.act(tmp, B, AF.Exp)
    P.act(tmp2, B, AF.Exp, scale=-1.0)
    P.cp("pool", dec, tmp.re("p (a b) -> p a b", b=128)[:, :, 127])
    w = slab(FM_AQ)
    for tb in range(NB):
        ps = nps()
        proj_fm(c, w, c.hT, tb, ps)
        P.stt("dve", qe[:, blk(tb)], ps, 32.0 ** -0.5, tmp[:, blk(tb)], ALU.mult, ALU.mult)
    for h in range(4):
        w = slab(FM_AKP + h)
        for tb in range(NB):
            ps = nps()
            proj_fm(c, w, c.hT, tb, ps)
            P.tt("dve", kpad[:, h, blk(tb)], ps, tmp2[:, blk(tb)], ALU.mult)
    w = slab(FM_AK)
    for tb in range(NB):
        ps = nps()
        proj_fm(c, w, c.hT, tb, ps)
        for t4 in range(4):
            tl = tile_(tb * 4 + t4)
            last = tb * 512 + t4 * 128 + 127
            P.act(tmp[:, tl], B[:, tl], AF.Exp, bias=B[:, last:last + 1], scale=-1.0)
        P.tt("dve", kupdT[:, blk(tb)], ps, tmp[:, blk(tb)], ALU.mult)
    for t in range(NT):
        ps = nps()
        P.tr(ps[:, 0:128], kupdT[:, tile_(t)], c.ident)
        P.cp("act", kupdtok[:, t, :], ps[:, 0:128])
    P.memset("pool", S, 0.0)
    Sb2 = [Sbf, Sbf2]

    def att_(t):
        psA = c.psb[4 + t % 2]
        for h in range(4):
            P.mm(psA[:, h * 128:(h + 1) * 128], kpad[:, h, tile_(t)], qe[:, tile_(t)])
        am = attm[:, t % 2].sub(t % 2)
        P.tt("dve", am, psA.re("p (h f) -> p h f", h=4), c.tri_ui.re("p (o f) -> p o f", o=1).bc([128, 4, 128]), ALU.mult)
    att_(0)
    for t in range(NT):
        tl = tile_(t)
        if t + 1 < NT:
            att_(t + 1)
        if t < NT - 1:
            psS = c.psb[2 + t % 2]
            P.mm(psS[:, 0:256], kupdtok[:, t, :], vtok[:, t, :])
            P.tt("dve", Stmp, psS[:, 0:256], c.bdmask, ALU.mult)
            P.stt("dve", S, S, dec[:, t:t + 1], Stmp, ALU.mult, ALU.add)
            P.cp("act", Sb2[(t + 1) % 2], S)
        am = attm[:, t % 2].sub(t % 2)
        psO = c.psb[6 + t % 2]
        for ch in range(2):
            for j in range(2):
                h = 2 * ch + j
                P.mm(psO[64 * j:64 * j + 64, ch * 128:(ch + 1) * 128], vtok[:, t, 64 * h:64 * h + 64], am[:, h, :],
                     start=True, stop=(t == 0))
            if t > 0:
                P.mm(psO[:, ch * 128:(ch + 1) * 128], Sb2[t % 2][:, ch * 128:(ch + 1) * 128], qe[:, tl], start=False, stop=True)
        P.cp("act", oT[:, :, tl], psO[:, 0:256].re("p (c f) -> p c f", c=2))
    for ch in range(2):
        for tb in range(NB):
            ps = nps()
            sv = sqb[:, tb % 2, :].sub(tb % 2)
            P.act(sv, oT[:, ch, blk(tb)], AF.Square)
            P.mm(ps, c.bd64bf, sv)
            rsqrt(c, tmp[:, blk(tb)], ps, 1.0 / 64.0)
            P.tt("dve", tmp[:, blk(tb)], oT[:, ch, blk(tb)], tmp[:, blk(tb)], ALU.mult)
            P.stt("dve", obr[:, 1, ch, blk(tb)], tmp[:, blk(tb)], c.cst[:, l, C_GLA_GN:C_GLA_GN + 1], rs[:, ch, blk(tb)],
                  ALU.mult, ALU.mult)
    ar.release(m)


def mixer_gdn(c, l, obr):
    P, ar = c.P, c.ar
    m = ar.mark()
    b0 = ar.alloc("b0", [3 + T])
    b1 = ar.alloc("b1", [T])
    b2 = ar.alloc("b2", [T])
    b3 = ar.alloc("b3", [T])
    qnT = ar.alloc("qnT", [T]); knT = ar.alloc("knT", [T])
    ktok = ar.alloc("ktok", [NT, 128]); vtok = ar.alloc("vtok", [NT, 128])
    S1tok = ar.alloc("S1tok", [NT, 96]); S2tok = ar.alloc("S2tok", [NT, 64])
    dgT = ar.alloc("dgT", [T], BF16)
    oT = ar.alloc("oT", [T])
    wsl = ar.alloc("wsl", [3, KC, 128], BF16)
    sqb = ar.alloc("sqb", [2, 512], BF16)
    Acol = ar.alloc("Acol", [1]); negA = ar.alloc("negA", [1])
    Sp = ar.alloc("Spair", [64])
    NCH = 4
    ch_t = []
    for i in range(NCH):
        d = {}
        for nm in ("ea", "eb", "egb", "Xf"):
            d[nm] = ar.alloc("%s%d" % (nm, i), [128])
        for nm in ("Xa", "Xta", "Xb", "Xtb", "Tt", "Aqk", "qdec", "wT"):
            d[nm] = ar.alloc("%s%d" % (nm, i), [128], BF16)
        d["u"] = ar.alloc("u%d" % i, [64])
        for nm in ("vb", "kbe", "kdec", "vnew"):
            d[nm] = ar.alloc("%s%d" % (nm, i), [64], BF16)
        ch_t.append(d)
    knTb = ar.alloc("knTb", [T], BF16); qnTb = ar.alloc("qnTb", [T], BF16)
    Spb = ar.alloc("Spb", [64], BF16)
    SL = Slabs(c, [wsl[:, i].sub(i) for i in range(3)],
               [c.w_in_fm[l, i] for i in (FM_S1, FM_S2, FM_DQ, FM_DK, FM_DV, FM_DG, FM_DQ + 1, FM_DK + 1, FM_DV + 1, FM_DG + 1)])

    def slab(idx):
        return SL.nxt()
    psn = [0]

    def nps():
        p = c.psb[psn[0] % 2]
        psn[0] += 1
        return p
    cs = c.cst
    P.dma("sp", b1, c.rmask_d)
    w = slab(FM_S1)
    for tb in range(NB):
        ps = nps()
        proj_fm(c, w, c.hT, tb, ps)
        P.cp("act", b0[:, blk(tb)], ps)
    P.act(Acol, cs[:, l, C_GDN_ALOG:C_GDN_ALOG + 1], AF.Exp)
    P.ts("pool", negA, Acol, -1.0, ALU.mult)
    z1 = b0[:, 0:T]
    P.act(z1, z1, AF.Exp, bias=cs[:, l, C_GDN_DTB:C_GDN_DTB + 1], scale=1.0)
    P.act(z1, z1, AF.Ln, bias=c.onec, scale=1.0)
    P.ts("dve", z1, z1, negA, ALU.mult)
    P.scan(b2, b1, z1, 0.0, ALU.mult, ALU.add)
    w = slab(FM_S2)
    for tb in range(NB):
        ps = nps()
        proj_fm(c, w, c.hT, tb, ps)
        P.act(b3[:, blk(tb)], ps, AF.Sigmoid)
    g3 = b2[32:64, :].re("p (a b) -> p a b", b=128)
    P.tt("dve", b0[32:64, 0:T].re("p (a b) -> p a b", b=128), g3[:, :, 127:128].bc([32, NT, 128]), g3, ALU.subtract)
    P.act(b2[32:64, :], b0[32:64, 0:T], AF.Exp)
    P.act(b2[64:96, :], b2[64:96, :], AF.Exp)
    P.tt("dve", b2[64:96, :], b2[64:96, :], b3[64:96, :], ALU.mult)
    P.ts("pool", b3[32:64, :], b3[32:64, :], -1.0, ALU.mult)
    for t in range(NT):
        ps = nps()
        P.tr(ps[:, 0:128], b2[:, tile_(t)], c.ident)
        P.cp("act", S1tok[:, t, :], ps[:, 0:96])
        ps = nps()
        P.tr(ps[:, 0:128], b3[:, tile_(t)], c.ident)
        P.cp("act", S2tok[:, t, :], ps[:, 0:64])
    P.memset("pool", b0[:, 0:3], 0.0)
    for pr in range(2):
        for which, fmi, dst in ((0, FM_DQ, qnT), (1, FM_DK, knT), (2, FM_DV, b3)):
            w = slab(fmi + pr)
            for tb in range(NB):
                ps = nps()
                proj_fm(c, w, c.hT, tb, ps)
                P.cp("act", b0[:, 3 + tb * 512:3 + (tb + 1) * 512], ps)
            cc = C_GDN_CONV + (which * 2 + pr) * 4
            P.ts("dve", b1, b0[:, 0:T], cs[:, l, cc:cc + 1], ALU.mult)
            for tap in range(1, 4):
                P.stt("dve", b1, b0[:, tap:tap + T], cs[:, l, cc + tap:cc + tap + 1], b1, ALU.mult, ALU.add)
            P.act(dst, b1, AF.Silu)
            if which < 2:
                for tb in range(NB):
                    ps = nps()
                    sv = sqb[:, tb % 2, :].sub(tb % 2)
                    P.act(sv, dst[:, blk(tb)], AF.Square)
                    P.mm(ps, c.bd64bf, sv)
                    rsqrt(c, b1[:, blk(tb)], ps, 1.0)
                    if which == 0:
                        P.stt("dve", dst[:, blk(tb)], dst[:, blk(tb)], 0.125, b1[:, blk(tb)], ALU.mult, ALU.mult)
                    else:
                        P.tt("dve", dst[:, blk(tb)], dst[:, blk(tb)], b1[:, blk(tb)], ALU.mult)
        P.cp("pool", qnTb, qnT)
        P.cp("pool", knTb, knT)
        w = slab(FM_DG + pr)
        for tb in range(NB):
            ps = nps()
            proj_fm(c, w, c.hT, tb, ps)
            P.act(dgT[:, blk(tb)], ps, AF.Silu)
        for t in range(NT):
            ps = nps()
            P.tr(ps[:, 0:128], knT[:, tile_(t)], c.ident)
            P.cp("act", ktok[:, t, :], ps[:, 0:128])
            ps = nps()
            P.tr(ps[:, 0:128], b3[:, tile_(t)], c.ident)
            P.cp("dve", vtok[:, t, :], ps[:, 0:128])
        P.memset("pool", Sp, 0.0)
        P.memset("pool", Spb, 0.0)
        P.barrier()
        for t0 in range(0, NT, 2):
            chains = []
            for dt_ in range(2):
                for j in range(2):
                    ci = dt_ * 2 + j
                    chains.append((ci, t0 + dt_, j))

            def q_(ci, b, qd):
                bank = c.psb[(ci * 2 + b) % 8]
                return bank[:, qd * 128:(qd + 1) * 128].sub(qd)
            for (ci, t, j) in chains:
                h = 2 * pr + j
                P.mm(q_(ci, 0, 0), c.selA[0:4, h, :], b2[0:4, tile_(t)])
            for (ci, t, j) in chains:
                h = 2 * pr + j; d = ch_t[ci]
                gcol = S1tok[:, t, h:h + 1]
                P.ts("dve", d["ea"], q_(ci, 0, 0), gcol, ALU.subtract, 0.0, ALU.max)
                P.ts("dve", d["eb"], q_(ci, 0, 0), gcol, ALU.subtract, 0.0, ALU.min)
                P.act(d["egb"], q_(ci, 0, 0), AF.Exp)
                P.act(d["ea"], d["ea"], AF.Exp, scale=-1.0)
                P.act(d["eb"], d["eb"], AF.Exp)
            for (ci, t, j) in chains:
                hr = slice(64 * j, 64 * j + 64)
                P.mm(q_(ci, 0, 1), knTb[hr, tile_(t)], knTb[hr, tile_(t)])
                P.mm(q_(ci, 0, 2), knTb[hr, tile_(t)], qnTb[hr, tile_(t)])
            for (ci, t, j) in chains:
                h = 2 * pr + j; d = ch_t[ci]
                P.tt("dve", d["Xf"], q_(ci, 0, 1), d["ea"], ALU.mult)
                P.stt("dve", d["Xf"], d["Xf"], S2tok[:, t, 32 + h:33 + h], c.tri_sl, ALU.mult, ALU.mult)
                P.cp("pool", d["Xa"], d["Xf"])
                P.tt("dve", d["eb"], q_(ci, 0, 2), d["eb"], ALU.mult)
                P.tt("pool", d["Aqk"], d["eb"], c.tri_ui, ALU.mult)
            for (ci, t, j) in chains:
                d = ch_t[ci]
                P.tr(q_(ci, 0, 3), d["Xf"], c.ident)
            for (ci, t, j) in chains:
                d = ch_t[ci]
                P.cp("act", d["Xta"], q_(ci, 0, 3))
                P.tt("dve", d["Tt"], q_(ci, 0, 3), c.ident, ALU.add)
            cur = {ci: ("Xa", "Xta") for ci in range(NCH)}
            for it in range(6):
                for (ci, t, j) in chains:
                    d = ch_t[ci]; xc, xtc = cur[ci]
                    P.mm(q_(ci, 1, 0), d[xtc], d[xc])
                    if it < 5:
                        P.mm(q_(ci, 1, 1), d[xc], d[xtc])
                for (ci, t, j) in chains:
                    d = ch_t[ci]; xc, xtc = cur[ci]
                    nx, nxt = ("Xb", "Xtb") if xc == "Xa" else ("Xa", "Xta")
                    P.cp("act", d[nx], q_(ci, 1, 0))
                    if it < 5:
                        P.cp("dve", d[nxt], q_(ci, 1, 1))
                    cur[ci] = (nx, nxt)
                for (ci, t, j) in chains:
                    d = ch_t[ci]; xc, xtc = cur[ci]
                    P.mm(q_(ci, 1, 2), d[xc], d["Tt"])
                for (ci, t, j) in chains:
                    d = ch_t[ci]
                    P.tt("dve", d["Tt"], d["Tt"], q_(ci, 1, 2), ALU.add)
            for (ci, t, j) in chains:
                h = 2 * pr + j; d = ch_t[ci]
                hc = slice(64 * j, 64 * j + 64)
                P.ts("pool", d["vb"], vtok[:, t, hc], S2tok[:, t, h:h + 1], ALU.mult)
                P.ts("pool", d["kbe"], ktok[:, t, hc], S1tok[:, t, 64 + h:65 + h], ALU.mult)
                P.ts("pool", d["kdec"], ktok[:, t, hc], S1tok[:, t, 32 + h:33 + h], ALU.mult)
                P.tt("pool", d["qdec"][hc, :], qnT[hc, tile_(t)], d["egb"][hc, :], ALU.mult)
            for (ci, t, j) in chains:
                d = ch_t[ci]
                hr = slice(64 * j, 64 * j + 64)
                P.mm(q_(ci, 1, 3)[:, 0:64], d["Tt"], d["vb"])
                P.mm(q_(ci, 0, 0)[hr, :], d["kbe"], d["Tt"])
            for (ci, t, j) in chains:
                d = ch_t[ci]
                hr = slice(64 * j, 64 * j + 64)
                P.cp("act", d["u"], q_(ci, 1, 3)[:, 0:64])
                P.cp("dve", d["wT"][hr, :], q_(ci, 0, 0)[hr, :])
            for (ci, t, j) in chains:
                d = ch_t[ci]
                hr = slice(64 * j, 64 * j + 64)
                P.mm(q_(ci, 0, 1)[:, 0:64], d["wT"][hr, :], Spb[hr, :].sub(j))
                P.tt("dve", d["vnew"], d["u"], q_(ci, 0, 1)[:, 0:64], ALU.subtract)
                P.mm(q_(ci, 0, 2)[hr, :], Spb[hr, :].sub(j), d["qdec"][hr, :], start=True, stop=False)
                P.mm(q_(ci, 0, 2)[hr, :], d["vnew"], d["Aqk"], start=False, stop=True)
                P.cp("act", oT[hr, tile_(t)].sub(j), q_(ci, 0, 2)[hr, :])
                P.mm(q_(ci, 0, 3)[hr, 0:64], d["kdec"], d["vnew"])
                P.stt("dve", Sp[hr, :].sub(j), Sp[hr, :].sub(j), d["egb"][hr, 127:128], q_(ci, 0, 3)[hr, 0:64], ALU.mult, ALU.add)
                P.cp("act", Spb[hr, :].sub(j), Sp[hr, :].sub(j))
        P.barrier()
        for tb in range(NB):
            ps = nps()
            sv = sqb[:, tb % 2, :].sub(tb % 2)
            P.act(sv, oT[:, blk(tb)], AF.Square)
            P.mm(ps, c.bd64bf, sv)
            rsqrt(c, b1[:, blk(tb)], ps, 1.0 / 64.0)
            P.tt("dve", b1[:, blk(tb)], oT[:, blk(tb)], b1[:, blk(tb)], ALU.mult)
            P.stt("dve", obr[:, 2, pr, blk(tb)], b1[:, blk(tb)], cs[:, l, C_GDN_GN:C_GDN_GN + 1], dgT[:, blk(tb)],
                  ALU.mult, ALU.mult)
    ar.release(m)


TINY = 1e-30


def mixer_nsa(c, l, obr):
    P, ar = c.P, c.ar
    m = ar.mark()
    gT = ar.alloc("gT", [T])
    kcT = ar.alloc("kcT", [T], BF16); vcT = ar.alloc("vcT", [T], BF16)
    w1 = ar.alloc("w1", [32, 128], BF16)
    peT = ar.alloc("peT", [32, 8], BF16)
    kcR = ar.alloc("kcR", [16, 128], BF16)
    w2d = ar.alloc("w2d", [2, 128], BF16); w2v = ar.alloc("w2v", [128], BF16)
    b1c = ar.alloc("b1c", [1])
    gl = ar.alloc("gl", [128], BF16)
    ckdup = ar.alloc("ckdup", [2, 128], BF16)
    cvtok = ar.alloc("cvtok", [128], BF16)
    qT = ar.alloc("qT", [2, T], BF16)
    ksd = ar.alloc("ksd", [2, T], BF16); kwd = ar.alloc("kwd", [2, T], BF16)
    vstok = ar.alloc("vstok", [NT, 128], BF16); vwtok = ar.alloc("vwtok", [NT, 128], BF16)
    selT = ar.alloc("selT", [T], BF16)
    cmask = ar.alloc("cmask", [T], BF16); P.dma("pool", cmask, c.cmask_d)
    Esel = ar.alloc("Esel", [T], BF16); P.dma("pool", Esel, c.Esel_d)
    Wlong = ar.alloc("Wlong", [1408], BF16); P.dma("pool", Wlong, c.Wlong_d)
    cover1 = ar.alloc("cover1", [33], BF16); P.dma("pool", cover1, c.cover1_d)
    keepT = ar.alloc("keepT", [NT, 32]); P.dma("sp", keepT, c.keepT_d)
    addT = ar.alloc("addT", [NT, 32]); P.dma("sp", addT, c.addT_d)
    validT = ar.alloc("validT", [NT, 32]); P.dma("sp", validT, c.validT_d)
    selg = ar.alloc("selg", [2, 3, 128]); P.dma("sp", selg, c.selg_d)
    wsl = ar.alloc("wsl", [3, KC, 128], BF16)
    wtm = ar.alloc("wtm", [KC, 256], BF16)
    SL = Slabs(c, [wsl[:, i].sub(i) for i in range(3)],
               [c.w_in_fm[l, FM_S3], c.w_in_fm[l, FM_NKC], c.w_in_fm[l, FM_NVC], c.w_in_fm[l, FM_NQ], c.w_in_fm[l, FM_NQ + 1],
                c.nsadup_d[l, 0], c.nsadup_d[l, 1], c.nsadup_d[l, 2], c.nsadup_d[l, 3]])
    wdma(c, wtm, c.w_in_tm[l, :, :, 256:512])
    ee = ar.alloc("ee", [3, 2, 512], BF16)
    em = ar.alloc("em", [3, 2, 512], BF16)
    oacc = ar.alloc("oacc", [T])
    rt = ar.alloc("rt", [2, 512])
    im = {nm: ar.alloc(nm, [32]) for nm in ("imp", "vals", "wa", "wb", "sel")}
    m8 = ar.alloc("m8", [8]); rden = ar.alloc("rden", [2])
    nw = [0]

    def slab(src):
        return SL.nxt()
    psn = [0]

    def nps():
        p = c.psb[psn[0] % 2]
        psn[0] += 1
        return p
    w = slab(c.w_in_fm[l, FM_S3])
    for tb in range(NB):
        ps = nps()
        proj_fm(c, w, c.hT, tb, ps)
        P.act(gT[:, blk(tb)], ps, AF.Sigmoid)
    for fmi, dst in ((FM_NKC, kcT), (FM_NVC, vcT)):
        w = slab(c.w_in_fm[l, fmi])
        for tb in range(NB):
            ps = nps()
            proj_fm(c, w, c.hT, tb, ps)
            P.cp("act", dst[:, blk(tb)], ps)
    P.dma("pool", w2d, c.w2dup_d[l].re("g p m -> p g m"))
    P.dma("pool", w2v, c.w2vbd_d[l])
    P.memset("pool", gl, 0.0)
    P.memset("pool", ckdup, 0.0)
    for kv, src in ((0, kcT), (1, vcT)):
        P.dma("pool", w1, c.w1bd_d[l, kv])
        P.dma("pool", peT, c.peT_d[l, kv])
        P.cp("dve", kcR, src.re("p (c r) -> p r c", r=16))
        psC = nps()
        psB = nps()
        for li in range(32):
            s_, r_ = li // 16, li % 16
            P.mm(psC[:, 0:127], w1[:, li, :], kcR[:, r_, s_:s_ + 127], start=(li == 0), stop=(li == 31))
        for li in range(32):
            P.mm(psB[:, 0:8], w1[:, li, :], peT[:, li, :], start=(li == 0), stop=(li == 31))
        P.cp("dve", b1c, psB[:, 0:1])
        P.act(gl[:, 0:127], psC[:, 0:127], AF.Gelu_apprx_tanh, bias=b1c, scale=1.0)
        if kv == 0:
            for g in range(2):
                ps = nps()
                P.mm(ps[:, 0:128], w2d[:, g, :], gl)
                P.cp("act", ckdup[:, g, :], ps[:, 0:128])
        else:
            ps = nps()
            P.mm(ps[:, 0:128], gl, w2v)
            P.cp("act", cvtok, ps[:, 0:128])
    for g in range(2):
        w = slab(c.w_in_fm[l, FM_NQ + g])
        for tb in range(NB):
            ps = nps()
            proj_fm(c, w, c.hT, tb, ps)
            P.ts("dve", qT[:, g, blk(tb)], ps, 0.125, ALU.mult)
    for i, dst in ((0, ksd), (2, kwd)):
        for g in range(2):
            w = slab(c.nsadup_d[l, i + g])
            for tb in range(NB):
                ps = nps()
                proj_fm(c, w, c.hT, tb, ps)
                P.cp("act", dst[:, g, blk(tb)], ps)
    for t in range(NT):
        ps = nps()
        for k in range(KC):
            P.mm(ps[:, 0:256], c.hT[:, k, tile_(t)], wtm[:, k, :], start=(k == 0), stop=(k == KC - 1))
        P.cp("act", vstok[:, t, :], ps[:, 0:128])
        P.cp("dve", vwtok[:, t, :], ps[:, 128:256])
    P.barrier()
    lvl = getattr(c, "nsa_level", 9)
    if "nsa1" in c.dbg_d:
        P.dma("pool", c.dbg_d["nsa1"][:, 0:256], ckdup.re("p g n -> p (g n)"))
        P.dma("pool", c.dbg_d["nsa1"][:, 256:384], cvtok)
    if lvl < 2:
        ar.release(m)
        return
    psb = c.psb
    nrot = [0]

    def combine(g, tb, br, psO, psD, first):
        psG = psb[4]
        P.mm(psG, selg[0:12, g, br, :], gT[0:12, blk(tb)])
        r = rt[:, 0, :].sub(0)
        r2 = rt[:, 1, :].sub(1)
        P.ts("dve", r, psD, TINY, ALU.max)
        P.op("dve", (lambda oa: lambda e: e.reciprocal(oa, oa))(r.ap), _k(r), _k(r))
        P.tt("dve", r, r, psG, ALU.mult)
        if first:
            P.tt("dve", oacc[:, blk(tb)], psO, r, ALU.mult)
        else:
            P.tt("dve", r2, psO, r, ALU.mult)
            P.tt("pool", oacc[:, blk(tb)], oacc[:, blk(tb)], r2, ALU.add)

    for g in range(2):
        for tb in range(NB):
            psO, psD = psb[6], psb[7]
            rot = nrot[0] % 2
            nrot[0] += 1
            for j in range(2):
                hr = slice(64 * j, 64 * j + 64)
                psS = psb[j]
                P.mm(psS, ckdup[hr, g, :], qT[hr, g, blk(tb)])
                e = ee[:, rot, j, :].sub((rot, j))
                P.act(e, psS, AF.Exp)
                P.tt("pool" if j else "dve", e, e, cmask[:, blk(tb)], ALU.mult)
                P.mm(psO[hr, :], cvtok[:, 64 * g:64 * g + 64], e)
                P.mm(psD[hr, :], c.onesbf[:, 0:64], e)
            for t4 in range(4):
                qt = tb * 4 + t4
                psI = psb[2 + t4 % 2]
                for j in range(2):
                    e = ee[:, rot, j, t4 * 128:(t4 + 1) * 128].sub((rot, j))
                    P.mm(psI[:, j * 64:j * 64 + 33], e, cover1)
                pI = psI[:, 0:128].re("p (j f) -> p j f", j=2)
                P.ts("dve", rden, pI[:, :, 32], TINY, ALU.max)
                P.op("dve", (lambda oa: lambda e_: e_.reciprocal(oa, oa))(rden.ap), _k(rden), _k(rden))
                P.ts("dve", im["imp"], psI[:, 0:32], rden[:, 0:1], ALU.mult)
                P.stt("dve", im["imp"], psI[:, 64:96], rden[:, 1:2], im["imp"], ALU.mult, ALU.add)
                P.tt("dve", im["vals"], im["imp"], keepT[:, qt, :], ALU.mult)
                P.tt("dve", im["vals"], im["vals"], addT[:, qt, :], ALU.add)
                va, wa, wb = im["vals"].ap, im["wa"].ap, im["wb"].ap
                m8a = m8.ap
                P.op("dve", lambda e_: e_.max(out=m8a, in_=va), _k(im["vals"]), _k(m8))
                P.op("dve", lambda e_: e_.match_replace(out=wa, in_to_replace=m8a, in_values=va, imm_value=-2.0),
                     _k(im["vals"], m8), _k(im["wa"]))
                P.op("dve", lambda e_: e_.max(out=m8a, in_=wa), _k(im["wa"]), _k(m8))
                P.op("dve", lambda e_: e_.match_replace(out=wb, in_to_replace=m8a, in_values=wa, imm_value=-2.0),
                     _k(im["wa"], m8), _k(im["wb"]))
                P.tt("dve", im["wb"], im["vals"], im["wb"], ALU.subtract)
                P.stt("dve", im["sel"], im["wb"], 0.0, validT[:, qt, :], ALU.is_gt, ALU.mult)
                psT = psb[4 + t4 % 2]
                P.tr(psT[0:32, 0:128], im["sel"], c.ident)
                P.cp("act", selT[0:32, tile_(qt)], psT[0:32, 0:128])
            combine(g, tb, 0, psO, psD, True)
        P.barrier()
        if "nsa2" in c.dbg_d and g == 0:
            P.dma("pool", c.dbg_d["nsa2"], selT[0:32, :])
        if lvl < 3:
            continue
        for br, kd, vtok_ in ((1, ksd, vstok), (2, kwd, vwtok)):
            for tb in range(NB):
                psO, psD = psb[6], psb[7]
                kts = list(range(0, 4 * tb + 4)) if br == 1 else list(range(max(0, 4 * tb - 4), 4 * tb + 4))
                nk = len(kts)
                depth = 2 if br == 1 else 3
                rots = []
                for ki in range(nk):
                    rots.append(nrot[0] % depth)
                    nrot[0] += 1

                def stage1(ki):
                    kt = kts[ki]
                    rot = rots[ki]
                    rel = kt - 4 * tb
                    if br == 1:
                        psM = psb[4 + rot]
                        P.mm(psM, Esel[0:32, tile_(kt)], selT[0:32, blk(tb)])
                    for j in range(2):
                        hr = slice(64 * j, 64 * j + 64)
                        psS = psb[2 * rot + j]
                        P.mm(psS, kd[hr, g, tile_(kt)], qT[hr, g, blk(tb)])
                        e = ee[:, rot, j, :].sub((rot, j))
                        P.act(e, psS, AF.Exp)
                        e2 = em[:, rot, j, :].sub((rot, j))
                        if br == 1:
                            P.tt("dve", e2, e, psM, ALU.mult)
                            if rel >= 0:
                                off = 384 - 128 * rel
                                P.tt("pool", e2, e2, Wlong[:, off:off + 512], ALU.mult)
                        else:
                            off = 384 - 128 * rel
                            P.tt("pool" if j else "dve", e2, e, Wlong[:, off:off + 512], ALU.mult)

                def stage2(ki):
                    kt = kts[ki]
                    rot = rots[ki]
                    for j in range(2):
                        hr = slice(64 * j, 64 * j + 64)
                        e2 = em[:, rot, j, :].sub((rot, j))
                        P.mm(psO[hr, :], vtok_[:, kt, 64 * g:64 * g + 64], e2, start=(ki == 0), stop=(ki == nk - 1))
                        P.mm(psD[hr, :], c.onesbf[:, 0:64], e2, start=(ki == 0), stop=(ki == nk - 1))
                for step in range(nk + depth - 1):
                    if step < nk:
                        stage1(step)
                    if step >= depth - 1:
                        stage2(step - (depth - 1))
                combine(g, tb, br, psO, psD, False)
        P.barrier()
        for tb in range(NB):
            P.cp("act", obr[:, 3, g, blk(tb)], oacc[:, blk(tb)])
    ar.release(m)


NCORES = 8
SEQ_PER_CORE = 4


def _to_fm(a):
    n, t, d = a.shape
    return np.ascontiguousarray(a.transpose(0, 2, 1).reshape(n, KC, 128, t).transpose(0, 2, 1, 3))


def kernel(**inputs):
    inp = {k: np.asarray(v) for k, v in inputs.items()}
    w = host_prep(inp)
    nc, c = build(nseq=SEQ_PER_CORE, nlayers=L)
    x = inp["x"].astype(np.float32, copy=False)
    mem = inp["mem"].astype(np.float32, copy=False)
    in_maps = []
    for core in range(NCORES):
        sl = slice(core * SEQ_PER_CORE, (core + 1) * SEQ_PER_CORE)
        m = dict(w)
        m["xT"] = _to_fm(x[sl])
        m["memT"] = _to_fm(mem[sl])
        in_maps.append(m)
    res = run_bass_kernel_spmd(nc, in_maps, core_ids=list(range(NCORES)))
    out = np.empty((NCORES * SEQ_PER_CORE, T, D), np.float32)
    for core in range(NCORES):
        oT = res.results[core]["outT"]
        for s in range(SEQ_PER_CORE):
            out[core * SEQ_PER_CORE + s] = oT[s].transpose(2, 1, 0).reshape(T, D)
    return out
```
